# Optimizing a Trainium2 kernel written in Bass

```python
import jax, jax.numpy as jnp
from jax import lax
import numpy as np

D_MODEL = 2048
BATCH = 4
SEQ = 8192
DEPTH = 4

EPS = 1e-6
N_BRANCH = 3
BRANCH_W = 1024

GLA_HEADS = 4
GLA_DK = 256
GLA_DV = 256
GLA_KW = GLA_HEADS * GLA_DK
GLA_VW = GLA_HEADS * GLA_DV
GLA_RANK = 16
GLA_TAU = 16.0
GLA_CHUNK = 64

NSA_HEADS = 16
NSA_GROUPS = 2
NSA_HPG = NSA_HEADS // NSA_GROUPS
NSA_DH = 64
NSA_QW = NSA_HEADS * NSA_DH
NSA_KVW = NSA_GROUPS * NSA_DH
NSA_CMP_LEN = 32
NSA_CMP_STRIDE = 16
NSA_SEL_BLOCK = 64
NSA_TOPN = 16
NSA_WINDOW = 512
NSA_QBLOCK = 128
BIG = 1e30

SSM_HEADS = 16
SSM_HEADDIM = 64
SSM_INNER = SSM_HEADS * SSM_HEADDIM
SSM_GROUPS = 4
SSM_HPG = SSM_HEADS // SSM_GROUPS
SSM_STATE = 128
SSM_CONV = 4
SSM_CONV_CH = SSM_INNER + 2 * SSM_GROUPS * SSM_STATE
SSM_CHUNK = 64

D_FF = ((8 * D_MODEL // 3 + 255) // 256) * 256

IN_SIZES = (GLA_KW, GLA_KW, GLA_VW, GLA_VW, GLA_RANK,
            NSA_QW, 6 * NSA_KVW, 3 * NSA_HEADS,
            SSM_INNER, SSM_CONV_CH, SSM_HEADS,
            N_BRANCH * D_MODEL)
D_IN = sum(IN_SIZES)
SPLIT_POINTS = tuple(int(v) for v in np.cumsum(IN_SIZES)[:-1])

kernel_name = 'hybrid_gla_nsa_ssd_trunk'


def rms_norm(x, g):
    xf = x.astype(jnp.float32)
    y = xf * lax.rsqrt(jnp.mean(xf * xf, axis=-1, keepdims=True) + EPS)
    return (y * g).astype(x.dtype)


def masked_softmax(s, mask):
    s = jnp.where(mask, s.astype(jnp.float32), -BIG)
    p = jax.nn.softmax(s, axis=-1)
    return jnp.where(mask, p, 0.0)


def alibi_slopes(n):
    return jnp.asarray(2.0 ** (-8.0 * np.arange(1, n + 1) / n), jnp.float32)


def causal_depthwise_conv(x, w, b):
    K = w.shape[0]
    y = lax.conv_general_dilated(x, w[:, None, :], window_strides=(1,), padding=((K - 1, 0),),
                                 dimension_numbers=('NWC', 'WIO', 'NWC'),
                                 feature_group_count=x.shape[-1])
    return y + b


def gla_mixer(q, k, v, r, a_low, w_a2, b_a, norm_g):
    f32 = jnp.float32
    bsz, T, _ = q.shape
    C = GLA_CHUNK
    n = T // C
    log_alpha = jax.nn.log_sigmoid((a_low @ w_a2 + b_a).astype(f32)) / GLA_TAU

    def chunks(z, d):
        return z.reshape(bsz, n, C, GLA_HEADS, d).transpose(0, 3, 1, 2, 4).astype(f32)

    qc = chunks(q, GLA_DK) * (GLA_DK ** -0.5)
    kc = chunks(k, GLA_DK)
    vc = chunks(v, GLA_DV)
    bcum = jnp.cumsum(chunks(log_alpha, GLA_DK), axis=3)
    b_last = bcum[:, :, :, -1:, :]
    q_dec = qc * jnp.exp(bcum)
    k_inv = kc * jnp.exp(-bcum)
    k_end = kc * jnp.exp(b_last - bcum)
    causal = jnp.tril(jnp.ones((C, C), dtype=bool))
    att = jnp.where(causal, jnp.einsum('bhncd,bhnsd->bhncs', q_dec, k_inv), 0.0)
    o_intra = jnp.einsum('bhncs,bhnse->bhnce', att, vc)

    def step(S, inp):
        qd, ke, vv, dl = inp
        o = jnp.einsum('bhcd,bhde->bhce', qd, S)
        S = jnp.exp(dl)[..., None] * S + jnp.einsum('bhcd,bhce->bhde', ke, vv)
        return S, o

    S0 = jnp.zeros((bsz, GLA_HEADS, GLA_DK, GLA_DV), f32)
    xs = (q_dec.transpose(2, 0, 1, 3, 4), k_end.transpose(2, 0, 1, 3, 4),
          vc.transpose(2, 0, 1, 3, 4), b_last[:, :, :, 0, :].transpose(2, 0, 1, 3))
    _, o_inter = lax.scan(step, S0, xs)
    o = o_intra + o_inter.transpose(1, 2, 0, 3, 4)
    o = o.transpose(0, 2, 3, 1, 4).reshape(bsz, T, GLA_HEADS, GLA_DV)
    o = rms_norm(o, norm_g).reshape(bsz, T, GLA_VW) * jax.nn.silu(r.astype(f32))
    return o.astype(q.dtype)


def nsa_mixer(q, kv, gate_logits, cmp_pos, cmp_w1, cmp_w2):
    f32 = jnp.float32
    bsz, T, _ = q.shape
    G, Hg, dh = NSA_GROUPS, NSA_HPG, NSA_DH
    q = q.reshape(bsz, T, G, Hg, dh).astype(f32) * (dh ** -0.5)
    k_c, v_c, k_s, v_s, k_w, v_w = jnp.split(kv, 6, axis=-1)

    def heads(z):
        return z.reshape(bsz, T, G, dh).transpose(0, 2, 1, 3).astype(f32)

    n_cmp = (T - NSA_CMP_LEN) // NSA_CMP_STRIDE + 1
    cmp_start = np.arange(n_cmp) * NSA_CMP_STRIDE
    tok_idx = cmp_start[:, None] + np.arange(NSA_CMP_LEN)[None, :]
    cmp_end = jnp.asarray(cmp_start + NSA_CMP_LEN - 1, f32)

    def compress(z, j):
        blocks = heads(z)[:, :, tok_idx] + cmp_pos[j]
        flat = blocks.reshape(bsz, G, n_cmp, NSA_CMP_LEN * dh)
        return jax.nn.silu(flat @ cmp_w1[j]) @ cmp_w2[j]

    kc = compress(k_c, 0)
    vc = compress(v_c, 1)

    n_sel = T // NSA_SEL_BLOCK
    n_top = min(NSA_TOPN, n_sel)
    ks = heads(k_s).reshape(bsz, G, n_sel, NSA_SEL_BLOCK, dh)
    vs = heads(v_s).reshape(bsz, G, n_sel, NSA_SEL_BLOCK, dh)
    overlap = np.zeros((n_cmp, n_sel), np.float32)
    np.add.at(overlap, (np.repeat(np.arange(n_cmp), NSA_CMP_LEN), (tok_idx // NSA_SEL_BLOCK).ravel()),
              1.0 / NSA_CMP_LEN)
    overlap = jnp.asarray(overlap)
    blk = jnp.arange(n_sel)

    pad = ((0, 0), (0, 0), (NSA_WINDOW, 0), (0, 0))
    kw = jnp.pad(heads(k_w), pad)
    vw = jnp.pad(heads(v_w), pad)

    gates = jax.nn.sigmoid(gate_logits.astype(f32)).reshape(bsz, T, 3, G, Hg)
    QB = NSA_QBLOCK
    nq = T // QB
    q_blk = q.reshape(bsz, nq, QB, G, Hg, dh).transpose(1, 0, 3, 4, 2, 5)
    g_blk = gates.reshape(bsz, nq, QB, 3, G, Hg).transpose(1, 3, 0, 4, 5, 2)
    slopes = alibi_slopes(NSA_HEADS).reshape(1, G, Hg, 1, 1)
    b_ix = jnp.arange(bsz)[:, None, None, None]
    g_ix = jnp.arange(G)[None, :, None, None]

    def one_block(inp):
        ci, qb, gb = inp
        t = ci * QB + jnp.arange(QB)
        tf = t.astype(f32)
        dist_c = tf[:, None] - cmp_end[None, :]
        s_c = jnp.einsum('bghqd,bgnd->bghqn', qb, kc) - slopes * dist_c
        p_c = masked_softmax(s_c, dist_c >= 0)
        o_cmp = jnp.einsum('bghqn,bgnd->bghqd', p_c, vc)
        imp = jnp.einsum('bghqn,ns->bgqs', p_c, overlap)
        cur = t // NSA_SEL_BLOCK
        forced = (blk[None, :] == 0) | (blk[None, :] == cur[:, None]) | (blk[None, :] == cur[:, None] - 1)
        future = blk[None, :] * NSA_SEL_BLOCK > t[:, None]
        imp = jnp.where(forced, BIG, jnp.where(future, -BIG, imp))
        _, idx = lax.top_k(imp, n_top)
        k_sel = ks[b_ix, g_ix, idx].reshape(bsz, G, QB, n_top * NSA_SEL_BLOCK, dh)
        v_sel = vs[b_ix, g_ix, idx].reshape(bsz, G, QB, n_top * NSA_SEL_BLOCK, dh)
        pos = (idx[..., None] * NSA_SEL_BLOCK + jnp.arange(NSA_SEL_BLOCK)).reshape(bsz, G, QB, -1)
        d_s = t[None, None, :, None] - pos
        s_s = jnp.einsum('bghqd,bgqsd->bghqs', qb, k_sel) - slopes * d_s[:, :, None].astype(f32)
        p_s = masked_softmax(s_s, (d_s >= 0)[:, :, None])
        o_sel = jnp.einsum('bghqs,bgqsd->bghqd', p_s, v_sel)
        k_win = lax.dynamic_slice_in_dim(kw, ci * QB, QB + NSA_WINDOW, axis=2)
        v_win = lax.dynamic_slice_in_dim(vw, ci * QB, QB + NSA_WINDOW, axis=2)
        pos_w = ci * QB - NSA_WINDOW + jnp.arange(QB + NSA_WINDOW)
        d_w = t[:, None] - pos_w[None, :]
        mask_w = (d_w >= 0) & (d_w < NSA_WINDOW) & (pos_w >= 0)[None, :]
        s_w = jnp.einsum('bghqd,bgkd->bghqk', qb, k_win) - slopes * d_w.astype(f32)
        p_w = masked_softmax(s_w, mask_w)
        o_win = jnp.einsum('bghqk,bgkd->bghqd', p_w, v_win)
        return gb[0][..., None] * o_cmp + gb[1][..., None] * o_sel + gb[2][..., None] * o_win

    out = lax.map(one_block, (jnp.arange(nq), q_blk, g_blk))
    out = out.transpose(1, 0, 4, 2, 3, 5).reshape(bsz, T, NSA_QW)
    return out.astype(kv.dtype)


def ssd_mixer(z, xbc, dt_raw, conv_w, conv_b, dt_bias, a_log, d_skip, norm_g):
    f32 = jnp.float32
    bsz, T, _ = z.shape
    G, Hp, P, N, L = SSM_GROUPS, SSM_HPG, SSM_HEADDIM, SSM_STATE, SSM_CHUNK
    nc = T // L
    xbc = jax.nn.silu(causal_depthwise_conv(xbc, conv_w, conv_b).astype(f32))
    xs, bm, cm = jnp.split(xbc, [SSM_INNER, SSM_INNER + G * N], axis=-1)
    x = xs.reshape(bsz, nc, L, G, Hp, P)
    bc = bm.reshape(bsz, nc, L, G, N)
    cc = cm.reshape(bsz, nc, L, G, N)
    dt = jax.nn.softplus(dt_raw.astype(f32) + dt_bias.astype(f32)).reshape(bsz, nc, L, G, Hp)
    a = dt * (-jnp.exp(a_log.astype(f32))).reshape(G, Hp)
    cum = jnp.cumsum(a, axis=2)
    cum_last = cum[:, :, -1]
    xdt = x * dt[..., None]
    cum_t = jnp.moveaxis(cum, 2, -1)
    seg = cum_t[..., :, None] - cum_t[..., None, :]
    causal = jnp.tril(jnp.ones((L, L), dtype=bool))
    decay = jnp.exp(jnp.where(causal, seg, -jnp.inf))
    cb = jnp.einsum('bnlgs,bnmgs->bnglm', cc, bc)
    y_intra = jnp.einsum('bnghlm,bnmghp->bnlghp', cb[:, :, :, None] * decay, xdt)
    dec_in = jnp.exp(cum)
    dec_end = jnp.exp(cum_last[:, :, None] - cum)

    def step(S, inp):
        c_n, b_n, xdt_n, din_n, dend_n, dl_n = inp
        y = jnp.einsum('blgs,bghps->blghp', c_n, S) * din_n[..., None]
        S = jnp.exp(dl_n)[..., None, None] * S + jnp.einsum('blgh,blgs,blghp->bghps', dend_n, b_n, xdt_n)
        return S, y

    def lead(arr):
        return jnp.moveaxis(arr, 1, 0)

    S0 = jnp.zeros((bsz, G, Hp, P, N), f32)
    _, y_inter = lax.scan(step, S0, (lead(cc), lead(bc), lead(xdt), lead(dec_in), lead(dec_end), lead(cum_last)))
    y = y_intra + jnp.moveaxis(y_inter, 0, 1) + x * d_skip.astype(f32).reshape(G, Hp, 1)
    y = y.reshape(bsz, T, SSM_INNER) * jax.nn.silu(z.astype(f32))
    y = rms_norm(y.reshape(bsz, T, G, SSM_INNER // G), norm_g.reshape(G, SSM_INNER // G))
    return y.reshape(bsz, T, SSM_INNER).astype(z.dtype)


def setup_inputs(seed: int = 0) -> dict:
    key = jax.random.key(seed)
    k = jax.random.split(key, 24)
    f32 = jnp.float32
    L, D = DEPTH, D_MODEL

    def dense(kk, shape, fan_in):
        return jax.random.normal(kk, shape, f32) * (fan_in ** -0.5)

    def gain(kk, shape):
        return 1.0 + 0.02 * jax.random.normal(kk, shape, f32)

    def small(kk, shape):
        return 0.02 * jax.random.normal(kk, shape, f32)

    dt0 = jnp.exp(jax.random.uniform(k[9], (L, SSM_HEADS), f32, np.log(1e-3), np.log(1e-1)))
    return {
        'x': jax.random.normal(k[0], (BATCH, SEQ, D), f32),
        'w_in': dense(k[1], (L, D, D_IN), D),
        'gla_a2': dense(k[2], (L, GLA_RANK, GLA_KW), GLA_RANK),
        'gla_a_bias': small(k[3], (L, GLA_KW)),
        'gla_norm': gain(k[4], (L, GLA_DV)),
        'nsa_cmp_pos': small(k[5], (L, 2, NSA_CMP_LEN, NSA_DH)),
        'nsa_cmp_w1': dense(k[6], (L, 2, NSA_CMP_LEN * NSA_DH, NSA_DH), NSA_CMP_LEN * NSA_DH),
        'nsa_cmp_w2': dense(k[7], (L, 2, NSA_DH, NSA_DH), NSA_DH),
        'ssm_conv_w': dense(k[8], (L, SSM_CONV, SSM_CONV_CH), SSM_CONV),
        'ssm_conv_b': small(k[10], (L, SSM_CONV_CH)),
        'ssm_dt_bias': dt0 + jnp.log(-jnp.expm1(-dt0)),
        'ssm_a_log': jnp.log(jax.random.uniform(k[11], (L, SSM_HEADS), f32, 1.0, 16.0)),
        'ssm_d': 1.0 + 0.1 * jax.random.normal(k[12], (L, SSM_HEADS), f32),
        'ssm_norm': gain(k[13], (L, SSM_INNER)),
        'w_branch': dense(k[14], (L, N_BRANCH, BRANCH_W, D), BRANCH_W),
        'w_out': dense(k[15], (L, D, D), D),
        'norm_pre_mix': gain(k[16], (L, D)),
        'norm_post_mix': gain(k[17], (L, D)),
        'norm_pre_ffn': gain(k[18], (L, D)),
        'norm_post_ffn': gain(k[19], (L, D)),
        'w_ffn_gate': dense(k[20], (L, D, D_FF), D),
        'w_ffn_up': dense(k[21], (L, D, D_FF), D),
        'w_ffn_down': dense(k[22], (L, D_FF, D), D_FF),
    }


def reference(x, w_in, gla_a2, gla_a_bias, gla_norm, nsa_cmp_pos, nsa_cmp_w1, nsa_cmp_w2,
              ssm_conv_w, ssm_conv_b, ssm_dt_bias, ssm_a_log, ssm_d, ssm_norm,
              w_branch, w_out, norm_pre_mix, norm_post_mix, norm_pre_ffn, norm_post_ffn,
              w_ffn_gate, w_ffn_up, w_ffn_down):
    bsz, T, D = x.shape
    for l in range(DEPTH):
        h = rms_norm(x, norm_pre_mix[l])
        proj = h @ w_in[l]
        (g_q, g_k, g_v, g_r, g_a, n_q, n_kv, n_g,
         s_z, s_xbc, s_dt, m_g) = jnp.split(proj, SPLIT_POINTS, axis=-1)
        y_gla = gla_mixer(g_q, g_k, g_v, g_r, g_a, gla_a2[l], gla_a_bias[l], gla_norm[l])
        y_nsa = nsa_mixer(n_q, n_kv, n_g, nsa_cmp_pos[l], nsa_cmp_w1[l], nsa_cmp_w2[l])
        y_ssm = ssd_mixer(s_z, s_xbc, s_dt, ssm_conv_w[l], ssm_conv_b[l], ssm_dt_bias[l],
                          ssm_a_log[l], ssm_d[l], ssm_norm[l])
        gate = jax.nn.sigmoid(m_g.astype(jnp.float32)).reshape(bsz, T, N_BRANCH, D)
        merged = (gate[:, :, 0] * (y_gla @ w_branch[l, 0])
                  + gate[:, :, 1] * (y_nsa @ w_branch[l, 1])
                  + gate[:, :, 2] * (y_ssm @ w_branch[l, 2])).astype(x.dtype)
        x = x + rms_norm(merged @ w_out[l], norm_post_mix[l])
        h = rms_norm(x, norm_pre_ffn[l])
        f = (jax.nn.silu(h @ w_ffn_gate[l]) * (h @ w_ffn_up[l])) @ w_ffn_down[l]
        x = x + rms_norm(f, norm_post_ffn[l])
    return x
```

```python
import numpy as np
import ml_dtypes
from contextlib import ExitStack
import concourse.bass as bass
import concourse.mybir as mybir
from concourse.bass_utils import run_bass_kernel_spmd

F32 = mybir.dt.float32
BF16 = mybir.dt.bfloat16
ALU = mybir.AluOpType
AF = mybir.ActivationFunctionType
AX = mybir.AxisListType

D = 2048
DIN = 15184
DFF = 5632
NCORES = 4
C_GQ, C_GK, C_GV, C_GR, C_GA = 0, 1024, 2048, 3072, 4096
C_NQ, C_NKV, C_NG = 4112, 5136, 5904
C_SZ, C_SX, C_SDT, C_MG = 5952, 6976, 9024, 9040
EPS = 1e-6


class Buf:
    __slots__ = ("w", "r")

    def __init__(self):
        self.w = None
        self.r = {}


class Sched:
    ENG = ("pe", "act", "dve", "pool", "sp")

    def __init__(self, nc, es, ndma=14):
        self.nc = nc
        self.q = {e: [] for e in self.ENG}
        self.cnt = {e: 0 for e in self.ENG[:4]}
        names = list(self.ENG[:4]) + ["d%d" % i for i in range(ndma)]
        self.sem = {n: es.enter_context(nc.semaphore("s_" + n)) for n in names}
        self.semval = {n: 0 for n in names}
        self.waited = {e: {} for e in self.ENG}
        self.ndma = ndma
        self.dma_i = 0
        self.n_ops = 0

    def _deps(self, eng, reads, writes):
        deps = {}
        for b in reads:
            if b.w is not None:
                s, v = b.w
                if deps.get(s, 0) < v:
                    deps[s] = v
        for b in writes:
            if b.w is not None:
                s, v = b.w
                if deps.get(s, 0) < v:
                    deps[s] = v
            for s, v in b.r.items():
                if deps.get(s, 0) < v:
                    deps[s] = v
        wd = self.waited[eng]
        q = self.q[eng]
        for s, v in deps.items():
            if eng == "pe" and s == "pe":
                continue
            if wd.get(s, 0) < v:
                wd[s] = v
                q.append((0, s, v))

    def _mark(self, tok, reads, writes):
        s, v = tok
        for b in reads:
            if b.r.get(s, 0) < v:
                b.r[s] = v
        for b in writes:
            b.w = tok
            b.r = {}

    def op(self, eng, fn, reads=(), writes=()):
        self._deps(eng, reads, writes)
        self.cnt[eng] += 1
        c = self.cnt[eng]
        self.semval[eng] = c
        self.q[eng].append((1, fn, eng))
        self._mark((eng, c), reads, writes)
        self.n_ops += 1

    def pe(self, fn, r=(), w=()):
        self.op("pe", fn, r, w)

    def act(self, fn, r=(), w=()):
        self.op("act", fn, r, w)

    def dve(self, fn, r=(), w=()):
        self.op("dve", fn, r, w)

    def pool(self, fn, r=(), w=()):
        self.op("pool", fn, r, w)

    def dma(self, out, in_, r=(), w=(), q="sp", slow=False):
        k = self.dma_i % self.ndma
        n = self.dma_i // self.ndma
        self.dma_i += 1
        s = "d%d" % k
        if n > 0:
            wd = self.waited[q]
            if wd.get(s, 0) < 16 * n:
                wd[s] = 16 * n
                self.q[q].append((0, s, 16 * n))
        self._deps(q, r, w)
        v = 16 * (n + 1)
        self.semval[s] = v
        self.q[q].append((2, out, in_, s, slow))
        self._mark((s, v), r, w)
        self.n_ops += 1

    def barrier(self):
        for e in self.ENG:
            wd = self.waited[e]
            q = self.q[e]
            for s, v in self.semval.items():
                if v > 0 and wd.get(s, 0) < v and not (e == "pe" and s == "pe"):
                    wd[s] = v
                    q.append((0, s, v))

    def flush(self):
        with self.nc.Block() as block:
            self.emit(block)
        for e in self.ENG:
            self.q[e] = []

    def emit(self, block):
        names = {"pe": "tensor", "act": "scalar", "dve": "vector", "pool": "gpsimd", "sp": "sync"}
        sem = self.sem
        for e, attr in names.items():
            items = self.q[e]

            def body(eng, items=items):
                for it in items:
                    if it[0] == 0:
                        eng.wait_ge(sem[it[1]], it[2])
                    elif it[0] == 1:
                        it[1](eng).then_inc(sem[it[2]], 1)
                    else:
                        if it[4]:
                            eng.dma_start(out=it[1], in_=it[2], allow_slow_non_contiguous=True).then_inc(sem[it[3]], 16)
                        else:
                            eng.dma_start(out=it[1], in_=it[2]).then_inc(sem[it[3]], 16)

            getattr(block, attr)(body)


class Ring:
    def __init__(self, tiles):
        self.tiles = tiles
        self.bufs = [Buf() for _ in tiles]
        self.i = 0

    def next(self):
        k = self.i % len(self.tiles)
        self.i += 1
        return self.tiles[k], self.bufs[k]


def _bf(a):
    return np.asarray(a, np.float32).astype(ml_dtypes.bfloat16)


def make_consts(T):
    c = {}
    i = np.arange(128)
    same = (i[:, None] // 64) == (i[None, :] // 64)
    up = i[:, None] <= i[None, :]
    c["c_identb"] = _bf(np.eye(128))
    c["c_identf"] = np.eye(128, dtype=np.float32)
    c["c_tris"] = np.where(same & up, -1.0 / 16.0, 0.0).astype(np.float32)
    c["c_tri1"] = np.where(same & up, 1.0, 0.0).astype(np.float32)
    c["c_blk"] = np.where(same, 1.0, 0.0).astype(np.float32)
    ch = np.zeros((128, 256), np.float32)
    ch[:64, :128] = 1.0
    ch[64:, 128:] = 1.0
    c["c_chsel"] = ch
    c["c_amask"] = _bf(np.where(same & up, 1.0, 0.0))
    mneg = np.where(same & up, 0.0, -1e9).astype(np.float32)
    c["c_mneg4"] = np.tile(mneg, (1, 4))
    c["c_onesf"] = np.ones((128, 128), np.float32)
    slopes = (2.0 ** (-8.0 * np.arange(1, 17) / 16.0)).astype(np.float64)
    irel = np.arange(512, dtype=np.float64)
    qa = np.zeros((16, 3, 512), np.float32)
    for h in range(16):
        v = (-slopes[h] * irel).astype(np.float32)
        hi = v.astype(ml_dtypes.bfloat16).astype(np.float32)
        r1 = v - hi
        mid = r1.astype(ml_dtypes.bfloat16).astype(np.float32)
        lo = (r1 - mid).astype(ml_dtypes.bfloat16).astype(np.float32)
        qa[h, 0], qa[h, 1], qa[h, 2] = hi, mid, lo
    c["c_qa"] = _bf(np.tile(qa, (1, 1, T // 512)))
    j = np.arange(128, dtype=np.float64)
    dv = np.arange(-64, 4, dtype=np.float64)
    kb = slopes[None, :, None] * (128.0 * dv[None, None, :] + j[:, None, None])
    c["c_kbsw"] = kb.astype(np.float32).reshape(128, 16 * 68)
    qg = np.arange(16, dtype=np.float64)
    nt = np.arange(4, dtype=np.float64)
    kc = slopes[None, :, None, None] * (16.0 * (128.0 * nt[None, None, None, :] + j[:, None, None, None]) + 31.0
                                        - 512.0 * qg[None, None, :, None])
    c["c_kbc"] = kc.astype(np.float32).reshape(128, 16 * 16 * 4)
    ii = np.arange(512)
    wm = np.zeros((8, 128, 512), np.float32)
    for m in range(8):
        u = ii[None, :] - i[:, None] + 512 - 128 * m
        wm[m] = ((u >= 0) & (u < 512)).astype(np.float32)
    c["c_wm"] = _bf(wm.transpose(1, 0, 2).reshape(128, 8 * 512))
    cm = np.zeros((5, 128, 512), np.float32)
    for k, cc in enumerate([31, -481, -993, -1505, -2017]):
        cm[k] = ((ii[None, :] - 16 * i[:, None]) >= cc).astype(np.float32)
    c["c_cm"] = _bf(cm.transpose(1, 0, 2).reshape(128, 5 * 512))
    n_cmp = (8192 - 32) // 16 + 1
    n_sel = 128
    ov = np.zeros((512, 129), np.float32)
    tok = (np.arange(n_cmp) * 16)[:, None] + np.arange(32)[None, :]
    np.add.at(ov, (np.repeat(np.arange(n_cmp), 32), (tok // 64).ravel()), 1.0 / 32)
    ov[:n_cmp, 128] = 1.0
    c["c_ov"] = _bf(ov.reshape(4, 128, 129).transpose(1, 0, 2).reshape(128, 4 * 129))
    e = np.zeros((128, T), np.float32)
    e[np.arange(T) // 64, np.arange(T)] = 1.0
    c["c_e"] = _bf(e)
    wd = np.zeros((128, 254), np.float32)
    dd = np.arange(254) - 126
    cur = (i >= 64).astype(np.int64)
    for q in range(128):
        row = np.zeros(254, np.float32)
        fut = dd > cur[q]
        row[fut] = -1e30 - 1e28 * dd[fut]
        row[dd == cur[q]] = 2e30
        row[dd == cur[q] - 1] = 3e30
        wd[q] = row
    c["c_wd"] = wd
    c["c_ones_bf"] = _bf(np.ones((128, 512)))
    return c


W_SPECS = [
    ("w_in", (D, DIN)), ("gla_a2", (16, 1024)), ("gla_a_bias", (1024,)), ("gla_norm", (256,)),
    ("nsa_cmp_pos", (2, 32, 64)), ("nsa_cmp_w1", (2, 2048, 64)), ("nsa_cmp_w2", (2, 64, 64)),
    ("ssm_conv_w", (4, 2048)), ("ssm_conv_b", (2048,)), ("ssm_dt_bias", (16,)), ("ssm_a_log", (16,)),
    ("ssm_d", (16,)), ("ssm_norm", (1024,)), ("w_branch", (3, 1024, D)), ("w_out", (D, D)),
    ("norm_pre_mix", (D,)), ("norm_post_mix", (D,)), ("norm_pre_ffn", (D,)), ("norm_post_ffn", (D,)),
    ("w_ffn_gate", (D, DFF)), ("w_ffn_up", (D, DFF)), ("w_ffn_down", (DFF, D)),
]


def build(T, NL, consts, dbg=()):
    nc = bass.Bass("TRN2", target_bir_lowering=False)
    es0 = ExitStack()
    S = Sched(nc, es0)
    NT = T // 128

    def din(name, shape, dt=F32):
        return nc.dram_tensor(name, list(shape), dt, kind="ExternalInput").ap()

    def dscr(name, shape, dt):
        if name in dbg:
            return nc.dram_tensor(name, list(shape), dt, kind="ExternalOutput").ap()
        return nc.dram_tensor(name, list(shape), dt).ap()

    x_in = din("x", (T, D))
    Wd = {n: din(n, (NL,) + s) for n, s in W_SPECS}
    Cd = {n: din(n, a.shape, BF16 if a.dtype != np.float32 else F32) for n, a in consts.items()}
    out = nc.dram_tensor("out", [T, D], F32, kind="ExternalOutput").ap()

    wb_in = dscr("wb_in", (NL, D, DIN), BF16)
    wb_br = dscr("wb_br", (NL, 3072, D), BF16)
    wb_out = dscr("wb_out", (NL, D, D), BF16)
    wb_g = dscr("wb_g", (NL, D, DFF), BF16)
    wb_u = dscr("wb_u", (NL, D, DFF), BF16)
    wb_d = dscr("wb_d", (NL, DFF, D), BF16)
    s_qT = dscr("s_qT", (1024, T), F32)
    s_kT = dscr("s_kT", (1024, T), F32)
    s_gv = dscr("s_gv", (T, 1024), BF16)
    s_gr = dscr("s_gr", (T, 1024), F32)
    s_aT = dscr("s_aT", (16, T), F32)
    s_nqT = dscr("s_nqT", (1024, T), BF16)
    s_nkvT = dscr("s_nkvT", (768, T), BF16)
    s_nkv = dscr("s_nkv", (T, 768), BF16)
    s_ngT = dscr("s_ngT", (48, T), F32)
    s_sz = dscr("s_sz", (T, 1024), F32)
    s_sxT = dscr("s_sxT", (2048, T), F32)
    s_sdt = dscr("s_sdt", (T, 16), F32)
    s_mgT = dscr("s_mgT", (6144, T), BF16)
    s_yT = [dscr("s_yT%d" % i, (1024, T), BF16) for i in range(3)]
    s_selT = dscr("s_selT", (2, 128, T), BF16)

    PS = [es0.enter_context(nc.psum_tensor("ps%d" % i, [128, 512], F32)) for i in range(8)]
    PSB = [Buf() for _ in range(8)]
    ps_i = [0]

    def psum(pool=None):
        if pool is not None:
            k = pool[ps_i[0] % len(pool)]
        else:
            k = ps_i[0] % 8
        ps_i[0] += 1
        return PS[k], PSB[k]

    sb_n = [0]

    def sb(es, name, shape, dt):
        sb_n[0] += 1
        return es.enter_context(nc.sbuf_tensor("%s_%d" % (name, sb_n[0]), list(shape), dt))

    def ACT(out_, in_, func, r, w, **kw):
        S.act(lambda e: e.activation(out=out_, in_=in_, func=func, **kw), r, w)

    def TSC(eng, out_, in0, s1, s2, op0, op1, r, w):
        if op1 is None:
            S.op(eng, lambda e: e.tensor_scalar(out=out_, in0=in0, scalar1=s1, scalar2=None, op0=op0), r, w)
        else:
            S.op(eng, lambda e: e.tensor_scalar(out=out_, in0=in0, scalar1=s1, scalar2=s2, op0=op0, op1=op1), r, w)

    def STT(eng, out_, in0, sc, in1, op0, op1, r, w):
        S.op(eng, lambda e: e.scalar_tensor_tensor(out=out_, in0=in0, scalar=sc, in1=in1, op0=op0, op1=op1), r, w)

    def TT(eng, out_, in0, in1, op, r, w):
        S.op(eng, lambda e: e.tensor_tensor(out=out_, in0=in0, in1=in1, op=op), r, w)

    def CP(eng, out_, in_, r, w):
        if eng == "act":
            S.act(lambda e: e.activation(out=out_, in_=in_, func=AF.Copy), r, w)
        else:
            S.op(eng, lambda e: e.tensor_copy(out=out_, in_=in_), r, w)

    def MM(out_, lhsT, rhs, start, stop, r, w):
        S.pe(lambda e: e.matmul(out_, lhsT, rhs, start=start, stop=stop, skip_group_check=True), r, w)

    def TR(out_, in_, ident, r, w):
        S.pe(lambda e: e.transpose(out_, in_, ident), r, w)

    def RSUM(eng, out_, in_, r, w):
        S.op(eng, lambda e: e.reduce_sum(out=out_, in_=in_, axis=AX.X), r, w)

    def RECIP(out_, in_, r, w):
        S.dve(lambda e: e.reciprocal(out=out_, in_=in_), r, w)

    def MSET(eng, ap, val, w):
        S.op(eng, lambda e: e.memset(ap, val), (), w)

    cst = {}
    cstb = Buf()
    for n in ("c_identb", "c_identf", "c_tris", "c_tri1", "c_blk", "c_chsel", "c_amask", "c_mneg4", "c_onesf"):
        a = consts[n]
        t = sb(es0, "k" + n, a.shape, BF16 if a.dtype != np.float32 else F32)
        S.dma(t[:], Cd[n][:, :], w=[cstb])
        cst[n] = t
    identb, identf = cst["c_identb"], cst["c_identf"]

    def bview(dram2d):
        return dram2d.rearrange("(kc p) n -> p kc n", p=128)

    def phase_cast():
        with ExitStack() as es:
            ring = Ring([sb(es, "cst%d" % i, [128, 8192], BF16) for i in range(3)])
            jobs = []
            for l in range(NL):
                jobs += [(Wd["w_in"][l], wb_in[l], D, DIN),
                         (Wd["w_branch"][l].rearrange("b k n -> (b k) n"), wb_br[l], 3072, D),
                         (Wd["w_out"][l], wb_out[l], D, D),
                         (Wd["w_ffn_gate"][l], wb_g[l], D, DFF), (Wd["w_ffn_up"][l], wb_u[l], D, DFF),
                         (Wd["w_ffn_down"][l], wb_d[l], DFF, D)]
            for src, dst, K, N in jobs:
                for r0 in range(0, K, 128):
                    for c0 in range(0, N, 8192):
                        w_ = min(8192, N - c0)
                        t, b = ring.next()
                        S.dma(t[:, :w_], src[r0:r0 + 128, c0:c0 + w_], w=[b], q="pool")
                        S.dma(dst[r0:r0 + 128, c0:c0 + w_], t[:, :w_], r=[b], q="sp")
            S.barrier()
            S.flush()

    def rms_to_bf16(xt, xb, gain, gainb, hb, hbb, junk, junkb, sm, smb):
        ACT(junk[:], xt, AF.Square, [xb], [junkb])
        RSUM("dve", sm[:, 0:1], junk[:], [junkb], [smb])
        ACT(sm[:, 1:2], sm[:, 0:1], AF.Sqrt, [smb], [smb], scale=1.0 / D, bias=EPS)
        RECIP(sm[:, 2:3], sm[:, 1:2], [smb], [smb])
        STT("dve", hb, xt, sm[:, 2:3], gain[:], ALU.mult, ALU.mult, [xb, smb, gainb], [hbb])

    def transpose_rows(hb, hbb, dstT, dstb, col0, nk=16, evac="act", pool=None):
        for half in range(0, nk, 8):
            n = min(8, nk - half)
            ps, pb = psum(pool)
            psv = ps[:].bitcast(BF16)
            for k in range(n):
                TR(psv[:, k * 128:(k + 1) * 128], hb[:, (half + k) * 128:(half + k + 1) * 128], identb[:], [hbb, cstb], [pb])
            CP(evac, dstT[:, half:half + n, col0:col0 + 128],
               psv[:, :n * 128].rearrange("p (k t) -> p k t", t=128), [pb], [dstb])

    TS1 = min(1024, T)
    P1_JOBS = [
        (C_GQ, 1024, "F", s_qT, None), (C_GK, 1024, "F", s_kT, None),
        (C_GV, 1024, "T", s_gv, None), (C_GR, 1024, "T", s_gr, AF.Silu),
        (C_GA, 16, "F", s_aT, None),
        (C_NQ, 1024, "F", s_nqT, "scale8"),
        (C_NKV, 768, "F", s_nkvT, None), (C_NKV, 768, "T", s_nkv, None),
        (C_NG, 48, "F", s_ngT, AF.Sigmoid),
        (C_SZ, 1024, "T", s_sz, AF.Silu), (C_SX, 2048, "F", s_sxT, None), (C_SDT, 16, "T", s_sdt, None),
        (C_MG, 6144, "F", s_mgT, AF.Sigmoid),
    ]

    def phase_p1(l):
        xsrc = x_in if l == 0 else out
        with ExitStack() as es:
            gain = sb(es, "p1gain", [128, D], F32)
            gainb = Buf()
            S.dma(gain[:], Wd["norm_pre_mix"][l:l + 1, :].partition_broadcast(128), w=[gainb])
            xt_r = Ring([sb(es, "p1xt%d" % i, [128, D], F32) for i in range(2)])
            hb_r = Ring([sb(es, "p1hb%d" % i, [128, D], BF16) for i in range(2)])
            junk = sb(es, "p1junk", [128, D], F32)
            junkb = Buf()
            sm_r = Ring([sb(es, "p1sm%d" % i, [128, 4], F32) for i in range(4)])
            hT = sb(es, "p1hT", [128, 16, TS1], BF16)
            hTb = Buf()
            w_r = Ring([sb(es, "p1w%d" % i, [128, 16, 512], BF16) for i in range(3)])
            stf_r = Ring([sb(es, "p1sf%d" % i, [128, 512], F32) for i in range(3)])
            stb_r = Ring([sb(es, "p1sb%d" % i, [128, 512], BF16) for i in range(3)])
            wv = bview(wb_in[l])
            for ts in range(T // TS1):
                tb = ts * TS1
                for tt in range(TS1 // 128):
                    t0 = tb + tt * 128
                    xt, xb = xt_r.next()
                    S.dma(xt[:], xsrc[t0:t0 + 128, :], w=[xb])
                    hb, hbb = hb_r.next()
                    sm, smb = sm_r.next()
                    rms_to_bf16(xt[:], xb, gain, gainb, hb[:], hbb, junk, junkb, sm, smb)
                    transpose_rows(hb, hbb, hT, hTb, tt * 128)
                for (c0, ncols, mode, dest, fn) in P1_JOBS:
                    isbf = dest.dtype == BF16
                    for cb in range(0, ncols, 512):
                        nb = min(512, ncols - cb)
                        wt, wtb = w_r.next()
                        S.dma(wt[:, :, :nb], wv[:, :, c0 + cb:c0 + cb + nb], w=[wtb])
                        if mode == "F":
                            for fs in range(0, nb, 128):
                                nf = min(128, nb - fs)
                                for tg in range(0, TS1, 512):
                                    ps, pb = psum()
                                    for kc in range(16):
                                        MM(ps[:nf, :], wt[:, kc, fs:fs + nf], hT[:, kc, tg:tg + 512], kc == 0, kc == 15,
                                           [wtb, hTb], [pb])
                                    st, stb = (stb_r if isbf else stf_r).next()
                                    if fn == "scale8":
                                        ACT(st[:nf, :], ps[:nf, :], AF.Copy, [pb], [stb], scale=0.125)
                                    elif fn is None:
                                        CP("dve", st[:nf, :], ps[:nf, :], [pb], [stb])
                                    else:
                                        ACT(st[:nf, :], ps[:nf, :], fn, [pb], [stb])
                                    f0 = cb + fs
                                    S.dma(dest[f0:f0 + nf, tb + tg:tb + tg + 512], st[:nf, :], r=[stb])
                        else:
                            for tt in range(TS1 // 128):
                                ps, pb = psum()
                                for kc in range(16):
                                    MM(ps[:, :nb], hT[:, kc, tt * 128:(tt + 1) * 128], wt[:, kc, :nb], kc == 0, kc == 15,
                                       [wtb, hTb], [pb])
                                st, stb = (stb_r if isbf else stf_r).next()
                                if fn is None:
                                    CP("dve", st[:, :nb], ps[:, :nb], [pb], [stb])
                                else:
                                    ACT(st[:, :nb], ps[:, :nb], fn, [pb], [stb])
                                t0 = tb + tt * 128
                                S.dma(dest[t0:t0 + 128, cb:cb + nb], st[:, :nb], r=[stb])
            S.barrier()
            S.flush()

    TS2 = 256
    NT2 = TS2 // 128

    def phase_p2(l):
        xsrc = x_in if l == 0 else out
        with ExitStack() as es:
            gA = sb(es, "p2gA", [128, D], F32)
            gB = sb(es, "p2gB", [128, D], F32)
            gAb, gBb = Buf(), Buf()
            big = sb(es, "p2big", [128, 48 * TS2], BF16)
            yT = [big[:, b * 8 * TS2:(b + 1) * 8 * TS2].rearrange("p (k t) -> p k t", t=TS2) for b in range(3)]
            mT = big[:, 32 * TS2:48 * TS2].rearrange("p (k t) -> p k t", t=TS2)
            zf = big[:, 0:32 * TS2].bitcast(F32).rearrange("p (n c) -> p n c", c=D)
            aT = big[:, 0:44 * TS2].rearrange("p (k t) -> p k t", t=TS2)
            yb = Buf()
            mb = [Buf() for _ in range(16)]
            zb = [Buf() for _ in range(NT2)]
            ab = [Buf() for _ in range(44)]
            xt = sb(es, "p2xt", [128, NT2, D], F32)
            xtb = Buf()
            h2T = sb(es, "p2h2T", [128, 16, TS2], BF16)
            h2Tb = Buf()
            w_r = Ring([sb(es, "p2w%d" % i, [128, 8192], BF16) for i in range(3)])
            g_r = Ring([sb(es, "p2g%d" % i, [128, 3, TS2], BF16) for i in range(2)])
            junk = sb(es, "p2junk", [128, D], F32)
            junkb = Buf()
            hb_r = Ring([sb(es, "p2hb%d" % i, [128, D], BF16) for i in range(2)])
            x1_r = Ring([sb(es, "p2x1%d" % i, [128, D], F32) for i in range(2)])
            sm_r = Ring([sb(es, "p2sm%d" % i, [128, 4], F32) for i in range(4)])
            tmp_r = Ring([sb(es, "p2tmp%d" % i, [128, TS2], F32) for i in range(3)])
            wbr = wb_br[l].rearrange("(b kc p) n -> p b kc n", p=128, kc=8)
            wov = bview(wb_out[l])
            wgv, wuv = bview(wb_g[l]), bview(wb_u[l])
            wdv = bview(wb_d[l])
            mgv = s_mgT.rearrange("(b f) t -> f b t", b=3)
            for ts in range(T // TS2):
                tb = ts * TS2
                S.dma(gA[:], Wd["norm_post_mix"][l:l + 1, :].partition_broadcast(128), w=[gAb])
                if ts == 0:
                    S.dma(gB[:], Wd["norm_pre_ffn"][l:l + 1, :].partition_broadcast(128), w=[gBb])
                for b in range(3):
                    S.dma(yT[b], s_yT[b].rearrange("(k p) t -> p k t", p=128)[:, :, tb:tb + TS2], w=[yb])
                for n in range(NT2):
                    S.dma(xt[:, n, :], xsrc[tb + n * 128:tb + (n + 1) * 128, :], w=[xtb])
                for cb in range(4):
                    wts = []
                    for b in range(3):
                        wt, wtb = w_r.next()
                        wtv = wt[:, :4096].rearrange("p (k n) -> p k n", n=512)
                        S.dma(wtv, wbr[:, b, :, cb * 512:(cb + 1) * 512], w=[wtb])
                        wts.append((wtv, wtb))
                    for fs in range(4):
                        fc = cb * 4 + fs
                        gt, gtb = g_r.next()
                        S.dma(gt[:], mgv[fc * 128:(fc + 1) * 128, :, tb:tb + TS2], w=[gtb])
                        tmp, tmpb = tmp_r.next()
                        for b in range(3):
                            ps, pb = psum()
                            wtv, wtb = wts[b]
                            for kc in range(8):
                                MM(ps[:, :TS2], wtv[:, kc, fs * 128:(fs + 1) * 128], yT[b][:, kc, :], kc == 0, kc == 7,
                                   [wtb, yb], [pb])
                            if b == 0:
                                TT("dve", tmp[:], ps[:, :TS2], gt[:, 0, :], ALU.mult, [pb, gtb], [tmpb])
                            elif b == 1:
                                tmp2, tmp2b = tmp_r.next()
                                TT("dve", tmp2[:], ps[:, :TS2], gt[:, 1, :], ALU.mult, [pb, gtb], [tmp2b])
                                TT("pool", tmp[:], tmp[:], tmp2[:], ALU.add, [tmpb, tmp2b], [tmpb])
                            else:
                                tmp2, tmp2b = tmp_r.next()
                                TT("dve", tmp2[:], ps[:, :TS2], gt[:, 2, :], ALU.mult, [pb, gtb], [tmp2b])
                                TT("pool", mT[:, fc, :], tmp[:], tmp2[:], ALU.add, [tmpb, tmp2b], [mb[fc]])
                S.barrier()
                for cb in range(4):
                    wt, wtb = w_r.next()
                    wtv = wt[:].rearrange("p (k n) -> p k n", n=512)
                    S.dma(wtv, wov[:, :, cb * 512:(cb + 1) * 512], w=[wtb])
                    for n in range(NT2):
                        ps, pb = psum()
                        for kc in range(16):
                            MM(ps[:], mT[:, kc, n * 128:(n + 1) * 128], wtv[:, kc, :], kc == 0, kc == 15, [wtb, mb[kc]], [pb])
                        CP("act", zf[:, n, cb * 512:(cb + 1) * 512], ps[:], [pb], [zb[n]])
                for n in range(NT2):
                    sm, smb = sm_r.next()
                    ACT(junk[:], zf[:, n, :], AF.Square, [zb[n]], [junkb])
                    RSUM("dve", sm[:, 0:1], junk[:], [junkb], [smb])
                    ACT(sm[:, 1:2], sm[:, 0:1], AF.Sqrt, [smb], [smb], scale=1.0 / D, bias=EPS)
                    RECIP(sm[:, 2:3], sm[:, 1:2], [smb], [smb])
                    STT("dve", junk[:], zf[:, n, :], sm[:, 2:3], gA[:], ALU.mult, ALU.mult, [zb[n], smb, gAb], [junkb])
                    TT("pool", xt[:, n, :], xt[:, n, :], junk[:], ALU.add, [xtb, junkb], [xtb])
                    S.dma(out[tb + n * 128:tb + (n + 1) * 128, :], xt[:, n, :], r=[xtb])
                    hb, hbb = hb_r.next()
                    sm, smb = sm_r.next()
                    rms_to_bf16(xt[:, n, :], xtb, gB, gBb, hb[:], hbb, junk, junkb, sm, smb)
                    transpose_rows(hb, hbb, h2T, h2Tb, n * 128)
                S.barrier()
                S.dma(gA[:], Wd["norm_post_ffn"][l:l + 1, :].partition_broadcast(128), w=[gAb])
                for fb in range(11):
                    wg, wgb = w_r.next()
                    wgv_ = wg[:].rearrange("p (k n) -> p k n", n=512)
                    S.dma(wgv_, wgv[:, :, fb * 512:(fb + 1) * 512], w=[wgb])
                    wu, wub = w_r.next()
                    wuv_ = wu[:].rearrange("p (k n) -> p k n", n=512)
                    S.dma(wuv_, wuv[:, :, fb * 512:(fb + 1) * 512], w=[wub])
                    for fs in range(4):
                        psg, pgb = psum()
                        for kc in range(16):
                            MM(psg[:, :TS2], wgv_[:, kc, fs * 128:(fs + 1) * 128], h2T[:, kc, :], kc == 0, kc == 15, [wgb, h2Tb], [pgb])
                        psu, pub = psum()
                        for kc in range(16):
                            MM(psu[:, :TS2], wuv_[:, kc, fs * 128:(fs + 1) * 128], h2T[:, kc, :], kc == 0, kc == 15, [wub, h2Tb], [pub])
                        tmp, tmpb = tmp_r.next()
                        ACT(tmp[:], psg[:, :TS2], AF.Silu, [pgb], [tmpb])
                        TT("dve", aT[:, fb * 4 + fs, :], tmp[:], psu[:, :TS2], ALU.mult, [tmpb, pub], [ab[fb * 4 + fs]])
                for cb in range(4):
                    pss = [psum() for _ in range(NT2)]
                    for kq in range(4):
                        wt, wtb = w_r.next()
                        wtv = wt[:, :11 * 512].rearrange("p (k n) -> p k n", n=512)
                        S.dma(wtv, wdv[:, kq * 11:(kq + 1) * 11, cb * 512:(cb + 1) * 512], w=[wtb])
                        for n in range(NT2):
                            ps, pb = pss[n]
                            for k in range(11):
                                kc = kq * 11 + k
                                MM(ps[:], aT[:, kc, n * 128:(n + 1) * 128], wtv[:, k, :], kc == 0, kc == 43, [wtb, ab[kc]], [pb])
                    for n in range(NT2):
                        ps, pb = pss[n]
                        CP("act", xt[:, n, cb * 512:(cb + 1) * 512], ps[:], [pb], [xtb])
                for n in range(NT2):
                    x1, x1b = x1_r.next()
                    S.dma(x1[:], out[tb + n * 128:tb + (n + 1) * 128, :], w=[x1b])
                    sm, smb = sm_r.next()
                    ACT(junk[:], xt[:, n, :], AF.Square, [xtb], [junkb])
                    RSUM("dve", sm[:, 0:1], junk[:], [junkb], [smb])
                    ACT(sm[:, 1:2], sm[:, 0:1], AF.Sqrt, [smb], [smb], scale=1.0 / D, bias=EPS)
                    RECIP(sm[:, 2:3], sm[:, 1:2], [smb], [smb])
                    STT("dve", junk[:], xt[:, n, :], sm[:, 2:3], gA[:], ALU.mult, ALU.mult, [xtb, smb, gAb], [junkb])
                    TT("pool", x1[:], x1[:], junk[:], ALU.add, [x1b, junkb], [x1b])
                    S.dma(out[tb + n * 128:tb + (n + 1) * 128, :], x1[:], r=[x1b])
                S.barrier()
                S.flush()

    def phase_gla(l):
        TG = 512
        with ExitStack() as es:
            wa2 = sb(es, "g_wa2", [17, 1024], F32)
            wa2b = Buf()
            S.dma(wa2[0:16, :], Wd["gla_a2"][l], w=[wa2b])
            S.dma(wa2[16:17, :], Wd["gla_a_bias"][l:l + 1, :], w=[wa2b])
            gng = sb(es, "g_gng", [128, 256], F32)
            gngb = Buf()
            S.dma(gng[:], Wd["gla_norm"][l:l + 1, :].partition_broadcast(128), w=[gngb])
            am4 = sb(es, "g_am4", [128, 512], BF16)
            am4b = Buf()
            for h in range(4):
                S.dma(am4[:, h * 128:(h + 1) * 128], Cd["c_amask"][:, :], w=[am4b])
            aTa = sb(es, "g_aTa", [17, TG], F32)
            aTab = Buf()
            MSET("dve", aTa[:], 1.0, [aTab])
            lt = sb(es, "g_lt", [128, 4, 1024], F32)
            ltb = [Buf() for _ in range(4)]
            Eq = sb(es, "g_Eq", [128, 8, TG], F32)
            Eqb = [Buf() for _ in range(8)]
            Ei_r = Ring([sb(es, "g_Ei%d" % i, [128, TG], F32) for i in range(2)])
            q_r = Ring([sb(es, "g_q%d" % i, [128, TG], F32) for i in range(2)])
            k_r = Ring([sb(es, "g_k%d" % i, [128, TG], F32) for i in range(2)])
            e_r = Ring([sb(es, "g_e%d" % i, [128, 512], F32) for i in range(2)])
            qdT = sb(es, "g_qdT", [128, 8, TG], BF16)
            kiT = sb(es, "g_kiT", [128, 8, TG], BF16)
            keT = sb(es, "g_keT", [128, 8, TG], BF16)
            qdb = [Buf() for _ in range(8)]
            kib = [Buf() for _ in range(8)]
            keb = [Buf() for _ in range(8)]
            ketok = sb(es, "g_ketok", [128, 4, 1024], BF16)
            ketokb = [Buf() for _ in range(4)]
            gv = sb(es, "g_gv", [128, 4, 1024], BF16)
            gvb = Buf()
            gr = sb(es, "g_gr", [128, 4, 1024], F32)
            grb = Buf()
            Sf = sb(es, "g_Sf", [128, 2, 4, 256], F32)
            Sb_ = sb(es, "g_Sb", [128, 2, 4, 256], BF16)
            Sfb, Sbb = Buf(), Buf()
            MSET("dve", Sf[:], 0.0, [Sfb])
            MSET("pool", Sb_[:], 0.0, [Sbb])
            att_r = Ring([sb(es, "g_att%d" % i, [128, 512], BF16) for i in range(2)])
            junk = sb(es, "g_junk", [128, 1024], F32)
            junkb = Buf()
            ytmp = sb(es, "g_ytmp", [128, 1024], F32)
            ytmpb = Buf()
            ybf_r = Ring([sb(es, "g_ybf%d" % i, [128, 1024], BF16) for i in range(2)])
            yTg = sb(es, "g_yTg", [128, 8, TG], BF16)
            yTgb = Buf()
            sm_r = Ring([sb(es, "g_sm%d" % i, [128, 12], F32) for i in range(4)])
            tris = cst["c_tris"]
            for g in range(T // TG):
                t0 = g * TG
                S.dma(aTa[0:16, :], s_aT[:, t0:t0 + TG], w=[aTab])
                S.dma(gv[:], s_gv[t0:t0 + TG, :].rearrange("(n p) c -> p n c", p=128), w=[gvb])
                S.dma(gr[:], s_gr[t0:t0 + TG, :].rearrange("(n p) c -> p n c", p=128), w=[grb])
                for tt in range(4):
                    for hf in range(2):
                        ps, pb = psum((2, 3))
                        MM(ps[:], aTa[:, tt * 128:(tt + 1) * 128], wa2[:, hf * 512:(hf + 1) * 512], True, True, [aTab, wa2b], [pb])
                        e_, eb = e_r.next()
                        ACT(e_[:], ps[:], AF.Exp, [pb], [eb], scale=-1.0)
                        ACT(lt[:, tt, hf * 512:(hf + 1) * 512], e_[:], AF.Ln, [eb], [ltb[tt]], bias=1.0)
                for fc in range(8):
                    ps, pb = psum((2, 3))
                    for tt in range(4):
                        MM(ps[:, tt * 128:(tt + 1) * 128], lt[:, tt, fc * 128:(fc + 1) * 128], tris[:], True, True, [ltb[tt], cstb], [pb])
                    ACT(Eq[:, fc, :], ps[:], AF.Exp, [pb], [Eqb[fc]])
                    Ei, Eib = Ei_r.next()
                    ACT(Ei[:], ps[:], AF.Exp, [pb], [Eib], scale=-1.0)
                    qt, qtb = q_r.next()
                    kt, ktb = k_r.next()
                    S.dma(qt[:], s_qT[fc * 128:(fc + 1) * 128, t0:t0 + TG], w=[qtb])
                    S.dma(kt[:], s_kT[fc * 128:(fc + 1) * 128, t0:t0 + TG], w=[ktb])
                    STT("dve", qdT[:, fc, :], qt[:], 0.0625, Eq[:, fc, :], ALU.mult, ALU.mult, [qtb, Eqb[fc]], [qdb[fc]])
                    TT("pool", kiT[:, fc, :], kt[:], Ei[:], ALU.mult, [ktb, Eib], [kib[fc]])
                    for c8 in range(8):
                        cs = slice(c8 * 64, (c8 + 1) * 64)
                        STT("dve", keT[:, fc, cs], kt[:, cs], Eq[:, fc, c8 * 64 + 63:c8 * 64 + 64], Ei[:, cs],
                            ALU.mult, ALU.mult, [ktb, Eqb[fc], Eib], [keb[fc]])
                for tt in range(4):
                    ps, pb = psum((2, 3))
                    psv = ps[:].bitcast(BF16)
                    for fc in range(8):
                        TR(psv[:, fc * 128:(fc + 1) * 128], keT[:, fc, tt * 128:(tt + 1) * 128], identb[:], [keb[fc], cstb], [pb])
                    CP("act", ketok[:, tt, :], psv[:, :1024], [pb], [ketokb[tt]])
                for tt in range(4):
                    ts_ = slice(tt * 128, (tt + 1) * 128)
                    ps, pb = psum((2, 3))
                    for h in range(4):
                        for c in range(2):
                            MM(ps[:, h * 128:(h + 1) * 128], kiT[:, 2 * h + c, ts_], qdT[:, 2 * h + c, ts_], c == 0, c == 1,
                               [kib[2 * h + c], qdb[2 * h + c]], [pb])
                    att, attb = att_r.next()
                    TT("dve", att[:], ps[:], am4[:], ALU.mult, [pb, am4b], [attb])
                    po = [(PS[0], PSB[0]), (PS[1], PSB[1])]
                    for ch in range(2):
                        c8 = tt * 2 + ch
                        r0 = ch * 64
                        rs = slice(r0, r0 + 64)
                        cs = slice(tt * 128 + r0, tt * 128 + r0 + 64)
                        for h in range(4):
                            pso, pob = po[h // 2]
                            oc = slice((h % 2) * 256, (h % 2) * 256 + 256)
                            MM(pso[rs, oc], qdT[:, 2 * h, cs], Sb_[:, 0, h, :], True, False, [qdb[2 * h], Sbb], [pob])
                            MM(pso[rs, oc], qdT[:, 2 * h + 1, cs], Sb_[:, 1, h, :], False, False, [qdb[2 * h + 1], Sbb], [pob])
                            MM(pso[rs, oc], att[rs, h * 128 + r0:h * 128 + r0 + 64], gv[rs, tt, h * 256:(h + 1) * 256], False, True,
                               [attb, gvb], [pob])
                        pu = [(PS[4 + h_], PSB[4 + h_]) for h_ in range(4)]
                        for h in range(4):
                            for c in range(2):
                                psu, pub = pu[h]
                                MM(psu[:, c * 256:(c + 1) * 256], ketok[rs, tt, (2 * h + c) * 128:(2 * h + c + 1) * 128],
                                   gv[rs, tt, h * 256:(h + 1) * 256], True, True, [ketokb[tt], gvb], [pub])
                        for h in range(4):
                            for c in range(2):
                                psu, pub = pu[h]
                                STT("dve", Sf[:, c, h, :], Sf[:, c, h, :], Eq[:, 2 * h + c, c8 * 64 + 63:c8 * 64 + 64],
                                    psu[:, c * 256:(c + 1) * 256], ALU.mult, ALU.add, [Sfb, Eqb[2 * h + c], pub], [Sfb])
                        CP("act", Sb_[:], Sf[:], [Sfb], [Sbb])
                    sm, smb = sm_r.next()
                    for hh in range(2):
                        pso, pob = po[hh]
                        ACT(junk[:, hh * 512:(hh + 1) * 512], pso[:], AF.Square, [pob], [junkb])
                    RSUM("dve", sm[:, 0:4], junk[:].rearrange("p (h d) -> p h d", d=256), [junkb], [smb])
                    ACT(sm[:, 4:8], sm[:, 0:4], AF.Sqrt, [smb], [smb], scale=1.0 / 256, bias=EPS)
                    RECIP(sm[:, 8:12], sm[:, 4:8], [smb], [smb])
                    for h in range(4):
                        pso, pob = po[h // 2]
                        oc = slice((h % 2) * 256, (h % 2) * 256 + 256)
                        STT("dve", ytmp[:, h * 256:(h + 1) * 256], pso[:, oc], sm[:, 8 + h:9 + h], gng[:], ALU.mult, ALU.mult,
                            [pob, smb, gngb], [ytmpb])
                    yb_, ybb = ybf_r.next()
                    TT("pool", yb_[:], ytmp[:], gr[:, tt, :], ALU.mult, [ytmpb, grb], [ybb])
                    transpose_rows(yb_, ybb, yTg, yTgb, tt * 128, nk=8, pool=(2, 3))
                S.dma(s_yT[0].rearrange("(k p) t -> p k t", p=128)[:, :, t0:t0 + TG], yTg[:], r=[yTgb])
            S.barrier()
            S.flush()

    def phase_ssd(l):
        TG = 512
        with ExitStack() as es:
            cw = sb(es, "s_cw", [128, 16, 4], F32)
            cbias = sb(es, "s_cb", [128, 16], F32)
            prm = sb(es, "s_prm", [128, 4, 16], F32)
            gn = sb(es, "s_gn", [128, 1024], F32)
            pb_ = Buf()
            for k in range(4):
                S.dma(cw[:, :, k], Wd["ssm_conv_w"][l, k].rearrange("(c p) -> p c", p=128), w=[pb_], slow=True)
            S.dma(cbias[:], Wd["ssm_conv_b"][l].rearrange("(c p) -> p c", p=128), w=[pb_], slow=True)
            S.dma(prm[:, 0, :], Wd["ssm_dt_bias"][l:l + 1, :].partition_broadcast(128), w=[pb_])
            S.dma(prm[:, 1, :], Wd["ssm_a_log"][l:l + 1, :].partition_broadcast(128), w=[pb_])
            S.dma(prm[:, 2, :], Wd["ssm_d"][l:l + 1, :].partition_broadcast(128), w=[pb_])
            S.dma(gn[:], Wd["ssm_norm"][l:l + 1, :].partition_broadcast(128), w=[pb_])
            ACT(prm[:, 3, :], prm[:, 1, :], AF.Exp, [pb_], [pb_])
            TSC("dve", prm[:, 1, :], prm[:, 3, :], -1.0, None, ALU.mult, None, [pb_], [pb_])
            xin_r = Ring([sb(es, "s_xin%d" % i, [128, TG + 3], F32) for i in range(2)])
            acc_r = Ring([sb(es, "s_acc%d" % i, [128, TG], F32) for i in range(2)])
            xsT = sb(es, "s_xsT", [128, 8, TG], F32)
            xsTb = [Buf() for _ in range(8)]
            BTf = sb(es, "s_BTf", [128, 4, TG], F32)
            BTfb = [Buf() for _ in range(4)]
            BT = sb(es, "s_BT", [128, 4, TG], BF16)
            CT = sb(es, "s_CT", [128, 4, TG], BF16)
            BTb = [Buf() for _ in range(4)]
            CTb = [Buf() for _ in range(4)]
            xs_r = Ring([sb(es, "s_xs%d" % i, [128, 1024], F32) for i in range(2)])
            bt_r = Ring([sb(es, "s_bt%d" % i, [128, 512], BF16) for i in range(2)])
            sz_r = Ring([sb(es, "s_sz%d" % i, [128, 1024], F32) for i in range(2)])
            dt_r = Ring([sb(es, "s_dt%d" % i, [128, 8, 16], F32) for i in range(2)])
            ecl_r = Ring([sb(es, "s_ecl%d" % i, [128, 32], F32) for i in range(2)])
            xdt_r = Ring([sb(es, "s_xdt%d" % i, [128, 1024], BF16) for i in range(2)])
            xde_r = Ring([sb(es, "s_xde%d" % i, [128, 1024], BF16) for i in range(2)])
            cbm_r = Ring([sb(es, "s_cbm%d" % i, [128, 128], F32) for i in range(2)])
            dg_r = Ring([sb(es, "s_dg%d" % i, [128, 512], F32) for i in range(2)])
            tt_r = Ring([sb(es, "s_t%d" % i, [128, 512], F32) for i in range(2)])
            w_r = Ring([sb(es, "s_w%d" % i, [128, 512], BF16) for i in range(2)])
            STf = sb(es, "s_STf", [128, 4, 256], F32)
            STb = sb(es, "s_STb", [128, 4, 256], BF16)
            STfb, STbb = Buf(), Buf()
            MSET("dve", STf[:], 0.0, [STfb])
            MSET("pool", STb[:], 0.0, [STbb])
            yi = sb(es, "s_yi", [128, 1024], F32)
            yib = Buf()
            y2 = sb(es, "s_y2", [128, 1024], F32)
            y2b = Buf()
            junk = sb(es, "s_junk", [128, 1024], F32)
            junkb = Buf()
            ybf_r = Ring([sb(es, "s_ybf%d" % i, [128, 1024], BF16) for i in range(2)])
            yTg = sb(es, "s_yTg", [128, 8, TG], BF16)
            yTgb = Buf()
            sm_r = Ring([sb(es, "s_sm%d" % i, [128, 12], F32) for i in range(4)])
            tri1, blk, chsel, mneg4, onesf = cst["c_tri1"], cst["c_blk"], cst["c_chsel"], cst["c_mneg4"], cst["c_onesf"]
            amask = cst["c_amask"]
            for g in range(T // TG):
                t0 = g * TG
                for c in range(16):
                    xin, xinb = xin_r.next()
                    if g == 0:
                        MSET("pool", xin[:, 0:3], 0.0, [xinb])
                        S.dma(xin[:, 3:], s_sxT[c * 128:(c + 1) * 128, 0:TG], w=[xinb])
                    else:
                        S.dma(xin[:], s_sxT[c * 128:(c + 1) * 128, t0 - 3:t0 + TG], w=[xinb])
                    acc, accb = acc_r.next()
                    TSC("dve", acc[:], xin[:, 3:3 + TG], cw[:, c, 3:4], None, ALU.mult, None, [xinb, pb_], [accb])
                    for k in range(3):
                        STT("dve", acc[:], xin[:, k:k + TG], cw[:, c, k:k + 1], acc[:], ALU.mult, ALU.add, [xinb, pb_, accb], [accb])
                    if c < 8:
                        ACT(xsT[:, c, :], acc[:], AF.Silu, [accb, pb_], [xsTb[c]], bias=cbias[:, c:c + 1])
                    elif c < 12:
                        ACT(BTf[:, c - 8, :], acc[:], AF.Silu, [accb, pb_], [BTfb[c - 8]], bias=cbias[:, c:c + 1])
                        CP("pool", BT[:, c - 8, :], BTf[:, c - 8, :], [BTfb[c - 8]], [BTb[c - 8]])
                    else:
                        ACT(CT[:, c - 12, :], acc[:], AF.Silu, [accb, pb_], [CTb[c - 12]], bias=cbias[:, c:c + 1])
                for tt in range(4):
                    tsl = slice(tt * 128, (tt + 1) * 128)
                    tok0 = t0 + tt * 128
                    xs, xsb = xs_r.next()
                    for half in range(2):
                        ps, pb = psum((1,))
                        for k in range(4):
                            c = half * 4 + k
                            TR(ps[:, k * 128:(k + 1) * 128], xsT[:, c, tsl], identf[:], [xsTb[c], cstb], [pb])
                        CP("act", xs[:, half * 512:(half + 1) * 512], ps[:], [pb], [xsb])
                    btok, btokb = bt_r.next()
                    ps, pb = psum((1,))
                    for k in range(4):
                        TR(ps[:, k * 128:(k + 1) * 128], BTf[:, k, tsl], identf[:], [BTfb[k], cstb], [pb])
                    CP("act", btok[:], ps[:], [pb], [btokb])
                    sz, szb = sz_r.next()
                    S.dma(sz[:], s_sz[tok0:tok0 + 128, :], w=[szb])
                    dtt, dtb = dt_r.next()
                    S.dma(dtt[:, 0, :], s_sdt[tok0:tok0 + 128, :], w=[dtb])
                    TT("dve", dtt[:, 6, :], dtt[:, 0, :], prm[:, 0, :], ALU.add, [dtb, pb_], [dtb])
                    ACT(dtt[:, 6, :], dtt[:, 6, :], AF.Exp, [dtb], [dtb])
                    ACT(dtt[:, 1, :], dtt[:, 6, :], AF.Ln, [dtb], [dtb], bias=1.0)
                    TT("dve", dtt[:, 2, :], dtt[:, 1, :], prm[:, 1, :], ALU.mult, [dtb, pb_], [dtb])
                    ps, pb = psum((0,))
                    MM(ps[:, 0:16], tri1[:], dtt[:, 2, :], True, True, [cstb, dtb], [pb])
                    MM(ps[:, 16:32], blk[:], dtt[:, 2, :], True, True, [cstb, dtb], [pb])
                    MM(ps[:, 32:48], chsel[:, 0:128], dtt[:, 2, :], True, True, [cstb, dtb], [pb])
                    MM(ps[:, 48:64], chsel[:, 128:256], dtt[:, 2, :], True, True, [cstb, dtb], [pb])
                    CP("dve", dtt[:, 3, :], ps[:, 0:16], [pb], [dtb])
                    ACT(dtt[:, 4, :], ps[:, 0:16], AF.Exp, [pb], [dtb])
                    TT("dve", dtt[:, 6, :], ps[:, 16:32], dtt[:, 3, :], ALU.subtract, [pb, dtb], [dtb])
                    ACT(dtt[:, 5, :], dtt[:, 6, :], AF.Exp, [dtb], [dtb])
                    ecl, eclb = ecl_r.next()
                    ACT(ecl[:], ps[:, 32:64], AF.Exp, [pb], [eclb])
                    xdt, xdtb = xdt_r.next()
                    xde, xdeb = xde_r.next()
                    for h in range(16):
                        hs = slice(h * 64, (h + 1) * 64)
                        TSC("dve" if h % 2 else "pool", xdt[:, hs], xs[:, hs], dtt[:, 1, h:h + 1], None, ALU.mult, None, [xsb, dtb], [xdtb])
                        TSC("pool" if h % 2 else "dve", xde[:, hs], xs[:, hs], dtt[:, 1, h:h + 1], dtt[:, 5, h:h + 1], ALU.mult, ALU.mult,
                            [xsb, dtb], [xdeb])
                    psY = [(PS[3], PSB[3]), (PS[4], PSB[4])]
                    psI = [(PS[5], PSB[5]), (PS[6], PSB[6])]
                    for gq in range(4):
                        ps, pb = psum((1,))
                        MM(ps[:, 0:128], BT[:, gq, tsl], CT[:, gq, tsl], True, True, [BTb[gq], CTb[gq]], [pb])
                        cbm, cbmb = cbm_r.next()
                        TT("dve", cbm[:], ps[:, 0:128], amask[:], ALU.mult, [pb, cstb], [cbmb])
                        dg, dgb = dg_r.next()
                        for hq in range(4):
                            TSC("pool", dg[:, hq * 128:(hq + 1) * 128], identf[:], dtt[:, 3, gq * 4 + hq:gq * 4 + hq + 1], None, ALU.mult, None,
                                [cstb, dtb], [dgb])
                        ps2, pb2 = psum((2,))
                        MM(ps2[:], onesf[:], dg[:], True, False, [cstb, dgb], [pb2])
                        MM(ps2[:], identf[:], mneg4[:], False, True, [cstb], [pb2])
                        tq, tqb = tt_r.next()
                        for hq in range(4):
                            TSC("dve", tq[:, hq * 128:(hq + 1) * 128], ps2[:, hq * 128:(hq + 1) * 128], dtt[:, 3, gq * 4 + hq:gq * 4 + hq + 1],
                                None, ALU.subtract, None, [pb2, dtb], [tqb])
                        ACT(tq[:], tq[:], AF.Exp, [tqb], [tqb])
                        wq, wqb = w_r.next()
                        for hq in range(4):
                            TT("pool", wq[:, hq * 128:(hq + 1) * 128], tq[:, hq * 128:(hq + 1) * 128], cbm[:], ALU.mult, [tqb, cbmb], [wqb])
                        py, pyb = psY[gq // 2]
                        for hq in range(4):
                            h = gq * 4 + hq
                            oc = slice((gq % 2) * 256 + hq * 64, (gq % 2) * 256 + hq * 64 + 64)
                            MM(py[:, oc], wq[:, hq * 128:(hq + 1) * 128], xdt[:, h * 64:(h + 1) * 64], True, True, [wqb, xdtb], [pyb])
                    for ch in range(2):
                        r0 = ch * 64
                        rs = slice(r0, r0 + 64)
                        cs = slice(tt * 128 + r0, tt * 128 + r0 + 64)
                        for gq in range(4):
                            pi, pib = psI[gq // 2]
                            MM(pi[rs, (gq % 2) * 256:(gq % 2) * 256 + 256], CT[:, gq, cs], STb[:, gq, :], True, True, [CTb[gq], STbb], [pib])
                        for gq in range(4):
                            pS, pSb = psum((7, 0))
                            MM(pS[:, 0:256], btok[rs, gq * 128:(gq + 1) * 128], xde[rs, gq * 256:(gq + 1) * 256], True, True, [btokb, xdeb], [pSb])
                            for hq in range(4):
                                h = gq * 4 + hq
                                STT("dve", STf[:, gq, hq * 64:(hq + 1) * 64], STf[:, gq, hq * 64:(hq + 1) * 64], ecl[:, ch * 16 + h:ch * 16 + h + 1],
                                    pS[:, hq * 64:(hq + 1) * 64], ALU.mult, ALU.add, [STfb, eclb, pSb], [STfb])
                        CP("act", STb[:], STf[:], [STfb], [STbb])
                    for hh in range(2):
                        py, pyb = psY[hh]
                        CP("act", yi[:, hh * 512:(hh + 1) * 512], py[:], [pyb], [yib])
                    for h in range(16):
                        hs = slice(h * 64, (h + 1) * 64)
                        pi, pib = psI[h // 8]
                        STT("dve", y2[:, hs], pi[:, (h % 8) * 64:(h % 8) * 64 + 64], dtt[:, 4, h:h + 1], yi[:, hs], ALU.mult, ALU.add,
                            [pib, dtb, yib], [y2b])
                    for h in range(16):
                        hs = slice(h * 64, (h + 1) * 64)
                        STT("dve", y2[:, hs], xs[:, hs], prm[:, 2, h:h + 1], y2[:, hs], ALU.mult, ALU.add, [xsb, pb_, y2b], [y2b])
                    TT("pool", y2[:], y2[:], sz[:], ALU.mult, [y2b, szb], [y2b])
                    sm, smb = sm_r.next()
                    ACT(junk[:], y2[:], AF.Square, [y2b], [junkb])
                    RSUM("dve", sm[:, 0:4], junk[:].rearrange("p (h d) -> p h d", d=256), [junkb], [smb])
                    ACT(sm[:, 4:8], sm[:, 0:4], AF.Sqrt, [smb], [smb], scale=1.0 / 256, bias=EPS)
                    RECIP(sm[:, 8:12], sm[:, 4:8], [smb], [smb])
                    yb_, ybb = ybf_r.next()
                    for gq in range(4):
                        gs = slice(gq * 256, (gq + 1) * 256)
                        STT("dve", yb_[:, gs], y2[:, gs], sm[:, 8 + gq:9 + gq], gn[:, gs], ALU.mult, ALU.mult, [y2b, smb, pb_], [ybb])
                    transpose_rows(yb_, ybb, yTg, yTgb, tt * 128, nk=8, pool=(1,))
                S.dma(s_yT[2].rearrange("(k p) t -> p k t", p=128)[:, :, t0:t0 + TG], yTg[:], r=[yTgb])
            S.barrier()
            S.flush()

    s_ocmp = dscr("s_ocmp", (1024, T), F32)
    NQG = T // 512
    NCMP = T // 16 - 1
    NTC = (NCMP + 127) // 128
    CM_C = [31, -481, -993, -1505, -2017]

    def phase_nsa(l):
        with ExitStack() as es:
            kb = Buf()
            E = sb(es, "n_E", [128, T], BF16)
            S.dma(E[:], Cd["c_e"][:, :], w=[kb])
            wm = sb(es, "n_wm", [128, 8 * 512], BF16)
            S.dma(wm[:], Cd["c_wm"][:, :], w=[kb])
            cm = sb(es, "n_cm", [128, 5 * 512], BF16)
            S.dma(cm[:], Cd["c_cm"][:, :], w=[kb])
            kbsw = sb(es, "n_kbsw", [128, 16 * 68], F32)
            S.dma(kbsw[:], Cd["c_kbsw"][:, :], w=[kb])
            kbc = sb(es, "n_kbc", [128, 1024], F32)
            S.dma(kbc[:], Cd["c_kbc"][:, :], w=[kb])
            ov = sb(es, "n_ov", [128, 4, 129], BF16)
            S.dma(ov[:], Cd["c_ov"].rearrange("p (n c) -> p n c", c=129), w=[kb])
            wdt = sb(es, "n_wd", [128, 254], F32)
            S.dma(wdt[:], Cd["c_wd"][:, :], w=[kb])
            onesf = cst["c_onesf"]
            w1s = sb(es, "n_w1", [128, 2, 16, 64], BF16)
            w2s = sb(es, "n_w2", [64, 2, 64], BF16)
            posf = sb(es, "n_posf", [128, 2, 16], F32)
            posb = sb(es, "n_posb", [128, 2, 16], BF16)
            ccon = sb(es, "n_ccon", [64, 2], F32)
            for j in range(2):
                S.dma(w1s[:, j], Wd["nsa_cmp_w1"][l, j].rearrange("(c p) o -> p c o", p=128), w=[kb], q="pool")
                S.dma(w2s[:, j, :], Wd["nsa_cmp_w2"][l, j], w=[kb], q="pool")
                S.dma(posf[:, j, :], Wd["nsa_cmp_pos"][l, j].rearrange("(c two) d -> (two d) c", two=2), w=[kb], slow=True)
            CP("dve", posb[:], posf[:], [kb], [kb])
            for j in range(2):
                ps, pb = psum((6, 7))
                for c in range(16):
                    MM(ps[:64, 0:1], w1s[:, j, c, :], posb[:, j, c:c + 1], c == 0, c == 15, [kb], [pb])
                CP("dve", ccon[:, j:j + 1], ps[:64, 0:1], [pb], [kb])
            AT = sb(es, "n_AT", [64, 512], BF16)
            ATb = Buf()
            Kc = sb(es, "n_Kc", [67, 512], BF16)
            Vc = sb(es, "n_Vc", [128, 4, 65], BF16)
            Ks = sb(es, "n_Ks", [67, T], BF16)
            Kw = sb(es, "n_Kw", [67, T], BF16)
            Vs = sb(es, "n_Vs", [128, NT, 65], BF16)
            Vw = sb(es, "n_Vw", [128, NT, 65], BF16)
            kvb = Buf()
            selT = sb(es, "n_selT", [128, T], BF16)
            selTb = Buf()
            kcs2, kcs2b = selT, selTb
            impacc = sb(es, "n_imp", [128, 4, 128], F32)
            impb = [Buf() for _ in range(4)]
            q_r = Ring([sb(es, "n_q%d" % i, [67, 512], BF16) for i in range(6)])
            p_r = Ring([sb(es, "n_p%d" % i, [128, 512], BF16) for i in range(6)])
            tf_r = Ring([sb(es, "n_tf%d" % i, [128, 512], F32) for i in range(2)])
            m_r = Ring([sb(es, "n_m%d" % i, [128, 512], BF16) for i in range(3)])
            osb_r = Ring([sb(es, "n_osb%d" % i, [65, 512], F32) for i in range(3)])
            fr_r = Ring([sb(es, "n_fr%d" % i, [65, 512], F32) for i in range(3)])
            gt_r = Ring([sb(es, "n_gt%d" % i, [65, 3, 512], F32) for i in range(5)])
            accy_r = Ring([sb(es, "n_acc%d" % i, [64, 512], F32) for i in range(5)])
            ctr_r = Ring([sb(es, "n_ctr%d" % i, [64, 512], F32) for i in range(3)])
            ybf_r = Ring([sb(es, "n_yb%d" % i, [64, 512], BF16) for i in range(3)])
            sm_r = Ring([sb(es, "n_sm%d" % i, [128, 20], F32) for i in range(4)])
            ti_r = Ring([sb(es, "n_ti%d" % i, [128, 128], F32) for i in range(4)])
            sb_r = Ring([sb(es, "n_sb%d" % i, [128, 128], BF16) for i in range(2)])

            def load_q(h, qg):
                qt, qtb = q_r.next()
                S.dma(qt[0:64, :], s_nqT[h * 64:(h + 1) * 64, qg * 512:(qg + 1) * 512], w=[qtb])
                S.dma(qt[64:67, :], Cd["c_qa"][h, :, qg * 512:(qg + 1) * 512], w=[qtb])
                return qt, qtb

            def load_g(h, qg):
                gt, gtb = gt_r.next()
                for br in range(3):
                    S.dma(gt[64:65, br, :], s_ngT[br * 16 + h:br * 16 + h + 1, qg * 512:(qg + 1) * 512], w=[gtb])
                return gt, gtb

            def tile_unit(qt, qtb, Kap, Vap, bias_ap, clamp, mask_ap, maskbufs, pso, psob, first, last, idx):
                ps, pb = psum((4, 5))
                MM(ps[:], Kap, qt[:], True, True, [kvb, qtb], [pb])
                pt, ptb = p_r.next()
                if clamp:
                    tf, tfb = tf_r.next()
                    TSC("dve", tf[:], ps[:], bias_ap, 30.0, ALU.add, ALU.min, [pb, kb], [tfb])
                    ACT(pt[:], tf[:], AF.Exp, [tfb], [ptb])
                else:
                    ACT(pt[:], ps[:], AF.Exp, [pb, kb], [ptb], bias=bias_ap)
                if mask_ap is not None:
                    TT("pool" if idx % 2 else "dve", pt[:], pt[:], mask_ap, ALU.mult, [ptb] + maskbufs, [ptb])
                MM(pso[0:65, :], Vap, pt[:], first, last, [kvb, ptb], [psob])
                return pt, ptb

            def finalize(pso, psob, gt, gtb, br):
                osb, osbb = osb_r.next()
                CP("act", osb[:], pso[0:65, :], [psob], [osbb])
                fr, frb = fr_r.next()
                TSC("dve", fr[64:65, :], osb[64:65, :], 1e-30, None, ALU.max, None, [osbb], [frb])
                RECIP(fr[64:65, :], fr[64:65, :], [frb], [frb])
                TT("dve", fr[64:65, :], fr[64:65, :], gt[64:65, br, :], ALU.mult, [frb, gtb], [frb])
                psb_, psbb = psum((6, 7))
                MM(psb_[0:64, :], onesf[64:65, 0:64], fr[64:65, :], True, True, [cstb, frb], [psbb])
                ctr, ctrb = ctr_r.next()
                TT("dve", ctr[:], osb[0:64, :], psb_[0:64, :], ALU.mult, [osbb, psbb], [ctrb])
                return ctr, ctrb

            for gi in range(2):
                MSET("dve", Ks[:], 1.0, [kvb])
                MSET("pool", Kw[:], 1.0, [kvb])
                MSET("dve", Vs[:], 1.0, [kvb])
                MSET("pool", Vw[:], 1.0, [kvb])
                MSET("dve", Kc[:], 1.0, [kvb])
                MSET("pool", Vc[:], 1.0, [kvb])
                S.dma(Ks[0:64, :], s_nkvT[256 + gi * 64:256 + (gi + 1) * 64, :], w=[kvb])
                S.dma(Kw[0:64, :], s_nkvT[512 + gi * 64:512 + (gi + 1) * 64, :], w=[kvb])
                nkv_v = s_nkv.rearrange("(n p) c -> p n c", p=128)
                S.dma(Vs[:, :, 0:64], nkv_v[:, :, 384 + gi * 64:384 + (gi + 1) * 64], w=[kvb])
                S.dma(Vw[:, :, 0:64], nkv_v[:, :, 640 + gi * 64:640 + (gi + 1) * 64], w=[kvb])
                for j in range(2):
                    MSET("dve", kcs2[:], 0.0, [kcs2b])
                    S.dma(kcs2[0:64, :], s_nkvT[j * 128 + gi * 64:j * 128 + (gi + 1) * 64, :], w=[kcs2b])
                    S.dma(kcs2[64:128, 0:T - 1], s_nkvT[j * 128 + gi * 64:j * 128 + (gi + 1) * 64, 1:T], w=[kcs2b])
                    kv_ = kcs2[:].rearrange("p (n s) -> p n s", s=16)
                    ps, pb = psum((6, 7))
                    for c in range(16):
                        o_ = 2 * c
                        rhs = kv_[:, o_ // 16:o_ // 16 + NCMP, o_ % 16]
                        MM(ps[:64, 0:NCMP], w1s[:, j, c, :], rhs, c == 0, c == 15, [kb, kcs2b], [pb])
                    MSET("dve", AT[:], 0.0, [ATb])
                    ACT(AT[:, 0:NCMP], ps[:64, 0:NCMP], AF.Silu, [pb, kb], [ATb], bias=ccon[:, j:j + 1])
                    if j == 0:
                        ps2, pb2 = psum((6, 7))
                        MM(ps2[:64, 0:NCMP], w2s[:, 0, :], AT[:, 0:NCMP], True, True, [kb, ATb], [pb2])
                        CP("act", Kc[0:64, 0:NCMP], ps2[:64, 0:NCMP], [pb2], [kvb])
                    else:
                        for nt in range(NTC):
                            ps2, pb2 = psum((6, 7))
                            MM(ps2[:, 0:64], AT[:, nt * 128:(nt + 1) * 128], w2s[:, 1, :], True, True, [ATb, kb], [pb2])
                            CP("act", Vc[:, nt, 0:64], ps2[:, 0:64], [pb2], [kvb])
                for qg in range(NQG):
                    q0 = qg * 512
                    for j4 in range(4):
                        MSET("pool", impacc[:, j4, :], 0.0, [impb[j4]])
                    for hh in range(8):
                        h = gi * 8 + hh
                        qt, qtb = load_q(h, qg)
                        gt, gtb = load_g(h, qg)
                        nts = [nt for nt in range(NTC) if 16 * (128 * nt) + 31 <= q0 + 511]
                        pso, psob = psum((0, 1, 2, 3))
                        pts = []
                        for i_, nt in enumerate(nts):
                            full = 16 * (128 * nt + 127) + 31 <= q0
                            mask_ap = None
                            if not full:
                                cc = 2048 * nt + 31 - q0
                                mi = CM_C.index(cc)
                                mask_ap = cm[:, mi * 512:(mi + 1) * 512]
                            bias_ap = kbc[:, (h * 16 + qg) * 4 + nt:(h * 16 + qg) * 4 + nt + 1]
                            pt, ptb = tile_unit(qt, qtb, Kc[:, nt * 128:(nt + 1) * 128], Vc[:, nt, :], bias_ap, not full, mask_ap, [kb],
                                                pso, psob, i_ == 0, i_ == len(nts) - 1, i_)
                            pts.append((nt, pt, ptb))
                        ctr, ctrb = finalize(pso, psob, gt, gtb, 0)
                        S.dma(s_ocmp[h * 64:(h + 1) * 64, q0:q0 + 512], ctr[:], r=[ctrb])
                        for j4 in range(4):
                            psi, psib = psum((6, 7))
                            for i_, (nt, pt, ptb) in enumerate(pts):
                                MM(psi[:, 0:129], pt[:, j4 * 128:(j4 + 1) * 128], ov[:, nt, :], i_ == 0, i_ == len(pts) - 1, [ptb, kb], [psib])
                            sm, smb = sm_r.next()
                            TSC("dve", sm[:, 0:1], psi[:, 128:129], 1e-30, None, ALU.max, None, [psib], [smb])
                            RECIP(sm[:, 1:2], sm[:, 0:1], [smb], [smb])
                            STT("dve", impacc[:, j4, :], psi[:, 0:128], sm[:, 1:2], impacc[:, j4, :], ALU.mult, ALU.add,
                                [psib, smb, impb[j4]], [impb[j4]])
                    for j4 in range(4):
                        qt_ = qg * 4 + j4
                        ti, tib = ti_r.next()
                        TT("dve", ti[:], impacc[:, j4, :], wdt[:, 126 - 2 * qt_:254 - 2 * qt_], ALU.add, [impb[j4], kb], [tib])
                        MSET("dve", ti[:, 0:1], 4e30, [tib])
                        sm, smb = sm_r.next()
                        S.dve(lambda e, o=sm[:, 0:8], i=ti[:]: e.max(out=o, in_=i), [tib], [smb])
                        t2, t2b = ti_r.next()
                        S.dve(lambda e, o=t2[:], r_=sm[:, 0:8], i=ti[:]: e.match_replace(out=o, in_to_replace=r_, in_values=i, imm_value=-3e38),
                              [tib, smb], [t2b])
                        S.dve(lambda e, o=sm[:, 8:16], i=t2[:]: e.max(out=o, in_=i), [t2b], [smb])
                        sbt, sbtb = sb_r.next()
                        TSC("dve", sbt[:], ti[:], sm[:, 15:16], None, ALU.is_ge, None, [tib, smb], [sbtb])
                        ps, pb = psum((6, 7))
                        psv = ps[:].bitcast(BF16)
                        TR(psv[:, 0:128], sbt[:], identb[:], [sbtb, cstb], [pb])
                        CP("act", selT[:, qt_ * 128:(qt_ + 1) * 128], psv[:, 0:128], [pb], [selTb])
                if "s_selT" in dbg:
                    S.dma(s_selT[gi], selT[:], r=[selTb])
                for qg in range(NQG):
                    q0 = qg * 512
                    for hq in range(2):
                        heads = [gi * 8 + hq * 4 + i for i in range(4)]
                        qs = [load_q(h, qg) for h in heads]
                        gs = [load_g(h, qg) for h in heads]
                        accs = []
                        for h in heads:
                            ac, acb = accy_r.next()
                            S.dma(ac[:], s_ocmp[h * 64:(h + 1) * 64, q0:q0 + 512], w=[acb])
                            accs.append((ac, acb))
                        for br in (1, 2):
                            if br == 1:
                                kts = list(range(0, min(4 * qg + 4, NT)))
                            else:
                                kts = [kt for kt in range(4 * qg - 4, 4 * qg + 4) if 0 <= kt < NT]
                            psos = [(PS[i], PSB[i]) for i in range(4)]
                            for i_, kt in enumerate(kts):
                                m = kt - 4 * qg
                                if br == 1:
                                    psm, psmb = psum((6, 7))
                                    MM(psm[:], E[:, kt * 128:(kt + 1) * 128], selT[:, q0:q0 + 512], True, True, [kb, selTb], [psmb])
                                    mt, mtb = m_r.next()
                                    if m >= 0:
                                        TT("dve", mt[:], psm[:], wm[:, (m + 4) * 512:(m + 5) * 512], ALU.mult, [psmb, kb], [mtb])
                                    else:
                                        CP("act", mt[:], psm[:], [psmb], [mtb])
                                    mask_ap, mbufs = mt[:], [mtb]
                                    Kt, Vt = Ks, Vs
                                else:
                                    mask_ap, mbufs = wm[:, (m + 4) * 512:(m + 5) * 512], [kb]
                                    Kt, Vt = Kw, Vw
                                for i4, h in enumerate(heads):
                                    bias_ap = kbsw[:, h * 68 + m + 64:h * 68 + m + 65]
                                    tile_unit(qs[i4][0], qs[i4][1], Kt[:, kt * 128:(kt + 1) * 128], Vt[:, kt, :], bias_ap, m >= 0, mask_ap, mbufs,
                                              psos[i4][0], psos[i4][1], i_ == 0, i_ == len(kts) - 1, i4)
                            for i4, h in enumerate(heads):
                                ctr, ctrb = finalize(psos[i4][0], psos[i4][1], gs[i4][0], gs[i4][1], br)
                                ac, acb = accs[i4]
                                TT("pool", ac[:], ac[:], ctr[:], ALU.add, [acb, ctrb], [acb])
                        for i4, h in enumerate(heads):
                            ac, acb = accs[i4]
                            yb_, ybb = ybf_r.next()
                            CP("act", yb_[:], ac[:], [acb], [ybb])
                            S.dma(s_yT[1][h * 64:(h + 1) * 64, q0:q0 + 512], yb_[:], r=[ybb])
                S.barrier()
                S.flush()

    PHASES = {"cast": phase_cast, "p1": phase_p1, "p2": phase_p2, "gla": phase_gla, "ssd": phase_ssd, "nsa": phase_nsa}
    return nc, S, PHASES, es0


_CACHE = {}


def kernel(**inputs):
    x = np.ascontiguousarray(np.asarray(inputs["x"], np.float32))
    B, T, _ = x.shape
    NL = int(np.asarray(inputs["w_in"]).shape[0])
    consts = make_consts(T)
    nc, S, PH, es0 = build(T, NL, consts)
    PH["cast"]()
    for l in range(NL):
        PH["p1"](l)
        PH["gla"](l)
        PH["ssd"](l)
        PH["nsa"](l)
        PH["p2"](l)
    es0.close()
    in_maps = []
    for b in range(B):
        m = {"x": x[b]}
        for n, _s in W_SPECS:
            m[n] = np.ascontiguousarray(np.asarray(inputs[n], np.float32))
        m.update(consts)
        in_maps.append(m)
    res = run_bass_kernel_spmd(nc, in_maps, core_ids=list(range(B)))
    return np.stack([np.asarray(res.results[b]["out"], np.float32) for b in range(B)], axis=0)
```

```python
import numpy as np
import ml_dtypes
from contextlib import ExitStack
import concourse.bass as bass
import concourse.mybir as mybir
from concourse.bass_utils import run_bass_kernel_spmd

F32 = mybir.dt.float32
BF16 = mybir.dt.bfloat16
ALU = mybir.AluOpType
AF = mybir.ActivationFunctionType
AX = mybir.AxisListType

D = 2048
DIN = 15184
DFF = 5632
NCORES = 4
C_GQ, C_GK, C_GV, C_GR, C_GA = 0, 1024, 2048, 3072, 4096
C_NQ, C_NKV, C_NG = 4112, 5136, 5904
C_SZ, C_SX, C_SDT, C_MG = 5952, 6976, 9024, 9040
EPS = 1e-6


class Buf:
    __slots__ = ("w", "r")

    def __init__(self):
        self.w = None
        self.r = {}


class Sched:
    ENG = ("pe", "act", "dve", "pool", "sp")

    def __init__(self, nc, es, ndma=14):
        self.nc = nc
        self.q = {e: [] for e in self.ENG}
        self.cnt = {e: 0 for e in self.ENG[:4]}
        names = list(self.ENG[:4]) + ["d%d" % i for i in range(ndma)]
        self.sem = {n: es.enter_context(nc.semaphore("s_" + n)) for n in names}
        self.semval = {n: 0 for n in names}
        self.waited = {e: {} for e in self.ENG}
        self.ndma = ndma
        self.dma_i = 0
        self.n_ops = 0

    def _deps(self, eng, reads, writes):
        deps = {}
        for b in reads:
            if b.w is not None:
                s, v = b.w
                if deps.get(s, 0) < v:
                    deps[s] = v
        for b in writes:
            if b.w is not None:
                s, v = b.w
                if deps.get(s, 0) < v:
                    deps[s] = v
            for s, v in b.r.items():
                if deps.get(s, 0) < v:
                    deps[s] = v
        wd = self.waited[eng]
        q = self.q[eng]
        for s, v in deps.items():
            if eng == "pe" and s == "pe":
                continue
            if wd.get(s, 0) < v:
                wd[s] = v
                q.append((0, s, v))

    def _mark(self, tok, reads, writes):
        s, v = tok
        for b in reads:
            if b.r.get(s, 0) < v:
                b.r[s] = v
        for b in writes:
            b.w = tok
            b.r = {}

    def op(self, eng, fn, reads=(), writes=()):
        self._deps(eng, reads, writes)
        self.cnt[eng] += 1
        c = self.cnt[eng]
        self.semval[eng] = c
        self.q[eng].append((1, fn, eng))
        self._mark((eng, c), reads, writes)
        self.n_ops += 1

    def pe(self, fn, r=(), w=()):
        self.op("pe", fn, r, w)

    def act(self, fn, r=(), w=()):
        self.op("act", fn, r, w)

    def dve(self, fn, r=(), w=()):
        self.op("dve", fn, r, w)

    def pool(self, fn, r=(), w=()):
        self.op("pool", fn, r, w)

    def dma(self, out, in_, r=(), w=(), q="sp", slow=False):
        k = self.dma_i % self.ndma
        n = self.dma_i // self.ndma
        self.dma_i += 1
        s = "d%d" % k
        if n > 0:
            wd = self.waited[q]
            if wd.get(s, 0) < 16 * n:
                wd[s] = 16 * n
                self.q[q].append((0, s, 16 * n))
        self._deps(q, r, w)
        v = 16 * (n + 1)
        self.semval[s] = v
        self.q[q].append((2, out, in_, s, slow))
        self._mark((s, v), r, w)
        self.n_ops += 1

    def barrier(self):
        for e in self.ENG:
            wd = self.waited[e]
            q = self.q[e]
            for s, v in self.semval.items():
                if v > 0 and wd.get(s, 0) < v and not (e == "pe" and s == "pe"):
                    wd[s] = v
                    q.append((0, s, v))

    def flush(self):
        with self.nc.Block() as block:
            self.emit(block)
        for e in self.ENG:
            self.q[e] = []

    def emit(self, block):
        names = {"pe": "tensor", "act": "scalar", "dve": "vector", "pool": "gpsimd", "sp": "sync"}
        sem = self.sem
        for e, attr in names.items():
            items = self.q[e]

            def body(eng, items=items):
                for it in items:
                    if it[0] == 0:
                        eng.wait_ge(sem[it[1]], it[2])
                    elif it[0] == 1:
                        it[1](eng).then_inc(sem[it[2]], 1)
                    else:
                        if it[4]:
                            eng.dma_start(out=it[1], in_=it[2], allow_slow_non_contiguous=True).then_inc(sem[it[3]], 16)
                        else:
                            eng.dma_start(out=it[1], in_=it[2]).then_inc(sem[it[3]], 16)

            getattr(block, attr)(body)


class Ring:
    def __init__(self, tiles):
        self.tiles = tiles
        self.bufs = [Buf() for _ in tiles]
        self.i = 0

    def next(self):
        k = self.i % len(self.tiles)
        self.i += 1
        return self.tiles[k], self.bufs[k]


class Feeder:
    def __init__(self, S, ring, jobs):
        self.S, self.ring, self.jobs = S, ring, jobs
        self.R = len(ring.tiles)
        self.issued = 0
        self.got = 0
        self.done = 0
        self.slots = {}

    def _issue(self):
        i = self.issued
        view_fn, src = self.jobs[i]
        t, b = self.ring.next()
        v = view_fn(t)
        self.S.dma(v, src, w=[b])
        self.slots[i] = (v, b)
        self.issued += 1

    def pump(self):
        while self.issued < len(self.jobs) and self.issued < self.done + self.R:
            self._issue()

    def get(self):
        self.pump()
        assert self.got < self.issued, "feeder ring too small"
        r = self.slots.pop(self.got)
        self.got += 1
        return r

    def release(self, n=1):
        self.done += n
        self.pump()


def _bf(a):
    return np.asarray(a, np.float32).astype(ml_dtypes.bfloat16)


def make_consts(T):
    c = {}
    i = np.arange(128)
    same = (i[:, None] // 64) == (i[None, :] // 64)
    up = i[:, None] <= i[None, :]
    c["c_identb"] = _bf(np.eye(128))
    c["c_identf"] = np.eye(128, dtype=np.float32)
    c["c_tris"] = np.where(same & up, -1.0 / 16.0, 0.0).astype(np.float32)
    c["c_tri1"] = np.where(same & up, 1.0, 0.0).astype(np.float32)
    c["c_blk"] = np.where(same, 1.0, 0.0).astype(np.float32)
    ch = np.zeros((128, 256), np.float32)
    ch[:64, :128] = 1.0
    ch[64:, 128:] = 1.0
    c["c_chsel"] = ch
    c["c_amask"] = _bf(np.where(same & up, 1.0, 0.0))
    mneg = np.where(same & up, 0.0, -1e9).astype(np.float32)
    c["c_mneg4"] = np.tile(mneg, (1, 4))
    c["c_onesf"] = np.ones((128, 128), np.float32)
    slopes = (2.0 ** (-8.0 * np.arange(1, 17) / 16.0)).astype(np.float64)
    irel = np.arange(512, dtype=np.float64)
    qa = np.zeros((16, 3, 512), np.float32)
    for h in range(16):
        v = (-slopes[h] * irel).astype(np.float32)
        hi = v.astype(ml_dtypes.bfloat16).astype(np.float32)
        r1 = v - hi
        mid = r1.astype(ml_dtypes.bfloat16).astype(np.float32)
        lo = (r1 - mid).astype(ml_dtypes.bfloat16).astype(np.float32)
        qa[h, 0], qa[h, 1], qa[h, 2] = hi, mid, lo
    c["c_qa"] = _bf(np.tile(qa, (1, 1, T // 512)))
    j = np.arange(128, dtype=np.float64)
    dv = np.arange(-64, 4, dtype=np.float64)
    kb = slopes[None, :, None] * (128.0 * dv[None, None, :] + j[:, None, None])
    c["c_kbsw"] = kb.astype(np.float32).reshape(128, 16 * 68)
    qg = np.arange(16, dtype=np.float64)
    nt = np.arange(4, dtype=np.float64)
    kc = slopes[None, :, None, None] * (16.0 * (128.0 * nt[None, None, None, :] + j[:, None, None, None]) + 31.0
                                        - 512.0 * qg[None, None, :, None])
    c["c_kbc"] = kc.astype(np.float32).reshape(128, 16 * 16 * 4)
    ii = np.arange(512)
    wm = np.zeros((8, 128, 512), np.float32)
    for m in range(8):
        u = ii[None, :] - i[:, None] + 512 - 128 * m
        wm[m] = ((u >= 0) & (u < 512)).astype(np.float32)
    c["c_wm"] = _bf(wm.transpose(1, 0, 2).reshape(128, 8 * 512))
    cm = np.zeros((5, 128, 512), np.float32)
    for k, cc in enumerate([31, -481, -993, -1505, -2017]):
        cm[k] = ((ii[None, :] - 16 * i[:, None]) >= cc).astype(np.float32)
    c["c_cm"] = _bf(cm.transpose(1, 0, 2).reshape(128, 5 * 512))
    n_cmp = (8192 - 32) // 16 + 1
    n_sel = 128
    ov = np.zeros((512, 129), np.float32)
    tok = (np.arange(n_cmp) * 16)[:, None] + np.arange(32)[None, :]
    np.add.at(ov, (np.repeat(np.arange(n_cmp), 32), (tok // 64).ravel()), 1.0 / 32)
    ov[:n_cmp, 128] = 1.0
    c["c_ov"] = _bf(ov.reshape(4, 128, 129).transpose(1, 0, 2).reshape(128, 4 * 129))
    e = np.zeros((128, T), np.float32)
    e[np.arange(T) // 64, np.arange(T)] = 1.0
    c["c_e"] = _bf(e)
    wd = np.zeros((128, 254), np.float32)
    dd = np.arange(254) - 126
    cur = (i >= 64).astype(np.int64)
    for q in range(128):
        row = np.zeros(254, np.float32)
        fut = dd > cur[q]
        row[fut] = -1e30 - 1e28 * dd[fut]
        row[dd == cur[q]] = 2e30
        row[dd == cur[q] - 1] = 3e30
        wd[q] = row
    c["c_wd"] = wd
    c["c_ones_bf"] = _bf(np.ones((128, 512)))
    return c


W_SPECS = [
    ("w_in", (D, DIN)), ("gla_a2", (16, 1024)), ("gla_a_bias", (1024,)), ("gla_norm", (256,)),
    ("nsa_cmp_pos", (2, 32, 64)), ("nsa_cmp_w1", (2, 2048, 64)), ("nsa_cmp_w2", (2, 64, 64)),
    ("ssm_conv_w", (4, 2048)), ("ssm_conv_b", (2048,)), ("ssm_dt_bias", (16,)), ("ssm_a_log", (16,)),
    ("ssm_d", (16,)), ("ssm_norm", (1024,)), ("w_branch", (3, 1024, D)), ("w_out", (D, D)),
    ("norm_pre_mix", (D,)), ("norm_post_mix", (D,)), ("norm_pre_ffn", (D,)), ("norm_post_ffn", (D,)),
    ("w_ffn_gate", (D, DFF)), ("w_ffn_up", (D, DFF)), ("w_ffn_down", (DFF, D)),
]


def build(T, NL, consts, dbg=()):
    nc = bass.Bass("TRN2", target_bir_lowering=False)
    es0 = ExitStack()
    S = Sched(nc, es0)
    NT = T // 128

    def din(name, shape, dt=F32):
        return nc.dram_tensor(name, list(shape), dt, kind="ExternalInput").ap()

    def dscr(name, shape, dt):
        if name in dbg:
            return nc.dram_tensor(name, list(shape), dt, kind="ExternalOutput").ap()
        return nc.dram_tensor(name, list(shape), dt).ap()

    x_in = din("x", (T, D))
    Wd = {n: din(n, (NL,) + s) for n, s in W_SPECS}
    Cd = {n: din(n, a.shape, BF16 if a.dtype != np.float32 else F32) for n, a in consts.items()}
    out = nc.dram_tensor("out", [T, D], F32, kind="ExternalOutput").ap()

    wb_in = dscr("wb_in", (NL, D, DIN), BF16)
    wb_br = dscr("wb_br", (NL, 3072, D), BF16)
    wb_out = dscr("wb_out", (NL, D, D), BF16)
    wb_g = dscr("wb_g", (NL, D, DFF), BF16)
    wb_u = dscr("wb_u", (NL, D, DFF), BF16)
    wb_d = dscr("wb_d", (NL, DFF, D), BF16)
    s_qT = dscr("s_qT", (1024, T), F32)
    s_kT = dscr("s_kT", (1024, T), F32)
    s_gv = dscr("s_gv", (T, 1024), BF16)
    s_gr = dscr("s_gr", (T, 1024), F32)
    s_aT = dscr("s_aT", (16, T), F32)
    s_nqT = dscr("s_nqT", (1024, T), BF16)
    s_nkvT = dscr("s_nkvT", (768, T), BF16)
    s_nkv = dscr("s_nkv", (T, 768), BF16)
    s_ngT = dscr("s_ngT", (48, T), F32)
    s_sz = dscr("s_sz", (T, 1024), F32)
    s_sxT = dscr("s_sxT", (2048, T), F32)
    s_sdt = dscr("s_sdt", (T, 16), F32)
    s_mgT = dscr("s_mgT", (6144, T), BF16)
    s_yT = [dscr("s_yT%d" % i, (1024, T), BF16) for i in range(3)]
    s_selT = dscr("s_selT", (2, 128, T), BF16)

    PS = [es0.enter_context(nc.psum_tensor("ps%d" % i, [128, 512], F32)) for i in range(8)]
    PSB = [Buf() for _ in range(8)]
    ps_i = [0]

    def psum(pool=None):
        if pool is not None:
            k = pool[ps_i[0] % len(pool)]
        else:
            k = ps_i[0] % 8
        ps_i[0] += 1
        return PS[k], PSB[k]

    sb_n = [0]

    def sb(es, name, shape, dt):
        sb_n[0] += 1
        return es.enter_context(nc.sbuf_tensor("%s_%d" % (name, sb_n[0]), list(shape), dt))

    def ACT(out_, in_, func, r, w, **kw):
        S.act(lambda e: e.activation(out=out_, in_=in_, func=func, **kw), r, w)

    def TSC(eng, out_, in0, s1, s2, op0, op1, r, w):
        if op1 is None:
            S.op(eng, lambda e: e.tensor_scalar(out=out_, in0=in0, scalar1=s1, scalar2=None, op0=op0), r, w)
        else:
            S.op(eng, lambda e: e.tensor_scalar(out=out_, in0=in0, scalar1=s1, scalar2=s2, op0=op0, op1=op1), r, w)

    def STT(eng, out_, in0, sc, in1, op0, op1, r, w):
        S.op(eng, lambda e: e.scalar_tensor_tensor(out=out_, in0=in0, scalar=sc, in1=in1, op0=op0, op1=op1), r, w)

    def TT(eng, out_, in0, in1, op, r, w):
        S.op(eng, lambda e: e.tensor_tensor(out=out_, in0=in0, in1=in1, op=op), r, w)

    def CP(eng, out_, in_, r, w):
        if eng == "act":
            S.act(lambda e: e.activation(out=out_, in_=in_, func=AF.Copy), r, w)
        else:
            S.op(eng, lambda e: e.tensor_copy(out=out_, in_=in_), r, w)

    def MM(out_, lhsT, rhs, start, stop, r, w):
        S.pe(lambda e: e.matmul(out_, lhsT, rhs, start=start, stop=stop, skip_group_check=True), r, w)

    def TR(out_, in_, ident, r, w):
        S.pe(lambda e: e.transpose(out_, in_, ident), r, w)

    def RSUM(eng, out_, in_, r, w):
        S.op(eng, lambda e: e.reduce_sum(out=out_, in_=in_, axis=AX.X), r, w)

    def RECIP(out_, in_, r, w):
        S.dve(lambda e: e.reciprocal(out=out_, in_=in_), r, w)

    def MSET(eng, ap, val, w):
        S.op(eng, lambda e: e.memset(ap, val), (), w)

    cst = {}
    cstb = Buf()
    for n in ("c_identb", "c_identf", "c_tris", "c_tri1", "c_blk", "c_chsel", "c_amask", "c_mneg4", "c_onesf"):
        a = consts[n]
        t = sb(es0, "k" + n, a.shape, BF16 if a.dtype != np.float32 else F32)
        S.dma(t[:], Cd[n][:, :], w=[cstb])
        cst[n] = t
    identb, identf = cst["c_identb"], cst["c_identf"]

    def bview(dram2d):
        return dram2d.rearrange("(kc p) n -> p kc n", p=128)

    def phase_cast():
        with ExitStack() as es:
            ring = Ring([sb(es, "cst%d" % i, [128, 8192], BF16) for i in range(3)])
            jobs = []
            for l in range(NL):
                jobs += [(Wd["w_in"][l], wb_in[l], D, DIN),
                         (Wd["w_branch"][l].rearrange("b k n -> (b k) n"), wb_br[l], 3072, D),
                         (Wd["w_out"][l], wb_out[l], D, D),
                         (Wd["w_ffn_gate"][l], wb_g[l], D, DFF), (Wd["w_ffn_up"][l], wb_u[l], D, DFF),
                         (Wd["w_ffn_down"][l], wb_d[l], DFF, D)]
            for src, dst, K, N in jobs:
                for r0 in range(0, K, 128):
                    for c0 in range(0, N, 8192):
                        w_ = min(8192, N - c0)
                        t, b = ring.next()
                        S.dma(t[:, :w_], src[r0:r0 + 128, c0:c0 + w_], w=[b], q="pool")
                        S.dma(dst[r0:r0 + 128, c0:c0 + w_], t[:, :w_], r=[b], q="sp")
            S.barrier()
            S.flush()

    def rms_to_bf16(xt, xb, gain, gainb, hb, hbb, junk, junkb, sm, smb):
        ACT(junk[:], xt, AF.Square, [xb], [junkb])
        RSUM("dve", sm[:, 0:1], junk[:], [junkb], [smb])
        ACT(sm[:, 1:2], sm[:, 0:1], AF.Sqrt, [smb], [smb], scale=1.0 / D, bias=EPS)
        RECIP(sm[:, 2:3], sm[:, 1:2], [smb], [smb])
        STT("dve", hb, xt, sm[:, 2:3], gain[:], ALU.mult, ALU.mult, [xb, smb, gainb], [hbb])

    def transpose_rows(hb, hbb, dstT, dstb, col0, nk=16, evac="act", pool=None):
        for half in range(0, nk, 8):
            n = min(8, nk - half)
            ps, pb = psum(pool)
            psv = ps[:].bitcast(BF16)
            for k in range(n):
                TR(psv[:, k * 128:(k + 1) * 128], hb[:, (half + k) * 128:(half + k + 1) * 128], identb[:], [hbb, cstb], [pb])
            CP(evac, dstT[:, half:half + n, col0:col0 + 128],
               psv[:, :n * 128].rearrange("p (k t) -> p k t", t=128), [pb], [dstb])

    TS1 = min(1024, T)
    P1_JOBS = [
        (C_GQ, 1024, "F", s_qT, None), (C_GK, 1024, "F", s_kT, None),
        (C_GV, 1024, "T", s_gv, None), (C_GR, 1024, "T", s_gr, AF.Silu),
        (C_GA, 16, "F", s_aT, None),
        (C_NQ, 1024, "F", s_nqT, "scale8"),
        (C_NKV, 768, "F", s_nkvT, None), (C_NKV, 768, "T", s_nkv, None),
        (C_NG, 48, "F", s_ngT, AF.Sigmoid),
        (C_SZ, 1024, "T", s_sz, AF.Silu), (C_SX, 2048, "F", s_sxT, None), (C_SDT, 16, "T", s_sdt, None),
        (C_MG, 6144, "F", s_mgT, AF.Sigmoid),
    ]

    def phase_p1(l):
        xsrc = x_in if l == 0 else out
        with ExitStack() as es:
            gain = sb(es, "p1gain", [128, D], F32)
            gainb = Buf()
            S.dma(gain[:], Wd["norm_pre_mix"][l:l + 1, :].partition_broadcast(128), w=[gainb])
            xt_r = Ring([sb(es, "p1xt%d" % i, [128, D], F32) for i in range(2)])
            hb_r = Ring([sb(es, "p1hb%d" % i, [128, D], BF16) for i in range(2)])
            junk = sb(es, "p1junk", [128, D], F32)
            junkb = Buf()
            sm_r = Ring([sb(es, "p1sm%d" % i, [128, 4], F32) for i in range(4)])
            hT = sb(es, "p1hT", [128, 16, TS1], BF16)
            hTb = Buf()
            w_r = Ring([sb(es, "p1w%d" % i, [128, 16, 512], BF16) for i in range(3)])
            stf_r = Ring([sb(es, "p1sf%d" % i, [128, 512], F32) for i in range(3)])
            stb_r = Ring([sb(es, "p1sb%d" % i, [128, 512], BF16) for i in range(3)])
            wv = bview(wb_in[l])
            wjobs = []
            for _ts in range(T // TS1):
                for (c0, ncols, mode, dest, fn) in P1_JOBS:
                    for cb in range(0, ncols, 512):
                        nb = min(512, ncols - cb)
                        wjobs.append(((lambda t, nb=nb: t[:, :, :nb]), wv[:, :, c0 + cb:c0 + cb + nb]))
            feed = Feeder(S, w_r, wjobs)
            for ts in range(T // TS1):
                tb = ts * TS1
                for tt in range(TS1 // 128):
                    t0 = tb + tt * 128
                    xt, xb = xt_r.next()
                    S.dma(xt[:], xsrc[t0:t0 + 128, :], w=[xb])
                    hb, hbb = hb_r.next()
                    sm, smb = sm_r.next()
                    rms_to_bf16(xt[:], xb, gain, gainb, hb[:], hbb, junk, junkb, sm, smb)
                    transpose_rows(hb, hbb, hT, hTb, tt * 128)
                for (c0, ncols, mode, dest, fn) in P1_JOBS:
                    isbf = dest.dtype == BF16
                    for cb in range(0, ncols, 512):
                        nb = min(512, ncols - cb)
                        wt, wtb = feed.get()
                        if mode == "F":
                            for fs in range(0, nb, 128):
                                nf = min(128, nb - fs)
                                for tg in range(0, TS1, 512):
                                    ps, pb = psum()
                                    for kc in range(16):
                                        MM(ps[:nf, :], wt[:, kc, fs:fs + nf], hT[:, kc, tg:tg + 512], kc == 0, kc == 15,
                                           [wtb, hTb], [pb])
                                    st, stb = (stb_r if isbf else stf_r).next()
                                    if fn == "scale8":
                                        ACT(st[:nf, :], ps[:nf, :], AF.Copy, [pb], [stb], scale=0.125)
                                    elif fn is None:
                                        CP("dve", st[:nf, :], ps[:nf, :], [pb], [stb])
                                    else:
                                        ACT(st[:nf, :], ps[:nf, :], fn, [pb], [stb])
                                    f0 = cb + fs
                                    S.dma(dest[f0:f0 + nf, tb + tg:tb + tg + 512], st[:nf, :], r=[stb])
                        else:
                            for tt in range(TS1 // 128):
                                ps, pb = psum()
                                for kc in range(16):
                                    MM(ps[:, :nb], hT[:, kc, tt * 128:(tt + 1) * 128], wt[:, kc, :nb], kc == 0, kc == 15,
                                       [wtb, hTb], [pb])
                                st, stb = (stb_r if isbf else stf_r).next()
                                if fn is None:
                                    CP("dve", st[:, :nb], ps[:, :nb], [pb], [stb])
                                else:
                                    ACT(st[:, :nb], ps[:, :nb], fn, [pb], [stb])
                                t0 = tb + tt * 128
                                S.dma(dest[t0:t0 + 128, cb:cb + nb], st[:, :nb], r=[stb])
                        feed.release()
            S.barrier()
            S.flush()

    TS2 = min(512, T)
    NT2 = TS2 // 128

    def phase_p2(l):
        xsrc = x_in if l == 0 else out
        with ExitStack() as es:
            gA = sb(es, "p2gA", [128, D], F32)
            gB = sb(es, "p2gB", [128, D], F32)
            gAb, gBb = Buf(), Buf()
            big = sb(es, "p2big", [128, 48 * TS2], BF16)
            yT = [big[:, b * 8 * TS2:(b + 1) * 8 * TS2].rearrange("p (k t) -> p k t", t=TS2) for b in range(3)]
            mT = big[:, 32 * TS2:48 * TS2].rearrange("p (k t) -> p k t", t=TS2)
            zf = big[:, 0:32 * TS2].bitcast(F32).rearrange("p (n c) -> p n c", c=D)
            aT = big[:, 0:44 * TS2].rearrange("p (k t) -> p k t", t=TS2)
            yb = Buf()
            mb = [Buf() for _ in range(16)]
            zb = [Buf() for _ in range(NT2)]
            ab = [Buf() for _ in range(44)]
            xt = sb(es, "p2xt", [128, NT2, D], F32)
            xtb = Buf()
            h2T = sb(es, "p2h2T", [128, 16, TS2], BF16)
            h2Tb = Buf()
            w_r = Ring([sb(es, "p2w%d" % i, [128, 8192], BF16) for i in range(3)])
            g_r = Ring([sb(es, "p2g%d" % i, [128, 3, TS2], BF16) for i in range(2)])
            junk = sb(es, "p2junk", [128, D], F32)
            junkb = Buf()
            hb_r = Ring([sb(es, "p2hb%d" % i, [128, D], BF16) for i in range(2)])
            x1_r = Ring([sb(es, "p2x1%d" % i, [128, D], F32) for i in range(1)])
            sm_r = Ring([sb(es, "p2sm%d" % i, [128, 4], F32) for i in range(4)])
            tmp_r = Ring([sb(es, "p2tmp%d" % i, [128, TS2], F32) for i in range(3)])
            wbr = wb_br[l].rearrange("(b kc p) n -> p b kc n", p=128, kc=8)
            wov = bview(wb_out[l])
            wgv, wuv = bview(wb_g[l]), bview(wb_u[l])
            wdv = bview(wb_d[l])
            mgv = s_mgT.rearrange("(b f) t -> f b t", b=3)
            wjobs = []
            v8 = lambda t: t[:, :4096].rearrange("p (k n) -> p k n", n=512)
            v16 = lambda t: t[:].rearrange("p (k n) -> p k n", n=512)
            v11 = lambda t: t[:, :11 * 512].rearrange("p (k n) -> p k n", n=512)
            for _ts in range(T // TS2):
                for cb in range(4):
                    for b in range(3):
                        wjobs.append((v8, wbr[:, b, :, cb * 512:(cb + 1) * 512]))
                for cb in range(4):
                    wjobs.append((v16, wov[:, :, cb * 512:(cb + 1) * 512]))
                for fb in range(11):
                    wjobs.append((v16, wgv[:, :, fb * 512:(fb + 1) * 512]))
                    wjobs.append((v16, wuv[:, :, fb * 512:(fb + 1) * 512]))
                for cb in range(4):
                    for kq in range(4):
                        wjobs.append((v11, wdv[:, kq * 11:(kq + 1) * 11, cb * 512:(cb + 1) * 512]))
            feed = Feeder(S, w_r, wjobs)
            for ts in range(T // TS2):
                tb = ts * TS2
                S.dma(gA[:], Wd["norm_post_mix"][l:l + 1, :].partition_broadcast(128), w=[gAb])
                if ts == 0:
                    S.dma(gB[:], Wd["norm_pre_ffn"][l:l + 1, :].partition_broadcast(128), w=[gBb])
                for b in range(3):
                    S.dma(yT[b], s_yT[b].rearrange("(k p) t -> p k t", p=128)[:, :, tb:tb + TS2], w=[yb])
                for n in range(NT2):
                    S.dma(xt[:, n, :], xsrc[tb + n * 128:tb + (n + 1) * 128, :], w=[xtb])
                for cb in range(4):
                    wts = []
                    for b in range(3):
                        wts.append(feed.get())
                    for fs in range(4):
                        fc = cb * 4 + fs
                        gt, gtb = g_r.next()
                        S.dma(gt[:], mgv[fc * 128:(fc + 1) * 128, :, tb:tb + TS2], w=[gtb])
                        tmp, tmpb = tmp_r.next()
                        for b in range(3):
                            ps, pb = psum()
                            wtv, wtb = wts[b]
                            for kc in range(8):
                                MM(ps[:, :TS2], wtv[:, kc, fs * 128:(fs + 1) * 128], yT[b][:, kc, :], kc == 0, kc == 7,
                                   [wtb, yb], [pb])
                            if b == 0:
                                TT("dve", tmp[:], ps[:, :TS2], gt[:, 0, :], ALU.mult, [pb, gtb], [tmpb])
                            elif b == 1:
                                tmp2, tmp2b = tmp_r.next()
                                TT("dve", tmp2[:], ps[:, :TS2], gt[:, 1, :], ALU.mult, [pb, gtb], [tmp2b])
                                TT("pool", tmp[:], tmp[:], tmp2[:], ALU.add, [tmpb, tmp2b], [tmpb])
                            else:
                                tmp2, tmp2b = tmp_r.next()
                                TT("dve", tmp2[:], ps[:, :TS2], gt[:, 2, :], ALU.mult, [pb, gtb], [tmp2b])
                                TT("pool", mT[:, fc, :], tmp[:], tmp2[:], ALU.add, [tmpb, tmp2b], [mb[fc]])
                    feed.release(3)
                S.barrier()
                for cb in range(4):
                    wtv, wtb = feed.get()
                    for n in range(NT2):
                        ps, pb = psum()
                        for kc in range(16):
                            MM(ps[:], mT[:, kc, n * 128:(n + 1) * 128], wtv[:, kc, :], kc == 0, kc == 15, [wtb, mb[kc]], [pb])
                        CP("act", zf[:, n, cb * 512:(cb + 1) * 512], ps[:], [pb], [zb[n]])
                    feed.release()
                for n in range(NT2):
                    sm, smb = sm_r.next()
                    ACT(junk[:], zf[:, n, :], AF.Square, [zb[n]], [junkb])
                    RSUM("dve", sm[:, 0:1], junk[:], [junkb], [smb])
                    ACT(sm[:, 1:2], sm[:, 0:1], AF.Sqrt, [smb], [smb], scale=1.0 / D, bias=EPS)
                    RECIP(sm[:, 2:3], sm[:, 1:2], [smb], [smb])
                    STT("dve", junk[:], zf[:, n, :], sm[:, 2:3], gA[:], ALU.mult, ALU.mult, [zb[n], smb, gAb], [junkb])
                    TT("pool", xt[:, n, :], xt[:, n, :], junk[:], ALU.add, [xtb, junkb], [xtb])
                    S.dma(out[tb + n * 128:tb + (n + 1) * 128, :], xt[:, n, :], r=[xtb])
                    hb, hbb = hb_r.next()
                    sm, smb = sm_r.next()
                    rms_to_bf16(xt[:, n, :], xtb, gB, gBb, hb[:], hbb, junk, junkb, sm, smb)
                    transpose_rows(hb, hbb, h2T, h2Tb, n * 128)
                S.barrier()
                S.dma(gA[:], Wd["norm_post_ffn"][l:l + 1, :].partition_broadcast(128), w=[gAb])
                for fb in range(11):
                    wgv_, wgb = feed.get()
                    wuv_, wub = feed.get()
                    for fs in range(4):
                        psg, pgb = psum()
                        for kc in range(16):
                            MM(psg[:, :TS2], wgv_[:, kc, fs * 128:(fs + 1) * 128], h2T[:, kc, :], kc == 0, kc == 15, [wgb, h2Tb], [pgb])
                        psu, pub = psum()
                        for kc in range(16):
                            MM(psu[:, :TS2], wuv_[:, kc, fs * 128:(fs + 1) * 128], h2T[:, kc, :], kc == 0, kc == 15, [wub, h2Tb], [pub])
                        tmp, tmpb = tmp_r.next()
                        ACT(tmp[:], psg[:, :TS2], AF.Silu, [pgb], [tmpb])
                        TT("dve", aT[:, fb * 4 + fs, :], tmp[:], psu[:, :TS2], ALU.mult, [tmpb, pub], [ab[fb * 4 + fs]])
                    feed.release(2)
                for cb in range(4):
                    pss = [psum() for _ in range(NT2)]
                    for kq in range(4):
                        wtv, wtb = feed.get()
                        for n in range(NT2):
                            ps, pb = pss[n]
                            for k in range(11):
                                kc = kq * 11 + k
                                MM(ps[:], aT[:, kc, n * 128:(n + 1) * 128], wtv[:, k, :], kc == 0, kc == 43, [wtb, ab[kc]], [pb])
                        feed.release()
                    for n in range(NT2):
                        ps, pb = pss[n]
                        CP("act", xt[:, n, cb * 512:(cb + 1) * 512], ps[:], [pb], [xtb])
                for n in range(NT2):
                    x1, x1b = x1_r.next()
                    S.dma(x1[:], out[tb + n * 128:tb + (n + 1) * 128, :], w=[x1b])
                    sm, smb = sm_r.next()
                    ACT(junk[:], xt[:, n, :], AF.Square, [xtb], [junkb])
                    RSUM("dve", sm[:, 0:1], junk[:], [junkb], [smb])
                    ACT(sm[:, 1:2], sm[:, 0:1], AF.Sqrt, [smb], [smb], scale=1.0 / D, bias=EPS)
                    RECIP(sm[:, 2:3], sm[:, 1:2], [smb], [smb])
                    STT("dve", junk[:], xt[:, n, :], sm[:, 2:3], gA[:], ALU.mult, ALU.mult, [xtb, smb, gAb], [junkb])
                    TT("pool", x1[:], x1[:], junk[:], ALU.add, [x1b, junkb], [x1b])
                    S.dma(out[tb + n * 128:tb + (n + 1) * 128, :], x1[:], r=[x1b])
                S.barrier()
                S.flush()

    def phase_gla(l):
        TG = 512
        with ExitStack() as es:
            wa2 = sb(es, "g_wa2", [17, 1024], F32)
            wa2b = Buf()
            S.dma(wa2[0:16, :], Wd["gla_a2"][l], w=[wa2b])
            S.dma(wa2[16:17, :], Wd["gla_a_bias"][l:l + 1, :], w=[wa2b])
            gng = sb(es, "g_gng", [128, 256], F32)
            gngb = Buf()
            S.dma(gng[:], Wd["gla_norm"][l:l + 1, :].partition_broadcast(128), w=[gngb])
            am4 = sb(es, "g_am4", [128, 512], BF16)
            am4b = Buf()
            for h in range(4):
                S.dma(am4[:, h * 128:(h + 1) * 128], Cd["c_amask"][:, :], w=[am4b])
            aTa = sb(es, "g_aTa", [17, TG], F32)
            aTab = Buf()
            MSET("dve", aTa[:], 1.0, [aTab])
            lt = sb(es, "g_lt", [128, 4, 1024], F32)
            ltb = [Buf() for _ in range(4)]
            Eq = sb(es, "g_Eq", [128, 8, TG], F32)
            Eqb = [Buf() for _ in range(8)]
            Ei_r = Ring([sb(es, "g_Ei%d" % i, [128, TG], F32) for i in range(2)])
            q_r = Ring([sb(es, "g_q%d" % i, [128, TG], F32) for i in range(2)])
            k_r = Ring([sb(es, "g_k%d" % i, [128, TG], F32) for i in range(2)])
            e_r = Ring([sb(es, "g_e%d" % i, [128, 512], F32) for i in range(2)])
            qdT = sb(es, "g_qdT", [128, 8, TG], BF16)
            kiT = sb(es, "g_kiT", [128, 8, TG], BF16)
            keT = sb(es, "g_keT", [128, 8, TG], BF16)
            qdb = [Buf() for _ in range(8)]
            kib = [Buf() for _ in range(8)]
            keb = [Buf() for _ in range(8)]
            ketok = sb(es, "g_ketok", [128, 4, 1024], BF16)
            ketokb = [Buf() for _ in range(4)]
            gv = sb(es, "g_gv", [128, 4, 1024], BF16)
            gvb = Buf()
            gr = sb(es, "g_gr", [128, 4, 1024], F32)
            grb = Buf()
            Sf = sb(es, "g_Sf", [128, 2, 4, 256], F32)
            Sb_ = sb(es, "g_Sb", [128, 2, 4, 256], BF16)
            Sfb, Sbb = Buf(), Buf()
            MSET("dve", Sf[:], 0.0, [Sfb])
            MSET("pool", Sb_[:], 0.0, [Sbb])
            att_r = Ring([sb(es, "g_att%d" % i, [128, 512], BF16) for i in range(2)])
            junk = sb(es, "g_junk", [128, 1024], F32)
            junkb = Buf()
            ytmp = sb(es, "g_ytmp", [128, 1024], F32)
            ytmpb = Buf()
            ybf_r = Ring([sb(es, "g_ybf%d" % i, [128, 1024], BF16) for i in range(2)])
            yTg = sb(es, "g_yTg", [128, 8, TG], BF16)
            yTgb = Buf()
            sm_r = Ring([sb(es, "g_sm%d" % i, [128, 12], F32) for i in range(4)])
            tris = cst["c_tris"]
            for g in range(T // TG):
                t0 = g * TG
                S.dma(aTa[0:16, :], s_aT[:, t0:t0 + TG], w=[aTab])
                S.dma(gv[:], s_gv[t0:t0 + TG, :].rearrange("(n p) c -> p n c", p=128), w=[gvb])
                S.dma(gr[:], s_gr[t0:t0 + TG, :].rearrange("(n p) c -> p n c", p=128), w=[grb])
                for tt in range(4):
                    for hf in range(2):
                        ps, pb = psum((2, 3))
                        MM(ps[:], aTa[:, tt * 128:(tt + 1) * 128], wa2[:, hf * 512:(hf + 1) * 512], True, True, [aTab, wa2b], [pb])
                        e_, eb = e_r.next()
                        ACT(e_[:], ps[:], AF.Exp, [pb], [eb], scale=-1.0)
                        ACT(lt[:, tt, hf * 512:(hf + 1) * 512], e_[:], AF.Ln, [eb], [ltb[tt]], bias=1.0)
                for fc in range(8):
                    ps, pb = psum((2, 3))
                    for tt in range(4):
                        MM(ps[:, tt * 128:(tt + 1) * 128], lt[:, tt, fc * 128:(fc + 1) * 128], tris[:], True, True, [ltb[tt], cstb], [pb])
                    ACT(Eq[:, fc, :], ps[:], AF.Exp, [pb], [Eqb[fc]])
                    Ei, Eib = Ei_r.next()
                    ACT(Ei[:], ps[:], AF.Exp, [pb], [Eib], scale=-1.0)
                    qt, qtb = q_r.next()
                    kt, ktb = k_r.next()
                    S.dma(qt[:], s_qT[fc * 128:(fc + 1) * 128, t0:t0 + TG], w=[qtb])
                    S.dma(kt[:], s_kT[fc * 128:(fc + 1) * 128, t0:t0 + TG], w=[ktb])
                    STT("dve", qdT[:, fc, :], qt[:], 0.0625, Eq[:, fc, :], ALU.mult, ALU.mult, [qtb, Eqb[fc]], [qdb[fc]])
                    TT("pool", kiT[:, fc, :], kt[:], Ei[:], ALU.mult, [ktb, Eib], [kib[fc]])
                    for c8 in range(8):
                        cs = slice(c8 * 64, (c8 + 1) * 64)
                        STT("dve", keT[:, fc, cs], kt[:, cs], Eq[:, fc, c8 * 64 + 63:c8 * 64 + 64], Ei[:, cs],
                            ALU.mult, ALU.mult, [ktb, Eqb[fc], Eib], [keb[fc]])
                for tt in range(4):
                    ps, pb = psum((2, 3))
                    psv = ps[:].bitcast(BF16)
                    for fc in range(8):
                        TR(psv[:, fc * 128:(fc + 1) * 128], keT[:, fc, tt * 128:(tt + 1) * 128], identb[:], [keb[fc], cstb], [pb])
                    CP("act", ketok[:, tt, :], psv[:, :1024], [pb], [ketokb[tt]])
                for tt in range(4):
                    ts_ = slice(tt * 128, (tt + 1) * 128)
                    ps, pb = psum((2, 3))
                    for h in range(4):
                        for c in range(2):
                            MM(ps[:, h * 128:(h + 1) * 128], kiT[:, 2 * h + c, ts_], qdT[:, 2 * h + c, ts_], c == 0, c == 1,
                               [kib[2 * h + c], qdb[2 * h + c]], [pb])
                    att, attb = att_r.next()
                    TT("dve", att[:], ps[:], am4[:], ALU.mult, [pb, am4b], [attb])
                    po = [(PS[0], PSB[0]), (PS[1], PSB[1])]
                    for ch in range(2):
                        c8 = tt * 2 + ch
                        r0 = ch * 64
                        rs = slice(r0, r0 + 64)
                        cs = slice(tt * 128 + r0, tt * 128 + r0 + 64)
                        for h in range(4):
                            pso, pob = po[h // 2]
                            oc = slice((h % 2) * 256, (h % 2) * 256 + 256)
                            MM(pso[rs, oc], qdT[:, 2 * h, cs], Sb_[:, 0, h, :], True, False, [qdb[2 * h], Sbb], [pob])
                            MM(pso[rs, oc], qdT[:, 2 * h + 1, cs], Sb_[:, 1, h, :], False, False, [qdb[2 * h + 1], Sbb], [pob])
                            MM(pso[rs, oc], att[rs, h * 128 + r0:h * 128 + r0 + 64], gv[rs, tt, h * 256:(h + 1) * 256], False, True,
                               [attb, gvb], [pob])
                        pu = [(PS[4 + h_], PSB[4 + h_]) for h_ in range(4)]
                        for h in range(4):
                            for c in range(2):
                                psu, pub = pu[h]
                                MM(psu[:, c * 256:(c + 1) * 256], ketok[rs, tt, (2 * h + c) * 128:(2 * h + c + 1) * 128],
                                   gv[rs, tt, h * 256:(h + 1) * 256], True, True, [ketokb[tt], gvb], [pub])
                        for h in range(4):
                            for c in range(2):
                                psu, pub = pu[h]
                                STT("dve", Sf[:, c, h, :], Sf[:, c, h, :], Eq[:, 2 * h + c, c8 * 64 + 63:c8 * 64 + 64],
                                    psu[:, c * 256:(c + 1) * 256], ALU.mult, ALU.add, [Sfb, Eqb[2 * h + c], pub], [Sfb])
                        CP("act", Sb_[:], Sf[:], [Sfb], [Sbb])
                    sm, smb = sm_r.next()
                    for hh in range(2):
                        pso, pob = po[hh]
                        ACT(junk[:, hh * 512:(hh + 1) * 512], pso[:], AF.Square, [pob], [junkb])
                    RSUM("dve", sm[:, 0:4], junk[:].rearrange("p (h d) -> p h d", d=256), [junkb], [smb])
                    ACT(sm[:, 4:8], sm[:, 0:4], AF.Sqrt, [smb], [smb], scale=1.0 / 256, bias=EPS)
                    RECIP(sm[:, 8:12], sm[:, 4:8], [smb], [smb])
                    for h in range(4):
                        pso, pob = po[h // 2]
                        oc = slice((h % 2) * 256, (h % 2) * 256 + 256)
                        STT("dve", ytmp[:, h * 256:(h + 1) * 256], pso[:, oc], sm[:, 8 + h:9 + h], gng[:], ALU.mult, ALU.mult,
                            [pob, smb, gngb], [ytmpb])
                    yb_, ybb = ybf_r.next()
                    TT("pool", yb_[:], ytmp[:], gr[:, tt, :], ALU.mult, [ytmpb, grb], [ybb])
                    transpose_rows(yb_, ybb, yTg, yTgb, tt * 128, nk=8, pool=(2, 3))
                S.dma(s_yT[0].rearrange("(k p) t -> p k t", p=128)[:, :, t0:t0 + TG], yTg[:], r=[yTgb])
            S.barrier()
            S.flush()

    def phase_ssd(l):
        TG = 512
        with ExitStack() as es:
            cw = sb(es, "s_cw", [128, 16, 4], F32)
            cbias = sb(es, "s_cb", [128, 16], F32)
            prm = sb(es, "s_prm", [128, 4, 16], F32)
            gn = sb(es, "s_gn", [128, 1024], F32)
            pb_ = Buf()
            for k in range(4):
                S.dma(cw[:, :, k], Wd["ssm_conv_w"][l, k].rearrange("(c p) -> p c", p=128), w=[pb_], slow=True)
            S.dma(cbias[:], Wd["ssm_conv_b"][l].rearrange("(c p) -> p c", p=128), w=[pb_], slow=True)
            S.dma(prm[:, 0, :], Wd["ssm_dt_bias"][l:l + 1, :].partition_broadcast(128), w=[pb_])
            S.dma(prm[:, 1, :], Wd["ssm_a_log"][l:l + 1, :].partition_broadcast(128), w=[pb_])
            S.dma(prm[:, 2, :], Wd["ssm_d"][l:l + 1, :].partition_broadcast(128), w=[pb_])
            S.dma(gn[:], Wd["ssm_norm"][l:l + 1, :].partition_broadcast(128), w=[pb_])
            ACT(prm[:, 3, :], prm[:, 1, :], AF.Exp, [pb_], [pb_])
            TSC("dve", prm[:, 1, :], prm[:, 3, :], -1.0, None, ALU.mult, None, [pb_], [pb_])
            xin_r = Ring([sb(es, "s_xin%d" % i, [128, TG + 3], F32) for i in range(2)])
            acc_r = Ring([sb(es, "s_acc%d" % i, [128, TG], F32) for i in range(2)])
            xsT = sb(es, "s_xsT", [128, 8, TG], F32)
            xsTb = [Buf() for _ in range(8)]
            BTf = sb(es, "s_BTf", [128, 4, TG], F32)
            BTfb = [Buf() for _ in range(4)]
            BT = sb(es, "s_BT", [128, 4, TG], BF16)
            CT = sb(es, "s_CT", [128, 4, TG], BF16)
            BTb = [Buf() for _ in range(4)]
            CTb = [Buf() for _ in range(4)]
            xs_r = Ring([sb(es, "s_xs%d" % i, [128, 1024], F32) for i in range(2)])
            bt_r = Ring([sb(es, "s_bt%d" % i, [128, 512], BF16) for i in range(2)])
            sz_r = Ring([sb(es, "s_sz%d" % i, [128, 1024], F32) for i in range(2)])
            dt_r = Ring([sb(es, "s_dt%d" % i, [128, 8, 16], F32) for i in range(2)])
            ecl_r = Ring([sb(es, "s_ecl%d" % i, [128, 32], F32) for i in range(2)])
            xdt_r = Ring([sb(es, "s_xdt%d" % i, [128, 1024], BF16) for i in range(2)])
            xde_r = Ring([sb(es, "s_xde%d" % i, [128, 1024], BF16) for i in range(2)])
            cbm_r = Ring([sb(es, "s_cbm%d" % i, [128, 128], F32) for i in range(2)])
            dg_r = Ring([sb(es, "s_dg%d" % i, [128, 512], F32) for i in range(2)])
            tt_r = Ring([sb(es, "s_t%d" % i, [128, 512], F32) for i in range(2)])
            w_r = Ring([sb(es, "s_w%d" % i, [128, 512], BF16) for i in range(2)])
            STf = sb(es, "s_STf", [128, 4, 256], F32)
            STb = sb(es, "s_STb", [128, 4, 256], BF16)
            STfb, STbb = Buf(), Buf()
            MSET("dve", STf[:], 0.0, [STfb])
            MSET("pool", STb[:], 0.0, [STbb])
            yi = sb(es, "s_yi", [128, 1024], F32)
            yib = Buf()
            y2 = sb(es, "s_y2", [128, 1024], F32)
            y2b = Buf()
            junk = sb(es, "s_junk", [128, 1024], F32)
            junkb = Buf()
            ybf_r = Ring([sb(es, "s_ybf%d" % i, [128, 1024], BF16) for i in range(2)])
            yTg = sb(es, "s_yTg", [128, 8, TG], BF16)
            yTgb = Buf()
            sm_r = Ring([sb(es, "s_sm%d" % i, [128, 12], F32) for i in range(4)])
            tri1, blk, chsel, mneg4, onesf = cst["c_tri1"], cst["c_blk"], cst["c_chsel"], cst["c_mneg4"], cst["c_onesf"]
            amask = cst["c_amask"]
            for g in range(T // TG):
                t0 = g * TG
                for c in range(16):
                    xin, xinb = xin_r.next()
                    if g == 0:
                        MSET("pool", xin[:, 0:3], 0.0, [xinb])
                        S.dma(xin[:, 3:], s_sxT[c * 128:(c + 1) * 128, 0:TG], w=[xinb])
                    else:
                        S.dma(xin[:], s_sxT[c * 128:(c + 1) * 128, t0 - 3:t0 + TG], w=[xinb])
                    acc, accb = acc_r.next()
                    TSC("dve", acc[:], xin[:, 3:3 + TG], cw[:, c, 3:4], None, ALU.mult, None, [xinb, pb_], [accb])
                    for k in range(3):
                        STT("dve", acc[:], xin[:, k:k + TG], cw[:, c, k:k + 1], acc[:], ALU.mult, ALU.add, [xinb, pb_, accb], [accb])
                    if c < 8:
                        ACT(xsT[:, c, :], acc[:], AF.Silu, [accb, pb_], [xsTb[c]], bias=cbias[:, c:c + 1])
                    elif c < 12:
                        ACT(BTf[:, c - 8, :], acc[:], AF.Silu, [accb, pb_], [BTfb[c - 8]], bias=cbias[:, c:c + 1])
                        CP("pool", BT[:, c - 8, :], BTf[:, c - 8, :], [BTfb[c - 8]], [BTb[c - 8]])
                    else:
                        ACT(CT[:, c - 12, :], acc[:], AF.Silu, [accb, pb_], [CTb[c - 12]], bias=cbias[:, c:c + 1])
                for tt in range(4):
                    tsl = slice(tt * 128, (tt + 1) * 128)
                    tok0 = t0 + tt * 128
                    xs, xsb = xs_r.next()
                    for half in range(2):
                        ps, pb = psum((1,))
                        for k in range(4):
                            c = half * 4 + k
                            TR(ps[:, k * 128:(k + 1) * 128], xsT[:, c, tsl], identf[:], [xsTb[c], cstb], [pb])
                        CP("act", xs[:, half * 512:(half + 1) * 512], ps[:], [pb], [xsb])
                    btok, btokb = bt_r.next()
                    ps, pb = psum((1,))
                    for k in range(4):
                        TR(ps[:, k * 128:(k + 1) * 128], BTf[:, k, tsl], identf[:], [BTfb[k], cstb], [pb])
                    CP("act", btok[:], ps[:], [pb], [btokb])
                    sz, szb = sz_r.next()
                    S.dma(sz[:], s_sz[tok0:tok0 + 128, :], w=[szb])
                    dtt, dtb = dt_r.next()
                    S.dma(dtt[:, 0, :], s_sdt[tok0:tok0 + 128, :], w=[dtb])
                    TT("dve", dtt[:, 6, :], dtt[:, 0, :], prm[:, 0, :], ALU.add, [dtb, pb_], [dtb])
                    ACT(dtt[:, 6, :], dtt[:, 6, :], AF.Exp, [dtb], [dtb])
                    ACT(dtt[:, 1, :], dtt[:, 6, :], AF.Ln, [dtb], [dtb], bias=1.0)
                    TT("dve", dtt[:, 2, :], dtt[:, 1, :], prm[:, 1, :], ALU.mult, [dtb, pb_], [dtb])
                    ps, pb = psum((0,))
                    MM(ps[:, 0:16], tri1[:], dtt[:, 2, :], True, True, [cstb, dtb], [pb])
                    MM(ps[:, 16:32], blk[:], dtt[:, 2, :], True, True, [cstb, dtb], [pb])
                    MM(ps[:, 32:48], chsel[:, 0:128], dtt[:, 2, :], True, True, [cstb, dtb], [pb])
                    MM(ps[:, 48:64], chsel[:, 128:256], dtt[:, 2, :], True, True, [cstb, dtb], [pb])
                    CP("dve", dtt[:, 3, :], ps[:, 0:16], [pb], [dtb])
                    ACT(dtt[:, 4, :], ps[:, 0:16], AF.Exp, [pb], [dtb])
                    TT("dve", dtt[:, 6, :], ps[:, 16:32], dtt[:, 3, :], ALU.subtract, [pb, dtb], [dtb])
                    ACT(dtt[:, 5, :], dtt[:, 6, :], AF.Exp, [dtb], [dtb])
                    ecl, eclb = ecl_r.next()
                    ACT(ecl[:], ps[:, 32:64], AF.Exp, [pb], [eclb])
                    xdt, xdtb = xdt_r.next()
                    xde, xdeb = xde_r.next()
                    for h in range(16):
                        hs = slice(h * 64, (h + 1) * 64)
                        TSC("dve" if h % 2 else "pool", xdt[:, hs], xs[:, hs], dtt[:, 1, h:h + 1], None, ALU.mult, None, [xsb, dtb], [xdtb])
                        TSC("pool" if h % 2 else "dve", xde[:, hs], xs[:, hs], dtt[:, 1, h:h + 1], dtt[:, 5, h:h + 1], ALU.mult, ALU.mult,
                            [xsb, dtb], [xdeb])
                    psY = [(PS[3], PSB[3]), (PS[4], PSB[4])]
                    psI = [(PS[5], PSB[5]), (PS[6], PSB[6])]
                    for gq in range(4):
                        ps, pb = psum((1,))
                        MM(ps[:, 0:128], BT[:, gq, tsl], CT[:, gq, tsl], True, True, [BTb[gq], CTb[gq]], [pb])
                        cbm, cbmb = cbm_r.next()
                        TT("dve", cbm[:], ps[:, 0:128], amask[:], ALU.mult, [pb, cstb], [cbmb])
                        dg, dgb = dg_r.next()
                        for hq in range(4):
                            TSC("pool", dg[:, hq * 128:(hq + 1) * 128], identf[:], dtt[:, 3, gq * 4 + hq:gq * 4 + hq + 1], None, ALU.mult, None,
                                [cstb, dtb], [dgb])
                        ps2, pb2 = psum((2,))
                        MM(ps2[:], onesf[:], dg[:], True, False, [cstb, dgb], [pb2])
                        MM(ps2[:], identf[:], mneg4[:], False, True, [cstb], [pb2])
                        tq, tqb = tt_r.next()
                        for hq in range(4):
                            TSC("dve", tq[:, hq * 128:(hq + 1) * 128], ps2[:, hq * 128:(hq + 1) * 128], dtt[:, 3, gq * 4 + hq:gq * 4 + hq + 1],
                                None, ALU.subtract, None, [pb2, dtb], [tqb])
                        ACT(tq[:], tq[:], AF.Exp, [tqb], [tqb])
                        wq, wqb = w_r.next()
                        for hq in range(4):
                            TT("pool", wq[:, hq * 128:(hq + 1) * 128], tq[:, hq * 128:(hq + 1) * 128], cbm[:], ALU.mult, [tqb, cbmb], [wqb])
                        py, pyb = psY[gq // 2]
                        for hq in range(4):
                            h = gq * 4 + hq
                            oc = slice((gq % 2) * 256 + hq * 64, (gq % 2) * 256 + hq * 64 + 64)
                            MM(py[:, oc], wq[:, hq * 128:(hq + 1) * 128], xdt[:, h * 64:(h + 1) * 64], True, True, [wqb, xdtb], [pyb])
                    for ch in range(2):
                        r0 = ch * 64
                        rs = slice(r0, r0 + 64)
                        cs = slice(tt * 128 + r0, tt * 128 + r0 + 64)
                        for gq in range(4):
                            pi, pib = psI[gq // 2]
                            MM(pi[rs, (gq % 2) * 256:(gq % 2) * 256 + 256], CT[:, gq, cs], STb[:, gq, :], True, True, [CTb[gq], STbb], [pib])
                        for gq in range(4):
                            pS, pSb = psum((7, 0))
                            MM(pS[:, 0:256], btok[rs, gq * 128:(gq + 1) * 128], xde[rs, gq * 256:(gq + 1) * 256], True, True, [btokb, xdeb], [pSb])
                            for hq in range(4):
                                h = gq * 4 + hq
                                STT("dve", STf[:, gq, hq * 64:(hq + 1) * 64], STf[:, gq, hq * 64:(hq + 1) * 64], ecl[:, ch * 16 + h:ch * 16 + h + 1],
                                    pS[:, hq * 64:(hq + 1) * 64], ALU.mult, ALU.add, [STfb, eclb, pSb], [STfb])
                        CP("act", STb[:], STf[:], [STfb], [STbb])
                    for hh in range(2):
                        py, pyb = psY[hh]
                        CP("act", yi[:, hh * 512:(hh + 1) * 512], py[:], [pyb], [yib])
                    for h in range(16):
                        hs = slice(h * 64, (h + 1) * 64)
                        pi, pib = psI[h // 8]
                        STT("dve", y2[:, hs], pi[:, (h % 8) * 64:(h % 8) * 64 + 64], dtt[:, 4, h:h + 1], yi[:, hs], ALU.mult, ALU.add,
                            [pib, dtb, yib], [y2b])
                    for h in range(16):
                        hs = slice(h * 64, (h + 1) * 64)
                        STT("dve", y2[:, hs], xs[:, hs], prm[:, 2, h:h + 1], y2[:, hs], ALU.mult, ALU.add, [xsb, pb_, y2b], [y2b])
                    TT("pool", y2[:], y2[:], sz[:], ALU.mult, [y2b, szb], [y2b])
                    sm, smb = sm_r.next()
                    ACT(junk[:], y2[:], AF.Square, [y2b], [junkb])
                    RSUM("dve", sm[:, 0:4], junk[:].rearrange("p (h d) -> p h d", d=256), [junkb], [smb])
                    ACT(sm[:, 4:8], sm[:, 0:4], AF.Sqrt, [smb], [smb], scale=1.0 / 256, bias=EPS)
                    RECIP(sm[:, 8:12], sm[:, 4:8], [smb], [smb])
                    yb_, ybb = ybf_r.next()
                    for gq in range(4):
                        gs = slice(gq * 256, (gq + 1) * 256)
                        STT("dve", yb_[:, gs], y2[:, gs], sm[:, 8 + gq:9 + gq], gn[:, gs], ALU.mult, ALU.mult, [y2b, smb, pb_], [ybb])
                    transpose_rows(yb_, ybb, yTg, yTgb, tt * 128, nk=8, pool=(1,))
                S.dma(s_yT[2].rearrange("(k p) t -> p k t", p=128)[:, :, t0:t0 + TG], yTg[:], r=[yTgb])
            S.barrier()
            S.flush()

    s_ocmp = dscr("s_ocmp", (1024, T), F32)
    NQG = T // 512
    NCMP = T // 16 - 1
    NTC = (NCMP + 127) // 128
    CM_C = [31, -481, -993, -1505, -2017]

    def phase_nsa(l):
        with ExitStack() as es:
            kb = Buf()
            E = sb(es, "n_E", [128, T], BF16)
            S.dma(E[:], Cd["c_e"][:, :], w=[kb])
            wm = sb(es, "n_wm", [128, 8 * 512], BF16)
            S.dma(wm[:], Cd["c_wm"][:, :], w=[kb])
            cm = sb(es, "n_cm", [128, 5 * 512], BF16)
            S.dma(cm[:], Cd["c_cm"][:, :], w=[kb])
            kbsw = sb(es, "n_kbsw", [128, 16 * 68], F32)
            S.dma(kbsw[:], Cd["c_kbsw"][:, :], w=[kb])
            kbc = sb(es, "n_kbc", [128, 1024], F32)
            S.dma(kbc[:], Cd["c_kbc"][:, :], w=[kb])
            ov = sb(es, "n_ov", [128, 4, 129], BF16)
            S.dma(ov[:], Cd["c_ov"].rearrange("p (n c) -> p n c", c=129), w=[kb])
            wdt = sb(es, "n_wd", [128, 254], F32)
            S.dma(wdt[:], Cd["c_wd"][:, :], w=[kb])
            onesf = cst["c_onesf"]
            w1s = sb(es, "n_w1", [128, 2, 16, 64], BF16)
            w2s = sb(es, "n_w2", [64, 2, 64], BF16)
            posf = sb(es, "n_posf", [128, 2, 16], F32)
            posb = sb(es, "n_posb", [128, 2, 16], BF16)
            ccon = sb(es, "n_ccon", [64, 2], F32)
            for j in range(2):
                S.dma(w1s[:, j], Wd["nsa_cmp_w1"][l, j].rearrange("(c p) o -> p c o", p=128), w=[kb], q="pool")
                S.dma(w2s[:, j, :], Wd["nsa_cmp_w2"][l, j], w=[kb], q="pool")
                S.dma(posf[:, j, :], Wd["nsa_cmp_pos"][l, j].rearrange("(c two) d -> (two d) c", two=2), w=[kb], slow=True)
            CP("dve", posb[:], posf[:], [kb], [kb])
            for j in range(2):
                ps, pb = psum((7,))
                for c in range(16):
                    MM(ps[:64, 0:1], w1s[:, j, c, :], posb[:, j, c:c + 1], c == 0, c == 15, [kb], [pb])
                CP("dve", ccon[:, j:j + 1], ps[:64, 0:1], [pb], [kb])
            AT = sb(es, "n_AT", [64, 512], BF16)
            ATb = Buf()
            Kc = sb(es, "n_Kc", [67, 512], BF16)
            Vc = sb(es, "n_Vc", [128, 4, 65], BF16)
            Ks = sb(es, "n_Ks", [67, T], BF16)
            Kw = sb(es, "n_Kw", [67, T], BF16)
            Vs = sb(es, "n_Vs", [128, NT, 65], BF16)
            Vw = sb(es, "n_Vw", [128, NT, 65], BF16)
            kvb = Buf()
            selT = sb(es, "n_selT", [128, T], BF16)
            selTb = Buf()
            kcs2, kcs2b = selT, selTb
            impacc = sb(es, "n_imp", [128, 4, 128], F32)
            impb = [Buf() for _ in range(4)]
            q_r = Ring([sb(es, "n_q%d" % i, [67, 512], BF16) for i in range(6)])
            p_r = Ring([sb(es, "n_p%d" % i, [128, 512], BF16) for i in range(8)])
            tf_r = Ring([sb(es, "n_tf%d" % i, [128, 512], F32) for i in range(2)])
            m_r = Ring([sb(es, "n_m%d" % i, [128, 512], BF16) for i in range(3)])
            osb_r = Ring([sb(es, "n_osb%d" % i, [65, 512], F32) for i in range(3)])
            fr_r = Ring([sb(es, "n_fr%d" % i, [65, 512], F32) for i in range(3)])
            gt_r = Ring([sb(es, "n_gt%d" % i, [65, 3, 512], F32) for i in range(5)])
            accy_r = Ring([sb(es, "n_acc%d" % i, [64, 512], F32) for i in range(5)])
            ctr_r = Ring([sb(es, "n_ctr%d" % i, [64, 512], F32) for i in range(3)])
            ybf_r = Ring([sb(es, "n_yb%d" % i, [64, 512], BF16) for i in range(3)])
            sm_r = Ring([sb(es, "n_sm%d" % i, [128, 20], F32) for i in range(4)])
            ti_r = Ring([sb(es, "n_ti%d" % i, [128, 128], F32) for i in range(4)])
            sb_r = Ring([sb(es, "n_sb%d" % i, [128, 128], BF16) for i in range(2)])

            def load_q(h, qg):
                qt, qtb = q_r.next()
                S.dma(qt[0:64, :], s_nqT[h * 64:(h + 1) * 64, qg * 512:(qg + 1) * 512], w=[qtb])
                S.dma(qt[64:67, :], Cd["c_qa"][h, :, qg * 512:(qg + 1) * 512], w=[qtb])
                return qt, qtb

            def load_g(h, qg):
                gt, gtb = gt_r.next()
                for br in range(3):
                    S.dma(gt[64:65, br, :], s_ngT[br * 16 + h:br * 16 + h + 1, qg * 512:(qg + 1) * 512], w=[gtb])
                return gt, gtb

            pend = []
            LAG = 2

            def unit_back():
                Vap, pt, ptb, pso, psob, first, last = pend.pop(0)
                MM(pso[0:65, :], Vap, pt[:], first, last, [kvb, ptb], [psob])

            def flush_units():
                while pend:
                    unit_back()

            def tile_unit(qt, qtb, Kap, Vap, bias_ap, clamp, mask_ap, maskbufs, pso, psob, first, last, idx):
                ps, pb = psum((4, 5, 6))
                MM(ps[:], Kap, qt[:], True, True, [kvb, qtb], [pb])
                pt, ptb = p_r.next()
                if clamp:
                    tf, tfb = tf_r.next()
                    TSC("dve", tf[:], ps[:], bias_ap, 30.0, ALU.add, ALU.min, [pb, kb], [tfb])
                    ACT(pt[:], tf[:], AF.Exp, [tfb], [ptb])
                else:
                    ACT(pt[:], ps[:], AF.Exp, [pb, kb], [ptb], bias=bias_ap)
                if mask_ap is not None:
                    TT("pool" if idx % 2 else "dve", pt[:], pt[:], mask_ap, ALU.mult, [ptb] + maskbufs, [ptb])
                pend.append((Vap, pt, ptb, pso, psob, first, last))
                if len(pend) > LAG:
                    unit_back()
                return pt, ptb

            def finalize(pso, psob, gt, gtb, br):
                flush_units()
                osb, osbb = osb_r.next()
                CP("act", osb[:], pso[0:65, :], [psob], [osbb])
                fr, frb = fr_r.next()
                TSC("dve", fr[64:65, :], osb[64:65, :], 1e-30, None, ALU.max, None, [osbb], [frb])
                RECIP(fr[64:65, :], fr[64:65, :], [frb], [frb])
                TT("dve", fr[64:65, :], fr[64:65, :], gt[64:65, br, :], ALU.mult, [frb, gtb], [frb])
                psb_, psbb = psum((7,))
                MM(psb_[0:64, :], onesf[64:65, 0:64], fr[64:65, :], True, True, [cstb, frb], [psbb])
                ctr, ctrb = ctr_r.next()
                TT("dve", ctr[:], osb[0:64, :], psb_[0:64, :], ALU.mult, [osbb, psbb], [ctrb])
                return ctr, ctrb

            for gi in range(2):
                MSET("dve", Ks[:], 1.0, [kvb])
                MSET("pool", Kw[:], 1.0, [kvb])
                MSET("dve", Vs[:], 1.0, [kvb])
                MSET("pool", Vw[:], 1.0, [kvb])
                MSET("dve", Kc[:], 1.0, [kvb])
                MSET("pool", Vc[:], 1.0, [kvb])
                S.dma(Ks[0:64, :], s_nkvT[256 + gi * 64:256 + (gi + 1) * 64, :], w=[kvb])
                S.dma(Kw[0:64, :], s_nkvT[512 + gi * 64:512 + (gi + 1) * 64, :], w=[kvb])
                nkv_v = s_nkv.rearrange("(n p) c -> p n c", p=128)
                S.dma(Vs[:, :, 0:64], nkv_v[:, :, 384 + gi * 64:384 + (gi + 1) * 64], w=[kvb])
                S.dma(Vw[:, :, 0:64], nkv_v[:, :, 640 + gi * 64:640 + (gi + 1) * 64], w=[kvb])
                for j in range(2):
                    MSET("dve", kcs2[:], 0.0, [kcs2b])
                    S.dma(kcs2[0:64, :], s_nkvT[j * 128 + gi * 64:j * 128 + (gi + 1) * 64, :], w=[kcs2b])
                    S.dma(kcs2[64:128, 0:T - 1], s_nkvT[j * 128 + gi * 64:j * 128 + (gi + 1) * 64, 1:T], w=[kcs2b])
                    kv_ = kcs2[:].rearrange("p (n s) -> p n s", s=16)
                    ps, pb = psum((7,))
                    for c in range(16):
                        o_ = 2 * c
                        rhs = kv_[:, o_ // 16:o_ // 16 + NCMP, o_ % 16]
                        MM(ps[:64, 0:NCMP], w1s[:, j, c, :], rhs, c == 0, c == 15, [kb, kcs2b], [pb])
                    MSET("dve", AT[:], 0.0, [ATb])
                    ACT(AT[:, 0:NCMP], ps[:64, 0:NCMP], AF.Silu, [pb, kb], [ATb], bias=ccon[:, j:j + 1])
                    if j == 0:
                        ps2, pb2 = psum((7,))
                        MM(ps2[:64, 0:NCMP], w2s[:, 0, :], AT[:, 0:NCMP], True, True, [kb, ATb], [pb2])
                        CP("act", Kc[0:64, 0:NCMP], ps2[:64, 0:NCMP], [pb2], [kvb])
                    else:
                        for nt in range(NTC):
                            ps2, pb2 = psum((7,))
                            MM(ps2[:, 0:64], AT[:, nt * 128:(nt + 1) * 128], w2s[:, 1, :], True, True, [ATb, kb], [pb2])
                            CP("act", Vc[:, nt, 0:64], ps2[:, 0:64], [pb2], [kvb])
                for qg in range(NQG):
                    q0 = qg * 512
                    for j4 in range(4):
                        MSET("pool", impacc[:, j4, :], 0.0, [impb[j4]])
                    for hh in range(8):
                        h = gi * 8 + hh
                        qt, qtb = load_q(h, qg)
                        gt, gtb = load_g(h, qg)
                        nts = [nt for nt in range(NTC) if 16 * (128 * nt) + 31 <= q0 + 511]
                        pso, psob = psum((0, 1, 2, 3))
                        pts = []
                        for i_, nt in enumerate(nts):
                            full = 16 * (128 * nt + 127) + 31 <= q0
                            mask_ap = None
                            if not full:
                                cc = 2048 * nt + 31 - q0
                                mi = CM_C.index(cc)
                                mask_ap = cm[:, mi * 512:(mi + 1) * 512]
                            bias_ap = kbc[:, (h * 16 + qg) * 4 + nt:(h * 16 + qg) * 4 + nt + 1]
                            pt, ptb = tile_unit(qt, qtb, Kc[:, nt * 128:(nt + 1) * 128], Vc[:, nt, :], bias_ap, not full, mask_ap, [kb],
                                                pso, psob, i_ == 0, i_ == len(nts) - 1, i_)
                            pts.append((nt, pt, ptb))
                        ctr, ctrb = finalize(pso, psob, gt, gtb, 0)
                        S.dma(s_ocmp[h * 64:(h + 1) * 64, q0:q0 + 512], ctr[:], r=[ctrb])
                        for j4 in range(4):
                            psi, psib = psum((7,))
                            for i_, (nt, pt, ptb) in enumerate(pts):
                                MM(psi[:, 0:129], pt[:, j4 * 128:(j4 + 1) * 128], ov[:, nt, :], i_ == 0, i_ == len(pts) - 1, [ptb, kb], [psib])
                            sm, smb = sm_r.next()
                            TSC("dve", sm[:, 0:1], psi[:, 128:129], 1e-30, None, ALU.max, None, [psib], [smb])
                            RECIP(sm[:, 1:2], sm[:, 0:1], [smb], [smb])
                            STT("dve", impacc[:, j4, :], psi[:, 0:128], sm[:, 1:2], impacc[:, j4, :], ALU.mult, ALU.add,
                                [psib, smb, impb[j4]], [impb[j4]])
                    for j4 in range(4):
                        qt_ = qg * 4 + j4
                        ti, tib = ti_r.next()
                        TT("dve", ti[:], impacc[:, j4, :], wdt[:, 126 - 2 * qt_:254 - 2 * qt_], ALU.add, [impb[j4], kb], [tib])
                        MSET("dve", ti[:, 0:1], 4e30, [tib])
                        sm, smb = sm_r.next()
                        S.dve(lambda e, o=sm[:, 0:8], i=ti[:]: e.max(out=o, in_=i), [tib], [smb])
                        t2, t2b = ti_r.next()
                        S.dve(lambda e, o=t2[:], r_=sm[:, 0:8], i=ti[:]: e.match_replace(out=o, in_to_replace=r_, in_values=i, imm_value=-3e38),
                              [tib, smb], [t2b])
                        S.dve(lambda e, o=sm[:, 8:16], i=t2[:]: e.max(out=o, in_=i), [t2b], [smb])
                        sbt, sbtb = sb_r.next()
                        TSC("dve", sbt[:], ti[:], sm[:, 15:16], None, ALU.is_ge, None, [tib, smb], [sbtb])
                        ps, pb = psum((7,))
                        psv = ps[:].bitcast(BF16)
                        TR(psv[:, 0:128], sbt[:], identb[:], [sbtb, cstb], [pb])
                        CP("act", selT[:, qt_ * 128:(qt_ + 1) * 128], psv[:, 0:128], [pb], [selTb])
                if "s_selT" in dbg:
                    S.dma(s_selT[gi], selT[:], r=[selTb])
                for qg in range(NQG):
                    q0 = qg * 512
                    for hq in range(2):
                        heads = [gi * 8 + hq * 4 + i for i in range(4)]
                        qs = [load_q(h, qg) for h in heads]
                        gs = [load_g(h, qg) for h in heads]
                        accs = []
                        for h in heads:
                            ac, acb = accy_r.next()
                            S.dma(ac[:], s_ocmp[h * 64:(h + 1) * 64, q0:q0 + 512], w=[acb])
                            accs.append((ac, acb))
                        for br in (1, 2):
                            if br == 1:
                                kts = list(range(0, min(4 * qg + 4, NT)))
                            else:
                                kts = [kt for kt in range(4 * qg - 4, 4 * qg + 4) if 0 <= kt < NT]
                            psos = [(PS[i], PSB[i]) for i in range(4)]
                            for i_, kt in enumerate(kts):
                                m = kt - 4 * qg
                                if br == 1:
                                    psm, psmb = psum((7,))
                                    MM(psm[:], E[:, kt * 128:(kt + 1) * 128], selT[:, q0:q0 + 512], True, True, [kb, selTb], [psmb])
                                    mt, mtb = m_r.next()
                                    if m >= 0:
                                        TT("dve", mt[:], psm[:], wm[:, (m + 4) * 512:(m + 5) * 512], ALU.mult, [psmb, kb], [mtb])
                                    else:
                                        CP("act", mt[:], psm[:], [psmb], [mtb])
                                    mask_ap, mbufs = mt[:], [mtb]
                                    Kt, Vt = Ks, Vs
                                else:
                                    mask_ap, mbufs = wm[:, (m + 4) * 512:(m + 5) * 512], [kb]
                                    Kt, Vt = Kw, Vw
                                for i4, h in enumerate(heads):
                                    bias_ap = kbsw[:, h * 68 + m + 64:h * 68 + m + 65]
                                    tile_unit(qs[i4][0], qs[i4][1], Kt[:, kt * 128:(kt + 1) * 128], Vt[:, kt, :], bias_ap, m >= 0, mask_ap, mbufs,
                                              psos[i4][0], psos[i4][1], i_ == 0, i_ == len(kts) - 1, i4)
                            for i4, h in enumerate(heads):
                                ctr, ctrb = finalize(psos[i4][0], psos[i4][1], gs[i4][0], gs[i4][1], br)
                                ac, acb = accs[i4]
                                TT("pool", ac[:], ac[:], ctr[:], ALU.add, [acb, ctrb], [acb])
                        for i4, h in enumerate(heads):
                            ac, acb = accs[i4]
                            yb_, ybb = ybf_r.next()
                            CP("act", yb_[:], ac[:], [acb], [ybb])
                            S.dma(s_yT[1][h * 64:(h + 1) * 64, q0:q0 + 512], yb_[:], r=[ybb])
                S.barrier()
                S.flush()

    PHASES = {"cast": phase_cast, "p1": phase_p1, "p2": phase_p2, "gla": phase_gla, "ssd": phase_ssd, "nsa": phase_nsa}
    return nc, S, PHASES, es0


_CACHE = {}


def kernel(**inputs):
    x = np.ascontiguousarray(np.asarray(inputs["x"], np.float32))
    B, T, _ = x.shape
    NL = int(np.asarray(inputs["w_in"]).shape[0])
    consts = make_consts(T)
    nc, S, PH, es0 = build(T, NL, consts)
    PH["cast"]()
    for l in range(NL):
        PH["p1"](l)
        PH["gla"](l)
        PH["ssd"](l)
        PH["nsa"](l)
        PH["p2"](l)
    es0.close()
    in_maps = []
    for b in range(B):
        m = {"x": x[b]}
        for n, _s in W_SPECS:
            m[n] = np.ascontiguousarray(np.asarray(inputs[n], np.float32))
        m.update(consts)
        in_maps.append(m)
    res = run_bass_kernel_spmd(nc, in_maps, core_ids=list(range(B)))
    return np.stack([np.asarray(res.results[b]["out"], np.float32) for b in range(B)], axis=0)
```

```python
import numpy as np
import ml_dtypes
from contextlib import ExitStack
import concourse.bass as bass
import concourse.mybir as mybir
from concourse.bass_utils import run_bass_kernel_spmd

F32 = mybir.dt.float32
BF16 = mybir.dt.bfloat16
ALU = mybir.AluOpType
AF = mybir.ActivationFunctionType
AX = mybir.AxisListType

D = 2048
DIN = 15184
DFF = 5632
NCORES = 4
C_GQ, C_GK, C_GV, C_GR, C_GA = 0, 1024, 2048, 3072, 4096
C_NQ, C_NKV, C_NG = 4112, 5136, 5904
C_SZ, C_SX, C_SDT, C_MG = 5952, 6976, 9024, 9040
EPS = 1e-6


class Buf:
    __slots__ = ("w", "r")

    def __init__(self):
        self.w = None
        self.r = {}


class Sched:
    ENG = ("pe", "act", "dve", "pool", "sp")

    def __init__(self, nc, es, ndma=14):
        self.nc = nc
        self.q = {e: [] for e in self.ENG}
        self.cnt = {e: 0 for e in self.ENG[:4]}
        names = list(self.ENG[:4]) + ["d%d" % i for i in range(ndma)]
        self.sem = {n: es.enter_context(nc.semaphore("s_" + n)) for n in names}
        self.semval = {n: 0 for n in names}
        self.waited = {e: {} for e in self.ENG}
        self.ndma = ndma
        self.dma_i = 0
        self.n_ops = 0

    def _deps(self, eng, reads, writes):
        deps = {}
        for b in reads:
            if b.w is not None:
                s, v = b.w
                if deps.get(s, 0) < v:
                    deps[s] = v
        for b in writes:
            if b.w is not None:
                s, v = b.w
                if deps.get(s, 0) < v:
                    deps[s] = v
            for s, v in b.r.items():
                if deps.get(s, 0) < v:
                    deps[s] = v
        wd = self.waited[eng]
        q = self.q[eng]
        for s, v in deps.items():
            if eng == "pe" and s == "pe":
                continue
            if wd.get(s, 0) < v:
                wd[s] = v
                q.append((0, s, v))

    def _mark(self, tok, reads, writes):
        s, v = tok
        for b in reads:
            if b.r.get(s, 0) < v:
                b.r[s] = v
        for b in writes:
            b.w = tok
            b.r = {}

    def op(self, eng, fn, reads=(), writes=()):
        self._deps(eng, reads, writes)
        self.cnt[eng] += 1
        c = self.cnt[eng]
        self.semval[eng] = c
        self.q[eng].append((1, fn, eng))
        self._mark((eng, c), reads, writes)
        self.n_ops += 1

    def pe(self, fn, r=(), w=()):
        self.op("pe", fn, r, w)

    def act(self, fn, r=(), w=()):
        self.op("act", fn, r, w)

    def dve(self, fn, r=(), w=()):
        self.op("dve", fn, r, w)

    def pool(self, fn, r=(), w=()):
        self.op("pool", fn, r, w)

    def dma(self, out, in_, r=(), w=(), q="sp", slow=False):
        k = self.dma_i % self.ndma
        n = self.dma_i // self.ndma
        self.dma_i += 1
        s = "d%d" % k
        if n > 0:
            wd = self.waited[q]
            if wd.get(s, 0) < 16 * n:
                wd[s] = 16 * n
                self.q[q].append((0, s, 16 * n))
        self._deps(q, r, w)
        v = 16 * (n + 1)
        self.semval[s] = v
        self.q[q].append((2, out, in_, s, slow))
        self._mark((s, v), r, w)
        self.n_ops += 1

    def barrier(self):
        for e in self.ENG:
            wd = self.waited[e]
            q = self.q[e]
            for s, v in self.semval.items():
                if v > 0 and wd.get(s, 0) < v and not (e == "pe" and s == "pe"):
                    wd[s] = v
                    q.append((0, s, v))

    def flush(self):
        with self.nc.Block() as block:
            self.emit(block)
        for e in self.ENG:
            self.q[e] = []

    def emit(self, block):
        names = {"pe": "tensor", "act": "scalar", "dve": "vector", "pool": "gpsimd", "sp": "sync"}
        sem = self.sem
        for e, attr in names.items():
            items = self.q[e]

            def body(eng, items=items):
                for it in items:
                    if it[0] == 0:
                        eng.wait_ge(sem[it[1]], it[2])
                    elif it[0] == 1:
                        it[1](eng).then_inc(sem[it[2]], 1)
                    else:
                        if it[4]:
                            eng.dma_start(out=it[1], in_=it[2], allow_slow_non_contiguous=True).then_inc(sem[it[3]], 16)
                        else:
                            eng.dma_start(out=it[1], in_=it[2]).then_inc(sem[it[3]], 16)

            getattr(block, attr)(body)


class Ring:
    def __init__(self, tiles):
        self.tiles = tiles
        self.bufs = [Buf() for _ in tiles]
        self.i = 0

    def next(self):
        k = self.i % len(self.tiles)
        self.i += 1
        return self.tiles[k], self.bufs[k]


class Feeder:
    def __init__(self, S, ring, jobs):
        self.S, self.ring, self.jobs = S, ring, jobs
        self.R = len(ring.tiles)
        self.issued = 0
        self.got = 0
        self.done = 0
        self.slots = {}

    def _issue(self):
        i = self.issued
        view_fn, src = self.jobs[i]
        t, b = self.ring.next()
        v = view_fn(t)
        self.S.dma(v, src, w=[b])
        self.slots[i] = (v, b)
        self.issued += 1

    def pump(self):
        while self.issued < len(self.jobs) and self.issued < self.done + self.R:
            self._issue()

    def get(self):
        self.pump()
        assert self.got < self.issued, "feeder ring too small"
        r = self.slots.pop(self.got)
        self.got += 1
        return r

    def release(self, n=1):
        self.done += n
        self.pump()


def _bf(a):
    return np.asarray(a, np.float32).astype(ml_dtypes.bfloat16)


def make_consts(T):
    c = {}
    i = np.arange(128)
    same = (i[:, None] // 64) == (i[None, :] // 64)
    up = i[:, None] <= i[None, :]
    c["c_identb"] = _bf(np.eye(128))
    c["c_identf"] = np.eye(128, dtype=np.float32)
    c["c_tris"] = np.where(same & up, -1.0 / 16.0, 0.0).astype(np.float32)
    c["c_tri1"] = np.where(same & up, 1.0, 0.0).astype(np.float32)
    c["c_blk"] = np.where(same, 1.0, 0.0).astype(np.float32)
    ch = np.zeros((128, 256), np.float32)
    ch[:64, :128] = 1.0
    ch[64:, 128:] = 1.0
    c["c_chsel"] = ch
    c["c_amask"] = _bf(np.where(same & up, 1.0, 0.0))
    mneg = np.where(same & up, 0.0, -1e9).astype(np.float32)
    c["c_mneg4"] = np.tile(mneg, (1, 4))
    c["c_onesf"] = np.ones((128, 128), np.float32)
    slopes = (2.0 ** (-8.0 * np.arange(1, 17) / 16.0)).astype(np.float64)
    irel = np.arange(512, dtype=np.float64)
    qa = np.zeros((16, 3, 512), np.float32)
    for h in range(16):
        v = (-slopes[h] * irel).astype(np.float32)
        hi = v.astype(ml_dtypes.bfloat16).astype(np.float32)
        r1 = v - hi
        mid = r1.astype(ml_dtypes.bfloat16).astype(np.float32)
        lo = (r1 - mid).astype(ml_dtypes.bfloat16).astype(np.float32)
        qa[h, 0], qa[h, 1], qa[h, 2] = hi, mid, lo
    c["c_qa"] = _bf(np.tile(qa, (1, 1, T // 512)))
    j = np.arange(128, dtype=np.float64)
    dv = np.arange(-64, 4, dtype=np.float64)
    kb = slopes[None, :, None] * (128.0 * dv[None, None, :] + j[:, None, None])
    c["c_kbsw"] = kb.astype(np.float32).reshape(128, 16 * 68)
    qg = np.arange(16, dtype=np.float64)
    nt = np.arange(4, dtype=np.float64)
    kc = slopes[None, :, None, None] * (16.0 * (128.0 * nt[None, None, None, :] + j[:, None, None, None]) + 31.0
                                        - 512.0 * qg[None, None, :, None])
    c["c_kbc"] = kc.astype(np.float32).reshape(128, 16 * 16 * 4)
    ii = np.arange(512)
    wm = np.zeros((8, 128, 512), np.float32)
    for m in range(8):
        u = ii[None, :] - i[:, None] + 512 - 128 * m
        wm[m] = ((u >= 0) & (u < 512)).astype(np.float32)
    c["c_wm"] = _bf((wm.transpose(1, 0, 2).reshape(128, 8 * 512) - 1.0) * 30000.0)
    cm = np.zeros((5, 128, 512), np.float32)
    for k, cc in enumerate([31, -481, -993, -1505, -2017]):
        cm[k] = ((ii[None, :] - 16 * i[:, None]) >= cc).astype(np.float32)
    c["c_cm"] = _bf((cm.transpose(1, 0, 2).reshape(128, 5 * 512) - 1.0) * 30000.0)
    n_cmp = (8192 - 32) // 16 + 1
    n_sel = 128
    ov = np.zeros((512, 129), np.float32)
    tok = (np.arange(n_cmp) * 16)[:, None] + np.arange(32)[None, :]
    np.add.at(ov, (np.repeat(np.arange(n_cmp), 32), (tok // 64).ravel()), 1.0 / 32)
    ov[:n_cmp, 128] = 1.0
    c["c_ov"] = _bf(ov.reshape(4, 128, 129).transpose(1, 0, 2).reshape(128, 4 * 129))
    e = np.zeros((128, T), np.float32)
    e[np.arange(T) // 64, np.arange(T)] = 1.0
    c["c_e"] = _bf(e)
    wd = np.zeros((128, 254), np.float32)
    dd = np.arange(254) - 126
    cur = (i >= 64).astype(np.int64)
    for q in range(128):
        row = np.zeros(254, np.float32)
        fut = dd > cur[q]
        row[fut] = -1e30 - 1e28 * dd[fut]
        row[dd == cur[q]] = 2e30
        row[dd == cur[q] - 1] = 3e30
        wd[q] = row
    c["c_wd"] = wd
    c["c_ones_bf"] = _bf(np.ones((128, 512)))
    return c


W_SPECS = [
    ("w_in", (D, DIN)), ("gla_a2", (16, 1024)), ("gla_a_bias", (1024,)), ("gla_norm", (256,)),
    ("nsa_cmp_pos", (2, 32, 64)), ("nsa_cmp_w1", (2, 2048, 64)), ("nsa_cmp_w2", (2, 64, 64)),
    ("ssm_conv_w", (4, 2048)), ("ssm_conv_b", (2048,)), ("ssm_dt_bias", (16,)), ("ssm_a_log", (16,)),
    ("ssm_d", (16,)), ("ssm_norm", (1024,)), ("w_branch", (3, 1024, D)), ("w_out", (D, D)),
    ("norm_pre_mix", (D,)), ("norm_post_mix", (D,)), ("norm_pre_ffn", (D,)), ("norm_post_ffn", (D,)),
    ("w_ffn_gate", (D, DFF)), ("w_ffn_up", (D, DFF)), ("w_ffn_down", (DFF, D)),
]


def build(T, NL, consts, dbg=()):
    nc = bass.Bass("TRN2", target_bir_lowering=False)
    es0 = ExitStack()
    S = Sched(nc, es0)
    NT = T // 128

    def din(name, shape, dt=F32):
        return nc.dram_tensor(name, list(shape), dt, kind="ExternalInput").ap()

    def dscr(name, shape, dt):
        if name in dbg:
            return nc.dram_tensor(name, list(shape), dt, kind="ExternalOutput").ap()
        return nc.dram_tensor(name, list(shape), dt).ap()

    x_in = din("x", (T, D))
    Wd = {n: din(n, (NL,) + s) for n, s in W_SPECS}
    Cd = {n: din(n, a.shape, BF16 if a.dtype != np.float32 else F32) for n, a in consts.items()}
    out = nc.dram_tensor("out", [T, D], F32, kind="ExternalOutput").ap()

    wb_in = dscr("wb_in", (NL, D, DIN), BF16)
    wb_br = dscr("wb_br", (NL, 3072, D), BF16)
    wb_out = dscr("wb_out", (NL, D, D), BF16)
    wb_g = dscr("wb_g", (NL, D, DFF), BF16)
    wb_u = dscr("wb_u", (NL, D, DFF), BF16)
    wb_d = dscr("wb_d", (NL, DFF, D), BF16)
    s_qT = dscr("s_qT", (1024, T), F32)
    s_kT = dscr("s_kT", (1024, T), F32)
    s_gv = dscr("s_gv", (T, 1024), BF16)
    s_gr = dscr("s_gr", (T, 1024), F32)
    s_aT = dscr("s_aT", (16, T), F32)
    s_nqT = dscr("s_nqT", (1024, T), BF16)
    s_nkvT = dscr("s_nkvT", (768, T), BF16)
    s_nkv = dscr("s_nkv", (T, 768), BF16)
    s_ngT = dscr("s_ngT", (48, T), F32)
    s_sz = dscr("s_sz", (T, 1024), F32)
    s_sxT = dscr("s_sxT", (2048, T), F32)
    s_sdt = dscr("s_sdt", (T, 16), F32)
    s_mgT = dscr("s_mgT", (6144, T), BF16)
    s_yT = [dscr("s_yT%d" % i, (1024, T), BF16) for i in range(3)]
    s_selT = dscr("s_selT", (2, 128, T), BF16)

    PS = [es0.enter_context(nc.psum_tensor("ps%d" % i, [128, 512], F32)) for i in range(8)]
    PSB = [Buf() for _ in range(8)]
    ps_i = [0]

    def psum(pool=None):
        if pool is not None:
            k = pool[ps_i[0] % len(pool)]
        else:
            k = ps_i[0] % 8
        ps_i[0] += 1
        return PS[k], PSB[k]

    sb_n = [0]

    def sb(es, name, shape, dt):
        sb_n[0] += 1
        return es.enter_context(nc.sbuf_tensor("%s_%d" % (name, sb_n[0]), list(shape), dt))

    def ACT(out_, in_, func, r, w, **kw):
        S.act(lambda e: e.activation(out=out_, in_=in_, func=func, **kw), r, w)

    def TSC(eng, out_, in0, s1, s2, op0, op1, r, w):
        if op1 is None:
            S.op(eng, lambda e: e.tensor_scalar(out=out_, in0=in0, scalar1=s1, scalar2=None, op0=op0), r, w)
        else:
            S.op(eng, lambda e: e.tensor_scalar(out=out_, in0=in0, scalar1=s1, scalar2=s2, op0=op0, op1=op1), r, w)

    def STT(eng, out_, in0, sc, in1, op0, op1, r, w):
        S.op(eng, lambda e: e.scalar_tensor_tensor(out=out_, in0=in0, scalar=sc, in1=in1, op0=op0, op1=op1), r, w)

    def TT(eng, out_, in0, in1, op, r, w):
        S.op(eng, lambda e: e.tensor_tensor(out=out_, in0=in0, in1=in1, op=op), r, w)

    def CP(eng, out_, in_, r, w):
        if eng == "act":
            S.act(lambda e: e.activation(out=out_, in_=in_, func=AF.Copy), r, w)
        else:
            S.op(eng, lambda e: e.tensor_copy(out=out_, in_=in_), r, w)

    def MM(out_, lhsT, rhs, start, stop, r, w):
        S.pe(lambda e: e.matmul(out_, lhsT, rhs, start=start, stop=stop, skip_group_check=True), r, w)

    def TR(out_, in_, ident, r, w):
        S.pe(lambda e: e.transpose(out_, in_, ident), r, w)

    def RSUM(eng, out_, in_, r, w):
        S.op(eng, lambda e: e.reduce_sum(out=out_, in_=in_, axis=AX.X), r, w)

    def RECIP(out_, in_, r, w):
        S.dve(lambda e: e.reciprocal(out=out_, in_=in_), r, w)

    def MSET(eng, ap, val, w):
        S.op(eng, lambda e: e.memset(ap, val), (), w)

    cst = {}
    cstb = Buf()
    for n in ("c_identb", "c_identf", "c_tris", "c_tri1", "c_blk", "c_chsel", "c_amask", "c_mneg4", "c_onesf"):
        a = consts[n]
        t = sb(es0, "k" + n, a.shape, BF16 if a.dtype != np.float32 else F32)
        S.dma(t[:], Cd[n][:, :], w=[cstb])
        cst[n] = t
    identb, identf = cst["c_identb"], cst["c_identf"]

    def bview(dram2d):
        return dram2d.rearrange("(kc p) n -> p kc n", p=128)

    def phase_cast():
        with ExitStack() as es:
            ring = Ring([sb(es, "cst%d" % i, [128, 8192], BF16) for i in range(3)])
            jobs = []
            for l in range(NL):
                jobs += [(Wd["w_in"][l], wb_in[l], D, DIN),
                         (Wd["w_branch"][l].rearrange("b k n -> (b k) n"), wb_br[l], 3072, D),
                         (Wd["w_out"][l], wb_out[l], D, D),
                         (Wd["w_ffn_gate"][l], wb_g[l], D, DFF), (Wd["w_ffn_up"][l], wb_u[l], D, DFF),
                         (Wd["w_ffn_down"][l], wb_d[l], DFF, D)]
            for src, dst, K, N in jobs:
                for r0 in range(0, K, 128):
                    for c0 in range(0, N, 8192):
                        w_ = min(8192, N - c0)
                        t, b = ring.next()
                        S.dma(t[:, :w_], src[r0:r0 + 128, c0:c0 + w_], w=[b], q="pool")
                        S.dma(dst[r0:r0 + 128, c0:c0 + w_], t[:, :w_], r=[b], q="sp")
            S.barrier()
            S.flush()

    def rms_to_bf16(xt, xb, gain, gainb, hb, hbb, junk, junkb, sm, smb):
        ACT(junk[:], xt, AF.Square, [xb], [junkb])
        RSUM("dve", sm[:, 0:1], junk[:], [junkb], [smb])
        ACT(sm[:, 1:2], sm[:, 0:1], AF.Sqrt, [smb], [smb], scale=1.0 / D, bias=EPS)
        RECIP(sm[:, 2:3], sm[:, 1:2], [smb], [smb])
        STT("dve", hb, xt, sm[:, 2:3], gain[:], ALU.mult, ALU.mult, [xb, smb, gainb], [hbb])

    def transpose_rows(hb, hbb, dstT, dstb, col0, nk=16, evac="act", pool=None):
        for half in range(0, nk, 8):
            n = min(8, nk - half)
            ps, pb = psum(pool)
            psv = ps[:].bitcast(BF16)
            for k in range(n):
                TR(psv[:, k * 128:(k + 1) * 128], hb[:, (half + k) * 128:(half + k + 1) * 128], identb[:], [hbb, cstb], [pb])
            CP(evac, dstT[:, half:half + n, col0:col0 + 128],
               psv[:, :n * 128].rearrange("p (k t) -> p k t", t=128), [pb], [dstb])

    TS1 = min(1024, T)
    P1_JOBS = [
        (C_GQ, 1024, "F", s_qT, None), (C_GK, 1024, "F", s_kT, None),
        (C_GV, 1024, "T", s_gv, None), (C_GR, 1024, "T", s_gr, AF.Silu),
        (C_GA, 16, "F", s_aT, None),
        (C_NQ, 1024, "F", s_nqT, "scale8"),
        (C_NKV, 768, "F", s_nkvT, None), (C_NKV, 768, "T", s_nkv, None),
        (C_NG, 48, "F", s_ngT, AF.Sigmoid),
        (C_SZ, 1024, "T", s_sz, AF.Silu), (C_SX, 2048, "F", s_sxT, None), (C_SDT, 16, "T", s_sdt, None),
        (C_MG, 6144, "F", s_mgT, AF.Sigmoid),
    ]

    def phase_p1(l):
        xsrc = x_in if l == 0 else out
        with ExitStack() as es:
            gain = sb(es, "p1gain", [128, D], F32)
            gainb = Buf()
            S.dma(gain[:], Wd["norm_pre_mix"][l:l + 1, :].partition_broadcast(128), w=[gainb])
            xt_r = Ring([sb(es, "p1xt%d" % i, [128, D], F32) for i in range(2)])
            hb_r = Ring([sb(es, "p1hb%d" % i, [128, D], BF16) for i in range(2)])
            junk = sb(es, "p1junk", [128, D], F32)
            junkb = Buf()
            sm_r = Ring([sb(es, "p1sm%d" % i, [128, 4], F32) for i in range(4)])
            hT = sb(es, "p1hT", [128, 16, TS1], BF16)
            hTb = Buf()
            w_r = Ring([sb(es, "p1w%d" % i, [128, 16, 512], BF16) for i in range(3)])
            stf_r = Ring([sb(es, "p1sf%d" % i, [128, 512], F32) for i in range(3)])
            stb_r = Ring([sb(es, "p1sb%d" % i, [128, 512], BF16) for i in range(3)])
            wv = bview(wb_in[l])
            wjobs = []
            for _ts in range(T // TS1):
                for (c0, ncols, mode, dest, fn) in P1_JOBS:
                    for cb in range(0, ncols, 512):
                        nb = min(512, ncols - cb)
                        wjobs.append(((lambda t, nb=nb: t[:, :, :nb]), wv[:, :, c0 + cb:c0 + cb + nb]))
            feed = Feeder(S, w_r, wjobs)
            for ts in range(T // TS1):
                tb = ts * TS1
                for tt in range(TS1 // 128):
                    t0 = tb + tt * 128
                    xt, xb = xt_r.next()
                    S.dma(xt[:], xsrc[t0:t0 + 128, :], w=[xb])
                    hb, hbb = hb_r.next()
                    sm, smb = sm_r.next()
                    rms_to_bf16(xt[:], xb, gain, gainb, hb[:], hbb, junk, junkb, sm, smb)
                    transpose_rows(hb, hbb, hT, hTb, tt * 128)
                for (c0, ncols, mode, dest, fn) in P1_JOBS:
                    isbf = dest.dtype == BF16
                    for cb in range(0, ncols, 512):
                        nb = min(512, ncols - cb)
                        wt, wtb = feed.get()
                        if mode == "F":
                            for fs in range(0, nb, 128):
                                nf = min(128, nb - fs)
                                for tg in range(0, TS1, 512):
                                    ps, pb = psum()
                                    for kc in range(16):
                                        MM(ps[:nf, :], wt[:, kc, fs:fs + nf], hT[:, kc, tg:tg + 512], kc == 0, kc == 15,
                                           [wtb, hTb], [pb])
                                    st, stb = (stb_r if isbf else stf_r).next()
                                    if fn == "scale8":
                                        ACT(st[:nf, :], ps[:nf, :], AF.Copy, [pb], [stb], scale=0.125)
                                    elif fn is None:
                                        CP("dve", st[:nf, :], ps[:nf, :], [pb], [stb])
                                    else:
                                        ACT(st[:nf, :], ps[:nf, :], fn, [pb], [stb])
                                    f0 = cb + fs
                                    S.dma(dest[f0:f0 + nf, tb + tg:tb + tg + 512], st[:nf, :], r=[stb])
                        else:
                            for tt in range(TS1 // 128):
                                ps, pb = psum()
                                for kc in range(16):
                                    MM(ps[:, :nb], hT[:, kc, tt * 128:(tt + 1) * 128], wt[:, kc, :nb], kc == 0, kc == 15,
                                       [wtb, hTb], [pb])
                                st, stb = (stb_r if isbf else stf_r).next()
                                if fn is None:
                                    CP("dve", st[:, :nb], ps[:, :nb], [pb], [stb])
                                else:
                                    ACT(st[:, :nb], ps[:, :nb], fn, [pb], [stb])
                                t0 = tb + tt * 128
                                S.dma(dest[t0:t0 + 128, cb:cb + nb], st[:, :nb], r=[stb])
                        feed.release()
            S.barrier()
            S.flush()

    TS2 = min(512, T)
    NT2 = TS2 // 128

    def phase_p2(l):
        xsrc = x_in if l == 0 else out
        with ExitStack() as es:
            gA = sb(es, "p2gA", [128, D], F32)
            gB = sb(es, "p2gB", [128, D], F32)
            gAb, gBb = Buf(), Buf()
            big = sb(es, "p2big", [128, 48 * TS2], BF16)
            yT = [big[:, b * 8 * TS2:(b + 1) * 8 * TS2].rearrange("p (k t) -> p k t", t=TS2) for b in range(3)]
            mT = big[:, 32 * TS2:48 * TS2].rearrange("p (k t) -> p k t", t=TS2)
            zf = big[:, 0:32 * TS2].bitcast(F32).rearrange("p (n c) -> p n c", c=D)
            aT = big[:, 0:44 * TS2].rearrange("p (k t) -> p k t", t=TS2)
            yb = Buf()
            mb = [Buf() for _ in range(16)]
            zb = [Buf() for _ in range(NT2)]
            ab = [Buf() for _ in range(44)]
            xt = sb(es, "p2xt", [128, NT2, D], F32)
            xtb = Buf()
            h2T = sb(es, "p2h2T", [128, 16, TS2], BF16)
            h2Tb = Buf()
            w_r = Ring([sb(es, "p2w%d" % i, [128, 8192], BF16) for i in range(3)])
            g_r = Ring([sb(es, "p2g%d" % i, [128, 3, TS2], BF16) for i in range(2)])
            junk = sb(es, "p2junk", [128, D], F32)
            junkb = Buf()
            hb_r = Ring([sb(es, "p2hb%d" % i, [128, D], BF16) for i in range(2)])
            x1_r = Ring([sb(es, "p2x1%d" % i, [128, D], F32) for i in range(1)])
            sm_r = Ring([sb(es, "p2sm%d" % i, [128, 4], F32) for i in range(4)])
            tmp_r = Ring([sb(es, "p2tmp%d" % i, [128, TS2], F32) for i in range(3)])
            wbr = wb_br[l].rearrange("(b kc p) n -> p b kc n", p=128, kc=8)
            wov = bview(wb_out[l])
            wgv, wuv = bview(wb_g[l]), bview(wb_u[l])
            wdv = bview(wb_d[l])
            mgv = s_mgT.rearrange("(b f) t -> f b t", b=3)
            wjobs = []
            v8 = lambda t: t[:, :4096].rearrange("p (k n) -> p k n", n=512)
            v16 = lambda t: t[:].rearrange("p (k n) -> p k n", n=512)
            v11 = lambda t: t[:, :11 * 512].rearrange("p (k n) -> p k n", n=512)
            for _ts in range(T // TS2):
                for cb in range(4):
                    for b in range(3):
                        wjobs.append((v8, wbr[:, b, :, cb * 512:(cb + 1) * 512]))
                for cb in range(4):
                    wjobs.append((v16, wov[:, :, cb * 512:(cb + 1) * 512]))
                for fb in range(11):
                    wjobs.append((v16, wgv[:, :, fb * 512:(fb + 1) * 512]))
                    wjobs.append((v16, wuv[:, :, fb * 512:(fb + 1) * 512]))
                for cb in range(4):
                    for kq in range(4):
                        wjobs.append((v11, wdv[:, kq * 11:(kq + 1) * 11, cb * 512:(cb + 1) * 512]))
            feed = Feeder(S, w_r, wjobs)
            for ts in range(T // TS2):
                tb = ts * TS2
                S.dma(gA[:], Wd["norm_post_mix"][l:l + 1, :].partition_broadcast(128), w=[gAb])
                if ts == 0:
                    S.dma(gB[:], Wd["norm_pre_ffn"][l:l + 1, :].partition_broadcast(128), w=[gBb])
                for b in range(3):
                    S.dma(yT[b], s_yT[b].rearrange("(k p) t -> p k t", p=128)[:, :, tb:tb + TS2], w=[yb])
                for n in range(NT2):
                    S.dma(xt[:, n, :], xsrc[tb + n * 128:tb + (n + 1) * 128, :], w=[xtb])
                for cb in range(4):
                    wts = []
                    for b in range(3):
                        wts.append(feed.get())
                    for fs in range(4):
                        fc = cb * 4 + fs
                        gt, gtb = g_r.next()
                        S.dma(gt[:], mgv[fc * 128:(fc + 1) * 128, :, tb:tb + TS2], w=[gtb])
                        tmp, tmpb = tmp_r.next()
                        for b in range(3):
                            ps, pb = psum()
                            wtv, wtb = wts[b]
                            for kc in range(8):
                                MM(ps[:, :TS2], wtv[:, kc, fs * 128:(fs + 1) * 128], yT[b][:, kc, :], kc == 0, kc == 7,
                                   [wtb, yb], [pb])
                            if b == 0:
                                TT("dve", tmp[:], ps[:, :TS2], gt[:, 0, :], ALU.mult, [pb, gtb], [tmpb])
                            elif b == 1:
                                tmp2, tmp2b = tmp_r.next()
                                TT("dve", tmp2[:], ps[:, :TS2], gt[:, 1, :], ALU.mult, [pb, gtb], [tmp2b])
                                TT("pool", tmp[:], tmp[:], tmp2[:], ALU.add, [tmpb, tmp2b], [tmpb])
                            else:
                                tmp2, tmp2b = tmp_r.next()
                                TT("dve", tmp2[:], ps[:, :TS2], gt[:, 2, :], ALU.mult, [pb, gtb], [tmp2b])
                                TT("pool", mT[:, fc, :], tmp[:], tmp2[:], ALU.add, [tmpb, tmp2b], [mb[fc]])
                    feed.release(3)
                S.barrier()
                for cb in range(4):
                    wtv, wtb = feed.get()
                    for n in range(NT2):
                        ps, pb = psum()
                        for kc in range(16):
                            MM(ps[:], mT[:, kc, n * 128:(n + 1) * 128], wtv[:, kc, :], kc == 0, kc == 15, [wtb, mb[kc]], [pb])
                        CP("act", zf[:, n, cb * 512:(cb + 1) * 512], ps[:], [pb], [zb[n]])
                    feed.release()
                for n in range(NT2):
                    sm, smb = sm_r.next()
                    ACT(junk[:], zf[:, n, :], AF.Square, [zb[n]], [junkb])
                    RSUM("dve", sm[:, 0:1], junk[:], [junkb], [smb])
                    ACT(sm[:, 1:2], sm[:, 0:1], AF.Sqrt, [smb], [smb], scale=1.0 / D, bias=EPS)
                    RECIP(sm[:, 2:3], sm[:, 1:2], [smb], [smb])
                    STT("dve", junk[:], zf[:, n, :], sm[:, 2:3], gA[:], ALU.mult, ALU.mult, [zb[n], smb, gAb], [junkb])
                    TT("pool", xt[:, n, :], xt[:, n, :], junk[:], ALU.add, [xtb, junkb], [xtb])
                    S.dma(out[tb + n * 128:tb + (n + 1) * 128, :], xt[:, n, :], r=[xtb])
                    hb, hbb = hb_r.next()
                    sm, smb = sm_r.next()
                    rms_to_bf16(xt[:, n, :], xtb, gB, gBb, hb[:], hbb, junk, junkb, sm, smb)
                    transpose_rows(hb, hbb, h2T, h2Tb, n * 128)
                S.barrier()
                S.dma(gA[:], Wd["norm_post_ffn"][l:l + 1, :].partition_broadcast(128), w=[gAb])
                for fb in range(11):
                    wgv_, wgb = feed.get()
                    wuv_, wub = feed.get()
                    for fs in range(4):
                        psg, pgb = psum()
                        for kc in range(16):
                            MM(psg[:, :TS2], wgv_[:, kc, fs * 128:(fs + 1) * 128], h2T[:, kc, :], kc == 0, kc == 15, [wgb, h2Tb], [pgb])
                        psu, pub = psum()
                        for kc in range(16):
                            MM(psu[:, :TS2], wuv_[:, kc, fs * 128:(fs + 1) * 128], h2T[:, kc, :], kc == 0, kc == 15, [wub, h2Tb], [pub])
                        tmp, tmpb = tmp_r.next()
                        ACT(tmp[:], psg[:, :TS2], AF.Silu, [pgb], [tmpb])
                        TT("dve", aT[:, fb * 4 + fs, :], tmp[:], psu[:, :TS2], ALU.mult, [tmpb, pub], [ab[fb * 4 + fs]])
                    feed.release(2)
                for cb in range(4):
                    pss = [psum() for _ in range(NT2)]
                    for kq in range(4):
                        wtv, wtb = feed.get()
                        for n in range(NT2):
                            ps, pb = pss[n]
                            for k in range(11):
                                kc = kq * 11 + k
                                MM(ps[:], aT[:, kc, n * 128:(n + 1) * 128], wtv[:, k, :], kc == 0, kc == 43, [wtb, ab[kc]], [pb])
                        feed.release()
                    for n in range(NT2):
                        ps, pb = pss[n]
                        CP("act", xt[:, n, cb * 512:(cb + 1) * 512], ps[:], [pb], [xtb])
                for n in range(NT2):
                    x1, x1b = x1_r.next()
                    S.dma(x1[:], out[tb + n * 128:tb + (n + 1) * 128, :], w=[x1b])
                    sm, smb = sm_r.next()
                    ACT(junk[:], xt[:, n, :], AF.Square, [xtb], [junkb])
                    RSUM("dve", sm[:, 0:1], junk[:], [junkb], [smb])
                    ACT(sm[:, 1:2], sm[:, 0:1], AF.Sqrt, [smb], [smb], scale=1.0 / D, bias=EPS)
                    RECIP(sm[:, 2:3], sm[:, 1:2], [smb], [smb])
                    STT("dve", junk[:], xt[:, n, :], sm[:, 2:3], gA[:], ALU.mult, ALU.mult, [xtb, smb, gAb], [junkb])
                    TT("pool", x1[:], x1[:], junk[:], ALU.add, [x1b, junkb], [x1b])
                    S.dma(out[tb + n * 128:tb + (n + 1) * 128, :], x1[:], r=[x1b])
                S.barrier()
                S.flush()

    def phase_gla(l):
        TG = 512
        with ExitStack() as es:
            wa2 = sb(es, "g_wa2", [17, 1024], F32)
            wa2b = Buf()
            S.dma(wa2[0:16, :], Wd["gla_a2"][l], w=[wa2b])
            S.dma(wa2[16:17, :], Wd["gla_a_bias"][l:l + 1, :], w=[wa2b])
            gng = sb(es, "g_gng", [128, 256], F32)
            gngb = Buf()
            S.dma(gng[:], Wd["gla_norm"][l:l + 1, :].partition_broadcast(128), w=[gngb])
            am4 = sb(es, "g_am4", [128, 512], BF16)
            am4b = Buf()
            for h in range(4):
                S.dma(am4[:, h * 128:(h + 1) * 128], Cd["c_amask"][:, :], w=[am4b])
            aTa = sb(es, "g_aTa", [17, TG], F32)
            aTab = Buf()
            MSET("dve", aTa[:], 1.0, [aTab])
            lt = sb(es, "g_lt", [128, 4, 1024], F32)
            ltb = [Buf() for _ in range(4)]
            Eq = sb(es, "g_Eq", [128, 8, TG], F32)
            Eqb = [Buf() for _ in range(8)]
            Ei_r = Ring([sb(es, "g_Ei%d" % i, [128, TG], F32) for i in range(2)])
            q_r = Ring([sb(es, "g_q%d" % i, [128, TG], F32) for i in range(2)])
            k_r = Ring([sb(es, "g_k%d" % i, [128, TG], F32) for i in range(2)])
            e_r = Ring([sb(es, "g_e%d" % i, [128, 512], F32) for i in range(2)])
            qdT = sb(es, "g_qdT", [128, 8, TG], BF16)
            kiT = sb(es, "g_kiT", [128, 8, TG], BF16)
            keT = sb(es, "g_keT", [128, 8, TG], BF16)
            qdb = [Buf() for _ in range(8)]
            kib = [Buf() for _ in range(8)]
            keb = [Buf() for _ in range(8)]
            ketok = sb(es, "g_ketok", [128, 4, 1024], BF16)
            ketokb = [Buf() for _ in range(4)]
            gv = sb(es, "g_gv", [128, 4, 1024], BF16)
            gvb = Buf()
            gr = sb(es, "g_gr", [128, 4, 1024], F32)
            grb = Buf()
            Sf = sb(es, "g_Sf", [128, 2, 4, 256], F32)
            Sb_ = sb(es, "g_Sb", [128, 2, 4, 256], BF16)
            Sfb, Sbb = Buf(), Buf()
            MSET("dve", Sf[:], 0.0, [Sfb])
            MSET("pool", Sb_[:], 0.0, [Sbb])
            att_r = Ring([sb(es, "g_att%d" % i, [128, 512], BF16) for i in range(2)])
            junk = sb(es, "g_junk", [128, 1024], F32)
            junkb = Buf()
            ytmp = sb(es, "g_ytmp", [128, 1024], F32)
            ytmpb = Buf()
            ybf_r = Ring([sb(es, "g_ybf%d" % i, [128, 1024], BF16) for i in range(2)])
            yTg = sb(es, "g_yTg", [128, 8, TG], BF16)
            yTgb = Buf()
            sm_r = Ring([sb(es, "g_sm%d" % i, [128, 12], F32) for i in range(4)])
            tris = cst["c_tris"]
            for g in range(T // TG):
                t0 = g * TG
                S.dma(aTa[0:16, :], s_aT[:, t0:t0 + TG], w=[aTab])
                S.dma(gv[:], s_gv[t0:t0 + TG, :].rearrange("(n p) c -> p n c", p=128), w=[gvb])
                S.dma(gr[:], s_gr[t0:t0 + TG, :].rearrange("(n p) c -> p n c", p=128), w=[grb])
                for tt in range(4):
                    for hf in range(2):
                        ps, pb = psum((2, 3))
                        MM(ps[:], aTa[:, tt * 128:(tt + 1) * 128], wa2[:, hf * 512:(hf + 1) * 512], True, True, [aTab, wa2b], [pb])
                        e_, eb = e_r.next()
                        ACT(e_[:], ps[:], AF.Exp, [pb], [eb], scale=-1.0)
                        ACT(lt[:, tt, hf * 512:(hf + 1) * 512], e_[:], AF.Ln, [eb], [ltb[tt]], bias=1.0)
                for fc in range(8):
                    ps, pb = psum((2, 3))
                    for tt in range(4):
                        MM(ps[:, tt * 128:(tt + 1) * 128], lt[:, tt, fc * 128:(fc + 1) * 128], tris[:], True, True, [ltb[tt], cstb], [pb])
                    ACT(Eq[:, fc, :], ps[:], AF.Exp, [pb], [Eqb[fc]])
                    Ei, Eib = Ei_r.next()
                    ACT(Ei[:], ps[:], AF.Exp, [pb], [Eib], scale=-1.0)
                    qt, qtb = q_r.next()
                    kt, ktb = k_r.next()
                    S.dma(qt[:], s_qT[fc * 128:(fc + 1) * 128, t0:t0 + TG], w=[qtb])
                    S.dma(kt[:], s_kT[fc * 128:(fc + 1) * 128, t0:t0 + TG], w=[ktb])
                    STT("dve", qdT[:, fc, :], qt[:], 0.0625, Eq[:, fc, :], ALU.mult, ALU.mult, [qtb, Eqb[fc]], [qdb[fc]])
                    TT("pool", kiT[:, fc, :], kt[:], Ei[:], ALU.mult, [ktb, Eib], [kib[fc]])
                    for c8 in range(8):
                        cs = slice(c8 * 64, (c8 + 1) * 64)
                        STT("dve", keT[:, fc, cs], kt[:, cs], Eq[:, fc, c8 * 64 + 63:c8 * 64 + 64], Ei[:, cs],
                            ALU.mult, ALU.mult, [ktb, Eqb[fc], Eib], [keb[fc]])
                for tt in range(4):
                    ps, pb = psum((2, 3))
                    psv = ps[:].bitcast(BF16)
                    for fc in range(8):
                        TR(psv[:, fc * 128:(fc + 1) * 128], keT[:, fc, tt * 128:(tt + 1) * 128], identb[:], [keb[fc], cstb], [pb])
                    CP("act", ketok[:, tt, :], psv[:, :1024], [pb], [ketokb[tt]])
                for tt in range(4):
                    ts_ = slice(tt * 128, (tt + 1) * 128)
                    ps, pb = psum((2, 3))
                    for h in range(4):
                        for c in range(2):
                            MM(ps[:, h * 128:(h + 1) * 128], kiT[:, 2 * h + c, ts_], qdT[:, 2 * h + c, ts_], c == 0, c == 1,
                               [kib[2 * h + c], qdb[2 * h + c]], [pb])
                    att, attb = att_r.next()
                    TT("dve", att[:], ps[:], am4[:], ALU.mult, [pb, am4b], [attb])
                    po = [(PS[0], PSB[0]), (PS[1], PSB[1])]
                    for ch in range(2):
                        c8 = tt * 2 + ch
                        r0 = ch * 64
                        rs = slice(r0, r0 + 64)
                        cs = slice(tt * 128 + r0, tt * 128 + r0 + 64)
                        for h in range(4):
                            pso, pob = po[h // 2]
                            oc = slice((h % 2) * 256, (h % 2) * 256 + 256)
                            MM(pso[rs, oc], qdT[:, 2 * h, cs], Sb_[:, 0, h, :], True, False, [qdb[2 * h], Sbb], [pob])
                            MM(pso[rs, oc], qdT[:, 2 * h + 1, cs], Sb_[:, 1, h, :], False, False, [qdb[2 * h + 1], Sbb], [pob])
                            MM(pso[rs, oc], att[rs, h * 128 + r0:h * 128 + r0 + 64], gv[rs, tt, h * 256:(h + 1) * 256], False, True,
                               [attb, gvb], [pob])
                        pu = [(PS[4 + h_], PSB[4 + h_]) for h_ in range(4)]
                        for h in range(4):
                            for c in range(2):
                                psu, pub = pu[h]
                                MM(psu[:, c * 256:(c + 1) * 256], ketok[rs, tt, (2 * h + c) * 128:(2 * h + c + 1) * 128],
                                   gv[rs, tt, h * 256:(h + 1) * 256], True, True, [ketokb[tt], gvb], [pub])
                        for h in range(4):
                            for c in range(2):
                                psu, pub = pu[h]
                                STT("dve", Sf[:, c, h, :], Sf[:, c, h, :], Eq[:, 2 * h + c, c8 * 64 + 63:c8 * 64 + 64],
                                    psu[:, c * 256:(c + 1) * 256], ALU.mult, ALU.add, [Sfb, Eqb[2 * h + c], pub], [Sfb])
                        CP("act", Sb_[:], Sf[:], [Sfb], [Sbb])
                    sm, smb = sm_r.next()
                    for hh in range(2):
                        pso, pob = po[hh]
                        ACT(junk[:, hh * 512:(hh + 1) * 512], pso[:], AF.Square, [pob], [junkb])
                    RSUM("dve", sm[:, 0:4], junk[:].rearrange("p (h d) -> p h d", d=256), [junkb], [smb])
                    ACT(sm[:, 4:8], sm[:, 0:4], AF.Sqrt, [smb], [smb], scale=1.0 / 256, bias=EPS)
                    RECIP(sm[:, 8:12], sm[:, 4:8], [smb], [smb])
                    for h in range(4):
                        pso, pob = po[h // 2]
                        oc = slice((h % 2) * 256, (h % 2) * 256 + 256)
                        STT("dve", ytmp[:, h * 256:(h + 1) * 256], pso[:, oc], sm[:, 8 + h:9 + h], gng[:], ALU.mult, ALU.mult,
                            [pob, smb, gngb], [ytmpb])
                    yb_, ybb = ybf_r.next()
                    TT("pool", yb_[:], ytmp[:], gr[:, tt, :], ALU.mult, [ytmpb, grb], [ybb])
                    transpose_rows(yb_, ybb, yTg, yTgb, tt * 128, nk=8, pool=(2, 3))
                S.dma(s_yT[0].rearrange("(k p) t -> p k t", p=128)[:, :, t0:t0 + TG], yTg[:], r=[yTgb])
            S.barrier()
            S.flush()

    def phase_ssd(l):
        TG = 512
        with ExitStack() as es:
            cw = sb(es, "s_cw", [128, 16, 4], F32)
            cbias = sb(es, "s_cb", [128, 16], F32)
            prm = sb(es, "s_prm", [128, 4, 16], F32)
            gn = sb(es, "s_gn", [128, 1024], F32)
            pb_ = Buf()
            for k in range(4):
                S.dma(cw[:, :, k], Wd["ssm_conv_w"][l, k].rearrange("(c p) -> p c", p=128), w=[pb_], slow=True)
            S.dma(cbias[:], Wd["ssm_conv_b"][l].rearrange("(c p) -> p c", p=128), w=[pb_], slow=True)
            S.dma(prm[:, 0, :], Wd["ssm_dt_bias"][l:l + 1, :].partition_broadcast(128), w=[pb_])
            S.dma(prm[:, 1, :], Wd["ssm_a_log"][l:l + 1, :].partition_broadcast(128), w=[pb_])
            S.dma(prm[:, 2, :], Wd["ssm_d"][l:l + 1, :].partition_broadcast(128), w=[pb_])
            S.dma(gn[:], Wd["ssm_norm"][l:l + 1, :].partition_broadcast(128), w=[pb_])
            ACT(prm[:, 3, :], prm[:, 1, :], AF.Exp, [pb_], [pb_])
            TSC("dve", prm[:, 1, :], prm[:, 3, :], -1.0, None, ALU.mult, None, [pb_], [pb_])
            xin_r = Ring([sb(es, "s_xin%d" % i, [128, TG + 3], F32) for i in range(2)])
            acc_r = Ring([sb(es, "s_acc%d" % i, [128, TG], F32) for i in range(2)])
            xsT = sb(es, "s_xsT", [128, 8, TG], F32)
            xsTb = [Buf() for _ in range(8)]
            BTf = sb(es, "s_BTf", [128, 4, TG], F32)
            BTfb = [Buf() for _ in range(4)]
            BT = sb(es, "s_BT", [128, 4, TG], BF16)
            CT = sb(es, "s_CT", [128, 4, TG], BF16)
            BTb = [Buf() for _ in range(4)]
            CTb = [Buf() for _ in range(4)]
            xs_r = Ring([sb(es, "s_xs%d" % i, [128, 1024], F32) for i in range(2)])
            bt_r = Ring([sb(es, "s_bt%d" % i, [128, 512], BF16) for i in range(2)])
            sz_r = Ring([sb(es, "s_sz%d" % i, [128, 1024], F32) for i in range(2)])
            dt_r = Ring([sb(es, "s_dt%d" % i, [128, 8, 16], F32) for i in range(2)])
            ecl_r = Ring([sb(es, "s_ecl%d" % i, [128, 32], F32) for i in range(2)])
            xdt_r = Ring([sb(es, "s_xdt%d" % i, [128, 1024], BF16) for i in range(2)])
            xde_r = Ring([sb(es, "s_xde%d" % i, [128, 1024], BF16) for i in range(2)])
            cbm_r = Ring([sb(es, "s_cbm%d" % i, [128, 128], F32) for i in range(2)])
            dg_r = Ring([sb(es, "s_dg%d" % i, [128, 512], F32) for i in range(2)])
            tt_r = Ring([sb(es, "s_t%d" % i, [128, 512], F32) for i in range(2)])
            w_r = Ring([sb(es, "s_w%d" % i, [128, 512], BF16) for i in range(2)])
            STf = sb(es, "s_STf", [128, 4, 256], F32)
            STb = sb(es, "s_STb", [128, 4, 256], BF16)
            STfb, STbb = Buf(), Buf()
            MSET("dve", STf[:], 0.0, [STfb])
            MSET("pool", STb[:], 0.0, [STbb])
            yi = sb(es, "s_yi", [128, 1024], F32)
            yib = Buf()
            y2 = sb(es, "s_y2", [128, 1024], F32)
            y2b = Buf()
            junk = sb(es, "s_junk", [128, 1024], F32)
            junkb = Buf()
            ybf_r = Ring([sb(es, "s_ybf%d" % i, [128, 1024], BF16) for i in range(2)])
            yTg = sb(es, "s_yTg", [128, 8, TG], BF16)
            yTgb = Buf()
            sm_r = Ring([sb(es, "s_sm%d" % i, [128, 12], F32) for i in range(4)])
            tri1, blk, chsel, mneg4, onesf = cst["c_tri1"], cst["c_blk"], cst["c_chsel"], cst["c_mneg4"], cst["c_onesf"]
            amask = cst["c_amask"]
            for g in range(T // TG):
                t0 = g * TG
                for c in range(16):
                    xin, xinb = xin_r.next()
                    if g == 0:
                        MSET("pool", xin[:, 0:3], 0.0, [xinb])
                        S.dma(xin[:, 3:], s_sxT[c * 128:(c + 1) * 128, 0:TG], w=[xinb])
                    else:
                        S.dma(xin[:], s_sxT[c * 128:(c + 1) * 128, t0 - 3:t0 + TG], w=[xinb])
                    acc, accb = acc_r.next()
                    TSC("dve", acc[:], xin[:, 3:3 + TG], cw[:, c, 3:4], None, ALU.mult, None, [xinb, pb_], [accb])
                    for k in range(3):
                        STT("dve", acc[:], xin[:, k:k + TG], cw[:, c, k:k + 1], acc[:], ALU.mult, ALU.add, [xinb, pb_, accb], [accb])
                    if c < 8:
                        ACT(xsT[:, c, :], acc[:], AF.Silu, [accb, pb_], [xsTb[c]], bias=cbias[:, c:c + 1])
                    elif c < 12:
                        ACT(BTf[:, c - 8, :], acc[:], AF.Silu, [accb, pb_], [BTfb[c - 8]], bias=cbias[:, c:c + 1])
                        CP("pool", BT[:, c - 8, :], BTf[:, c - 8, :], [BTfb[c - 8]], [BTb[c - 8]])
                    else:
                        ACT(CT[:, c - 12, :], acc[:], AF.Silu, [accb, pb_], [CTb[c - 12]], bias=cbias[:, c:c + 1])
                for tt in range(4):
                    tsl = slice(tt * 128, (tt + 1) * 128)
                    tok0 = t0 + tt * 128
                    xs, xsb = xs_r.next()
                    for half in range(2):
                        ps, pb = psum((1,))
                        for k in range(4):
                            c = half * 4 + k
                            TR(ps[:, k * 128:(k + 1) * 128], xsT[:, c, tsl], identf[:], [xsTb[c], cstb], [pb])
                        CP("act", xs[:, half * 512:(half + 1) * 512], ps[:], [pb], [xsb])
                    btok, btokb = bt_r.next()
                    ps, pb = psum((1,))
                    for k in range(4):
                        TR(ps[:, k * 128:(k + 1) * 128], BTf[:, k, tsl], identf[:], [BTfb[k], cstb], [pb])
                    CP("act", btok[:], ps[:], [pb], [btokb])
                    sz, szb = sz_r.next()
                    S.dma(sz[:], s_sz[tok0:tok0 + 128, :], w=[szb])
                    dtt, dtb = dt_r.next()
                    S.dma(dtt[:, 0, :], s_sdt[tok0:tok0 + 128, :], w=[dtb])
                    TT("dve", dtt[:, 6, :], dtt[:, 0, :], prm[:, 0, :], ALU.add, [dtb, pb_], [dtb])
                    ACT(dtt[:, 6, :], dtt[:, 6, :], AF.Exp, [dtb], [dtb])
                    ACT(dtt[:, 1, :], dtt[:, 6, :], AF.Ln, [dtb], [dtb], bias=1.0)
                    TT("dve", dtt[:, 2, :], dtt[:, 1, :], prm[:, 1, :], ALU.mult, [dtb, pb_], [dtb])
                    ps, pb = psum((0,))
                    MM(ps[:, 0:16], tri1[:], dtt[:, 2, :], True, True, [cstb, dtb], [pb])
                    MM(ps[:, 16:32], blk[:], dtt[:, 2, :], True, True, [cstb, dtb], [pb])
                    MM(ps[:, 32:48], chsel[:, 0:128], dtt[:, 2, :], True, True, [cstb, dtb], [pb])
                    MM(ps[:, 48:64], chsel[:, 128:256], dtt[:, 2, :], True, True, [cstb, dtb], [pb])
                    CP("dve", dtt[:, 3, :], ps[:, 0:16], [pb], [dtb])
                    ACT(dtt[:, 4, :], ps[:, 0:16], AF.Exp, [pb], [dtb])
                    TT("dve", dtt[:, 6, :], ps[:, 16:32], dtt[:, 3, :], ALU.subtract, [pb, dtb], [dtb])
                    ACT(dtt[:, 5, :], dtt[:, 6, :], AF.Exp, [dtb], [dtb])
                    ecl, eclb = ecl_r.next()
                    ACT(ecl[:], ps[:, 32:64], AF.Exp, [pb], [eclb])
                    xdt, xdtb = xdt_r.next()
                    xde, xdeb = xde_r.next()
                    for h in range(16):
                        hs = slice(h * 64, (h + 1) * 64)
                        TSC("dve" if h % 2 else "pool", xdt[:, hs], xs[:, hs], dtt[:, 1, h:h + 1], None, ALU.mult, None, [xsb, dtb], [xdtb])
                        TSC("pool" if h % 2 else "dve", xde[:, hs], xs[:, hs], dtt[:, 1, h:h + 1], dtt[:, 5, h:h + 1], ALU.mult, ALU.mult,
                            [xsb, dtb], [xdeb])
                    psY = [(PS[3], PSB[3]), (PS[4], PSB[4])]
                    psI = [(PS[5], PSB[5]), (PS[6], PSB[6])]
                    for gq in range(4):
                        ps, pb = psum((1,))
                        MM(ps[:, 0:128], BT[:, gq, tsl], CT[:, gq, tsl], True, True, [BTb[gq], CTb[gq]], [pb])
                        cbm, cbmb = cbm_r.next()
                        TT("dve", cbm[:], ps[:, 0:128], amask[:], ALU.mult, [pb, cstb], [cbmb])
                        dg, dgb = dg_r.next()
                        for hq in range(4):
                            TSC("pool", dg[:, hq * 128:(hq + 1) * 128], identf[:], dtt[:, 3, gq * 4 + hq:gq * 4 + hq + 1], None, ALU.mult, None,
                                [cstb, dtb], [dgb])
                        ps2, pb2 = psum((2,))
                        MM(ps2[:], onesf[:], dg[:], True, False, [cstb, dgb], [pb2])
                        MM(ps2[:], identf[:], mneg4[:], False, True, [cstb], [pb2])
                        tq, tqb = tt_r.next()
                        for hq in range(4):
                            TSC("dve", tq[:, hq * 128:(hq + 1) * 128], ps2[:, hq * 128:(hq + 1) * 128], dtt[:, 3, gq * 4 + hq:gq * 4 + hq + 1],
                                None, ALU.subtract, None, [pb2, dtb], [tqb])
                        ACT(tq[:], tq[:], AF.Exp, [tqb], [tqb])
                        wq, wqb = w_r.next()
                        for hq in range(4):
                            TT("pool", wq[:, hq * 128:(hq + 1) * 128], tq[:, hq * 128:(hq + 1) * 128], cbm[:], ALU.mult, [tqb, cbmb], [wqb])
                        py, pyb = psY[gq // 2]
                        for hq in range(4):
                            h = gq * 4 + hq
                            oc = slice((gq % 2) * 256 + hq * 64, (gq % 2) * 256 + hq * 64 + 64)
                            MM(py[:, oc], wq[:, hq * 128:(hq + 1) * 128], xdt[:, h * 64:(h + 1) * 64], True, True, [wqb, xdtb], [pyb])
                    for ch in range(2):
                        r0 = ch * 64
                        rs = slice(r0, r0 + 64)
                        cs = slice(tt * 128 + r0, tt * 128 + r0 + 64)
                        for gq in range(4):
                            pi, pib = psI[gq // 2]
                            MM(pi[rs, (gq % 2) * 256:(gq % 2) * 256 + 256], CT[:, gq, cs], STb[:, gq, :], True, True, [CTb[gq], STbb], [pib])
                        for gq in range(4):
                            pS, pSb = psum((7, 0))
                            MM(pS[:, 0:256], btok[rs, gq * 128:(gq + 1) * 128], xde[rs, gq * 256:(gq + 1) * 256], True, True, [btokb, xdeb], [pSb])
                            for hq in range(4):
                                h = gq * 4 + hq
                                STT("dve", STf[:, gq, hq * 64:(hq + 1) * 64], STf[:, gq, hq * 64:(hq + 1) * 64], ecl[:, ch * 16 + h:ch * 16 + h + 1],
                                    pS[:, hq * 64:(hq + 1) * 64], ALU.mult, ALU.add, [STfb, eclb, pSb], [STfb])
                        CP("act", STb[:], STf[:], [STfb], [STbb])
                    for hh in range(2):
                        py, pyb = psY[hh]
                        CP("act", yi[:, hh * 512:(hh + 1) * 512], py[:], [pyb], [yib])
                    for h in range(16):
                        hs = slice(h * 64, (h + 1) * 64)
                        pi, pib = psI[h // 8]
                        STT("dve", y2[:, hs], pi[:, (h % 8) * 64:(h % 8) * 64 + 64], dtt[:, 4, h:h + 1], yi[:, hs], ALU.mult, ALU.add,
                            [pib, dtb, yib], [y2b])
                    for h in range(16):
                        hs = slice(h * 64, (h + 1) * 64)
                        STT("dve", y2[:, hs], xs[:, hs], prm[:, 2, h:h + 1], y2[:, hs], ALU.mult, ALU.add, [xsb, pb_, y2b], [y2b])
                    TT("pool", y2[:], y2[:], sz[:], ALU.mult, [y2b, szb], [y2b])
                    sm, smb = sm_r.next()
                    ACT(junk[:], y2[:], AF.Square, [y2b], [junkb])
                    RSUM("dve", sm[:, 0:4], junk[:].rearrange("p (h d) -> p h d", d=256), [junkb], [smb])
                    ACT(sm[:, 4:8], sm[:, 0:4], AF.Sqrt, [smb], [smb], scale=1.0 / 256, bias=EPS)
                    RECIP(sm[:, 8:12], sm[:, 4:8], [smb], [smb])
                    yb_, ybb = ybf_r.next()
                    for gq in range(4):
                        gs = slice(gq * 256, (gq + 1) * 256)
                        STT("dve", yb_[:, gs], y2[:, gs], sm[:, 8 + gq:9 + gq], gn[:, gs], ALU.mult, ALU.mult, [y2b, smb, pb_], [ybb])
                    transpose_rows(yb_, ybb, yTg, yTgb, tt * 128, nk=8, pool=(1,))
                S.dma(s_yT[2].rearrange("(k p) t -> p k t", p=128)[:, :, t0:t0 + TG], yTg[:], r=[yTgb])
            S.barrier()
            S.flush()

    s_ocmp = dscr("s_ocmp", (1024, T), F32)
    NQG = T // 512
    NCMP = T // 16 - 1
    NTC = (NCMP + 127) // 128
    CM_C = [31, -481, -993, -1505, -2017]

    def phase_nsa(l):
        with ExitStack() as es:
            kb = Buf()
            E = sb(es, "n_E", [128, T], BF16)
            S.dma(E[:], Cd["c_e"][:, :], w=[kb])
            wm = sb(es, "n_wm", [128, 8 * 512], BF16)
            S.dma(wm[:], Cd["c_wm"][:, :], w=[kb])
            cm = sb(es, "n_cm", [128, 5 * 512], BF16)
            S.dma(cm[:], Cd["c_cm"][:, :], w=[kb])
            kbsw = sb(es, "n_kbsw", [128, 16 * 68], F32)
            S.dma(kbsw[:], Cd["c_kbsw"][:, :], w=[kb])
            kbc = sb(es, "n_kbc", [128, 1024], F32)
            S.dma(kbc[:], Cd["c_kbc"][:, :], w=[kb])
            ov = sb(es, "n_ov", [128, 4, 129], BF16)
            S.dma(ov[:], Cd["c_ov"].rearrange("p (n c) -> p n c", c=129), w=[kb])
            wdt = sb(es, "n_wd", [128, 254], F32)
            S.dma(wdt[:], Cd["c_wd"][:, :], w=[kb])
            onesf = cst["c_onesf"]
            w1s = sb(es, "n_w1", [128, 2, 16, 64], BF16)
            w2s = sb(es, "n_w2", [64, 2, 64], BF16)
            posf = sb(es, "n_posf", [128, 2, 16], F32)
            posb = sb(es, "n_posb", [128, 2, 16], BF16)
            ccon = sb(es, "n_ccon", [64, 2], F32)
            for j in range(2):
                S.dma(w1s[:, j], Wd["nsa_cmp_w1"][l, j].rearrange("(c p) o -> p c o", p=128), w=[kb], q="pool")
                S.dma(w2s[:, j, :], Wd["nsa_cmp_w2"][l, j], w=[kb], q="pool")
                S.dma(posf[:, j, :], Wd["nsa_cmp_pos"][l, j].rearrange("(c two) d -> (two d) c", two=2), w=[kb], slow=True)
            CP("dve", posb[:], posf[:], [kb], [kb])
            for j in range(2):
                ps, pb = psum((7,))
                for c in range(16):
                    MM(ps[:64, 0:1], w1s[:, j, c, :], posb[:, j, c:c + 1], c == 0, c == 15, [kb], [pb])
                CP("dve", ccon[:, j:j + 1], ps[:64, 0:1], [pb], [kb])
            AT = sb(es, "n_AT", [64, 512], BF16)
            ATb = Buf()
            Kc = sb(es, "n_Kc", [67, 512], BF16)
            Vc = sb(es, "n_Vc", [128, 4, 65], BF16)
            Ks = sb(es, "n_Ks", [67, T], BF16)
            Kw = sb(es, "n_Kw", [67, T], BF16)
            Vs = sb(es, "n_Vs", [128, NT, 65], BF16)
            Vw = sb(es, "n_Vw", [128, NT, 65], BF16)
            kvb = Buf()
            selT = sb(es, "n_selT", [128, T], BF16)
            selTb = Buf()
            kcs2, kcs2b = selT, selTb
            impacc = sb(es, "n_imp", [128, 4, 128], F32)
            impb = [Buf() for _ in range(4)]
            q_r = Ring([sb(es, "n_q%d" % i, [67, 512], BF16) for i in range(6)])
            p_r = Ring([sb(es, "n_p%d" % i, [128, 512], BF16) for i in range(16)])
            osb_r = Ring([sb(es, "n_osb%d" % i, [65, 512], F32) for i in range(6)])
            fr_r = Ring([sb(es, "n_fr%d" % i, [65, 512], F32) for i in range(6)])
            gt_r = Ring([sb(es, "n_gt%d" % i, [65, 1, 512], F32) for i in range(6)])
            accy_r = Ring([sb(es, "n_acc%d" % i, [64, 512], F32) for i in range(8)])
            ctr_r = Ring([sb(es, "n_ctr%d" % i, [64, 512], F32) for i in range(4)])
            ybf_r = Ring([sb(es, "n_yb%d" % i, [64, 512], BF16) for i in range(3)])
            sm_r = Ring([sb(es, "n_sm%d" % i, [128, 20], F32) for i in range(4)])
            ti_r = Ring([sb(es, "n_ti%d" % i, [128, 128], F32) for i in range(4)])
            sb_r = Ring([sb(es, "n_sb%d" % i, [128, 128], BF16) for i in range(4)])

            def load_q(h, qg):
                qt, qtb = q_r.next()
                S.dma(qt[0:64, :], s_nqT[h * 64:(h + 1) * 64, qg * 512:(qg + 1) * 512], w=[qtb])
                S.dma(qt[64:67, :], Cd["c_qa"][h, :, qg * 512:(qg + 1) * 512], w=[qtb])
                return qt, qtb

            def load_g(h, qg, brs):
                gt, gtb = gt_r.next()
                for k_, br in enumerate(brs):
                    S.dma(gt[64:65, k_, :], s_ngT[br * 16 + h:br * 16 + h + 1, qg * 512:(qg + 1) * 512], w=[gtb])
                return gt, gtb

            pend = []
            LAG = 5
            SPOOL = [(4, 5, 6)]

            def unit_back():
                it = pend.pop(0)
                if callable(it):
                    it()
                    return
                Vap, pt, ptb, pso, psob, first, last = it
                MM(pso[0:65, :], Vap, pt[:], first, last, [kvb, ptb], [psob])

            def flush_units():
                while pend:
                    unit_back()

            def tile_unit(qt, qtb, Kap, Vap, bias_ap, extras, pso, psob, first, last):
                ps, pb = psum(SPOOL[0])
                MM(ps[:], Kap, qt[:], True, len(extras) == 0, [kvb, qtb], [pb])
                for ie, (lt_, rt_, bufs_) in enumerate(extras):
                    MM(ps[:], lt_, rt_, False, ie == len(extras) - 1, bufs_, [pb])
                pt, ptb = p_r.next()
                ACT(pt[:], ps[:], AF.Exp, [pb, kb], [ptb], bias=bias_ap)
                pend.append((Vap, pt, ptb, pso, psob, first, last))
                while len(pend) > LAG:
                    unit_back()
                return pt, ptb

            def fin_front(pso, psob, gt, gtb, gslot):
                osb, osbb = osb_r.next()
                CP("act", osb[:], pso[0:65, :], [psob], [osbb])
                fr, frb = fr_r.next()
                TSC("dve", fr[64:65, :], osb[64:65, :], 1e-30, None, ALU.max, None, [osbb], [frb])
                RECIP(fr[64:65, :], fr[64:65, :], [frb], [frb])
                TT("dve", fr[64:65, :], fr[64:65, :], gt[64:65, gslot, :], ALU.mult, [frb, gtb], [frb])
                return osb, osbb, fr, frb

            def fin_back(rec):
                osb, osbb, fr, frb, kind, tgt = rec
                psb_, psbb = psum((7,))
                MM(psb_[0:64, :], onesf[64:65, 0:64], fr[64:65, :], True, True, [cstb, frb], [psbb])
                if kind == "cmp":
                    h, q0 = tgt
                    ctr, ctrb = ctr_r.next()
                    TT("dve", ctr[:], osb[0:64, :], psb_[0:64, :], ALU.mult, [osbb, psbb], [ctrb])
                    S.dma(s_ocmp[h * 64:(h + 1) * 64, q0:q0 + 512], ctr[:], r=[ctrb])
                else:
                    ac, acb, store = tgt
                    ctr, ctrb = ctr_r.next()
                    TT("dve", ctr[:], osb[0:64, :], psb_[0:64, :], ALU.mult, [osbb, psbb], [ctrb])
                    TT("pool", ac[:], ac[:], ctr[:], ALU.add, [acb, ctrb], [acb])
                    if store is not None:
                        h, q0 = store
                        yb_, ybb = ybf_r.next()
                        CP("pool", yb_[:], ac[:], [acb], [ybb])
                        S.dma(s_yT[1][h * 64:(h + 1) * 64, q0:q0 + 512], yb_[:], r=[ybb])

            for gi in range(2):
                MSET("dve", Ks[:], 1.0, [kvb])
                MSET("pool", Kw[:], 1.0, [kvb])
                MSET("dve", Vs[:], 1.0, [kvb])
                MSET("pool", Vw[:], 1.0, [kvb])
                MSET("dve", Kc[:], 1.0, [kvb])
                MSET("pool", Vc[:], 1.0, [kvb])
                S.dma(Ks[0:64, :], s_nkvT[256 + gi * 64:256 + (gi + 1) * 64, :], w=[kvb])
                S.dma(Kw[0:64, :], s_nkvT[512 + gi * 64:512 + (gi + 1) * 64, :], w=[kvb])
                nkv_v = s_nkv.rearrange("(n p) c -> p n c", p=128)
                S.dma(Vs[:, :, 0:64], nkv_v[:, :, 384 + gi * 64:384 + (gi + 1) * 64], w=[kvb])
                S.dma(Vw[:, :, 0:64], nkv_v[:, :, 640 + gi * 64:640 + (gi + 1) * 64], w=[kvb])
                for j in range(2):
                    MSET("dve", kcs2[:], 0.0, [kcs2b])
                    S.dma(kcs2[0:64, :], s_nkvT[j * 128 + gi * 64:j * 128 + (gi + 1) * 64, :], w=[kcs2b])
                    S.dma(kcs2[64:128, 0:T - 1], s_nkvT[j * 128 + gi * 64:j * 128 + (gi + 1) * 64, 1:T], w=[kcs2b])
                    kv_ = kcs2[:].rearrange("p (n s) -> p n s", s=16)
                    ps, pb = psum((7,))
                    for c in range(16):
                        o_ = 2 * c
                        rhs = kv_[:, o_ // 16:o_ // 16 + NCMP, o_ % 16]
                        MM(ps[:64, 0:NCMP], w1s[:, j, c, :], rhs, c == 0, c == 15, [kb, kcs2b], [pb])
                    MSET("dve", AT[:], 0.0, [ATb])
                    ACT(AT[:, 0:NCMP], ps[:64, 0:NCMP], AF.Silu, [pb, kb], [ATb], bias=ccon[:, j:j + 1])
                    if j == 0:
                        ps2, pb2 = psum((7,))
                        MM(ps2[:64, 0:NCMP], w2s[:, 0, :], AT[:, 0:NCMP], True, True, [kb, ATb], [pb2])
                        CP("act", Kc[0:64, 0:NCMP], ps2[:64, 0:NCMP], [pb2], [kvb])
                    else:
                        for nt in range(NTC):
                            ps2, pb2 = psum((7,))
                            MM(ps2[:, 0:64], AT[:, nt * 128:(nt + 1) * 128], w2s[:, 1, :], True, True, [ATb, kb], [pb2])
                            CP("act", Vc[:, nt, 0:64], ps2[:, 0:64], [pb2], [kvb])
                SPOOL[0] = (4, 5)
                prevs = [[]]
                bi = 0
                for qg in range(NQG):
                    q0 = qg * 512
                    for j4 in range(4):
                        MSET("pool", impacc[:, j4, :], 0.0, [impb[j4]])
                    nts = [nt for nt in range(NTC) if 16 * (128 * nt) + 31 <= q0 + 511]
                    for hp in range(4):
                        heads = [gi * 8 + hp * 2 + i for i in range(2)]
                        qs = [load_q(h, qg) for h in heads]
                        accb = [(PS[(bi % 2) * 2 + i], PSB[(bi % 2) * 2 + i]) for i in range(2)]
                        bi += 1
                        pts = [[], []]
                        for i_, nt in enumerate(nts):
                            full = 16 * (128 * nt + 127) + 31 <= q0
                            extras = []
                            if not full:
                                mi = CM_C.index(2048 * nt + 31 - q0)
                                extras = [(identb[:], cm[:, mi * 512:(mi + 1) * 512], [cstb, kb])]
                            for i2, h in enumerate(heads):
                                bias_ap = kbc[:, (h * 16 + qg) * 4 + nt:(h * 16 + qg) * 4 + nt + 1]
                                pt, ptb = tile_unit(qs[i2][0], qs[i2][1], Kc[:, nt * 128:(nt + 1) * 128], Vc[:, nt, :], bias_ap, extras,
                                                    accb[i2][0], accb[i2][1], i_ == 0, i_ == len(nts) - 1)
                                pts[i2].append((nt, pt, ptb))
                        def after_a(heads=heads, accb=accb, pts=pts, q0=q0, qg=qg):
                            gs = [load_g(h, qg, (0,)) for h in heads]
                            cur = []
                            for i2, h in enumerate(heads):
                                osb, osbb, fr, frb = fin_front(accb[i2][0], accb[i2][1], gs[i2][0], gs[i2][1], 0)
                                cur.append((osb, osbb, fr, frb, "cmp", (h, q0)))
                            for i2, h in enumerate(heads):
                                for j4 in range(4):
                                    psi, psib = psum((6, 7))
                                    for i_, (nt, pt, ptb) in enumerate(pts[i2]):
                                        MM(psi[:, 0:129], pt[:, j4 * 128:(j4 + 1) * 128], ov[:, nt, :], i_ == 0, i_ == len(pts[i2]) - 1,
                                           [ptb, kb], [psib])
                                    sm, smb = sm_r.next()
                                    TSC("dve", sm[:, 0:1], psi[:, 128:129], 1e-30, None, ALU.max, None, [psib], [smb])
                                    RECIP(sm[:, 1:2], sm[:, 0:1], [smb], [smb])
                                    STT("dve", impacc[:, j4, :], psi[:, 0:128], sm[:, 1:2], impacc[:, j4, :], ALU.mult, ALU.add,
                                        [psib, smb, impb[j4]], [impb[j4]])
                            for rec in prevs[0]:
                                fin_back(rec)
                            prevs[0] = cur
                        pend.append(after_a)
                    flush_units()
                    for j4 in range(4):
                        qt_ = qg * 4 + j4
                        ti, tib = ti_r.next()
                        TT("dve", ti[:], impacc[:, j4, :], wdt[:, 126 - 2 * qt_:254 - 2 * qt_], ALU.add, [impb[j4], kb], [tib])
                        MSET("dve", ti[:, 0:1], 4e30, [tib])
                        sm, smb = sm_r.next()
                        S.dve(lambda e, o=sm[:, 0:8], i=ti[:]: e.max(out=o, in_=i), [tib], [smb])
                        t2, t2b = ti_r.next()
                        S.dve(lambda e, o=t2[:], r_=sm[:, 0:8], i=ti[:]: e.match_replace(out=o, in_to_replace=r_, in_values=i, imm_value=-3e38),
                              [tib, smb], [t2b])
                        S.dve(lambda e, o=sm[:, 8:16], i=t2[:]: e.max(out=o, in_=i), [t2b], [smb])
                        sbt, sbtb = sb_r.next()
                        TSC("dve", sbt[:], ti[:], sm[:, 15:16], None, ALU.is_ge, None, [tib, smb], [sbtb])
                        sb2, sb2b = sb_r.next()
                        TSC("dve", sb2[:], sbt[:], -1.0, 30000.0, ALU.add, ALU.mult, [sbtb], [sb2b])
                        ps, pb = psum((6, 7))
                        psv = ps[:].bitcast(BF16)
                        TR(psv[:, 0:128], sb2[:], identb[:], [sb2b, cstb], [pb])
                        CP("act", selT[:, qt_ * 128:(qt_ + 1) * 128], psv[:, 0:128], [pb], [selTb])
                flush_units()
                for rec in prevs[0]:
                    fin_back(rec)
                prevs[0] = []
                if "s_selT" in dbg:
                    S.dma(s_selT[gi], selT[:], r=[selTb])
                SPOOL[0] = (4, 5, 6)
                for qg in range(NQG):
                    q0 = qg * 512
                    for hp in range(4):
                        heads = [gi * 8 + hp * 2 + i for i in range(2)]
                        qs = [load_q(h, qg) for h in heads]
                        accs = []
                        for h in heads:
                            ac, acb = accy_r.next()
                            S.dma(ac[:], s_ocmp[h * 64:(h + 1) * 64, q0:q0 + 512], w=[acb])
                            accs.append((ac, acb))
                        for br in (1, 2):
                            if br == 1:
                                kts = list(range(0, min(4 * qg + 4, NT)))
                            else:
                                kts = [kt for kt in range(4 * qg - 4, 4 * qg + 4) if 0 <= kt < NT]
                            accb = [(PS[(bi % 2) * 2 + i], PSB[(bi % 2) * 2 + i]) for i in range(2)]
                            bi += 1
                            for i_, kt in enumerate(kts):
                                m = kt - 4 * qg
                                extras = []
                                if br == 1:
                                    extras.append((E[:, kt * 128:(kt + 1) * 128], selT[:, q0:q0 + 512], [kb, selTb]))
                                    if m >= 0:
                                        extras.append((identb[:], wm[:, (m + 4) * 512:(m + 5) * 512], [cstb, kb]))
                                    Kt, Vt = Ks, Vs
                                else:
                                    extras.append((identb[:], wm[:, (m + 4) * 512:(m + 5) * 512], [cstb, kb]))
                                    Kt, Vt = Kw, Vw
                                for i2, h in enumerate(heads):
                                    bias_ap = kbsw[:, h * 68 + m + 64:h * 68 + m + 65]
                                    tile_unit(qs[i2][0], qs[i2][1], Kt[:, kt * 128:(kt + 1) * 128], Vt[:, kt, :], bias_ap, extras,
                                              accb[i2][0], accb[i2][1], i_ == 0, i_ == len(kts) - 1)
                            def after_c(heads=heads, accb=accb, accs=accs, br=br, q0=q0, qg=qg):
                                gs = [load_g(h, qg, (br,)) for h in heads]
                                cur = []
                                for i2, h in enumerate(heads):
                                    osb, osbb, fr, frb = fin_front(accb[i2][0], accb[i2][1], gs[i2][0], gs[i2][1], 0)
                                    cur.append((osb, osbb, fr, frb, "acc", (accs[i2][0], accs[i2][1], (h, q0) if br == 2 else None)))
                                for rec in prevs[0]:
                                    fin_back(rec)
                                prevs[0] = cur
                            pend.append(after_c)
                flush_units()
                for rec in prevs[0]:
                    fin_back(rec)
                prevs[0] = []
                S.barrier()
                S.flush()

    PHASES = {"cast": phase_cast, "p1": phase_p1, "p2": phase_p2, "gla": phase_gla, "ssd": phase_ssd, "nsa": phase_nsa}
    return nc, S, PHASES, es0


_CACHE = {}


def kernel(**inputs):
    x = np.ascontiguousarray(np.asarray(inputs["x"], np.float32))
    B, T, _ = x.shape
    NL = int(np.asarray(inputs["w_in"]).shape[0])
    consts = make_consts(T)
    nc, S, PH, es0 = build(T, NL, consts)
    PH["cast"]()
    for l in range(NL):
        PH["p1"](l)
        PH["gla"](l)
        PH["ssd"](l)
        PH["nsa"](l)
        PH["p2"](l)
    es0.close()
    in_maps = []
    for b in range(B):
        m = {"x": x[b]}
        for n, _s in W_SPECS:
            m[n] = np.ascontiguousarray(np.asarray(inputs[n], np.float32))
        m.update(consts)
        in_maps.append(m)
    res = run_bass_kernel_spmd(nc, in_maps, core_ids=list(range(B)))
    return np.stack([np.asarray(res.results[b]["out"], np.float32) for b in range(B)], axis=0)
```

```python
import numpy as np
import ml_dtypes
from contextlib import ExitStack
import concourse.bass as bass
import concourse.mybir as mybir
from concourse.bass_utils import run_bass_kernel_spmd

F32 = mybir.dt.float32
BF16 = mybir.dt.bfloat16
ALU = mybir.AluOpType
AF = mybir.ActivationFunctionType
AX = mybir.AxisListType

D = 2048
DIN = 15184
DFF = 5632
NCORES = 4
C_GQ, C_GK, C_GV, C_GR, C_GA = 0, 1024, 2048, 3072, 4096
C_NQ, C_NKV, C_NG = 4112, 5136, 5904
C_SZ, C_SX, C_SDT, C_MG = 5952, 6976, 9024, 9040
EPS = 1e-6


class Buf:
    __slots__ = ("w", "r")

    def __init__(self):
        self.w = None
        self.r = {}


class Sched:
    ENG = ("pe", "act", "dve", "pool", "sp")

    def __init__(self, nc, es, ndma=14):
        self.nc = nc
        self.q = {e: [] for e in self.ENG}
        self.cnt = {e: 0 for e in self.ENG[:4]}
        names = list(self.ENG[:4]) + ["d%d" % i for i in range(ndma)]
        self.sem = {n: es.enter_context(nc.semaphore("s_" + n)) for n in names}
        self.semval = {n: 0 for n in names}
        self.waited = {e: {} for e in self.ENG}
        self.ndma = ndma
        self.dma_i = 0
        self.n_ops = 0

    def _deps(self, eng, reads, writes):
        deps = {}
        for b in reads:
            if b.w is not None:
                s, v = b.w
                if deps.get(s, 0) < v:
                    deps[s] = v
        for b in writes:
            if b.w is not None:
                s, v = b.w
                if deps.get(s, 0) < v:
                    deps[s] = v
            for s, v in b.r.items():
                if deps.get(s, 0) < v:
                    deps[s] = v
        wd = self.waited[eng]
        q = self.q[eng]
        for s, v in deps.items():
            if eng == "pe" and s == "pe":
                continue
            if wd.get(s, 0) < v:
                wd[s] = v
                q.append((0, s, v))

    def _mark(self, tok, reads, writes):
        s, v = tok
        for b in reads:
            if b.r.get(s, 0) < v:
                b.r[s] = v
        for b in writes:
            b.w = tok
            b.r = {}

    def op(self, eng, fn, reads=(), writes=()):
        self._deps(eng, reads, writes)
        self.cnt[eng] += 1
        c = self.cnt[eng]
        self.semval[eng] = c
        self.q[eng].append((1, fn, eng))
        self._mark((eng, c), reads, writes)
        self.n_ops += 1

    def pe(self, fn, r=(), w=()):
        self.op("pe", fn, r, w)

    def act(self, fn, r=(), w=()):
        self.op("act", fn, r, w)

    def dve(self, fn, r=(), w=()):
        self.op("dve", fn, r, w)

    def pool(self, fn, r=(), w=()):
        self.op("pool", fn, r, w)

    def dma(self, out, in_, r=(), w=(), q="sp", slow=False):
        k = self.dma_i % self.ndma
        n = self.dma_i // self.ndma
        self.dma_i += 1
        s = "d%d" % k
        if n > 0:
            wd = self.waited[q]
            if wd.get(s, 0) < 16 * n:
                wd[s] = 16 * n
                self.q[q].append((0, s, 16 * n))
        self._deps(q, r, w)
        v = 16 * (n + 1)
        self.semval[s] = v
        self.q[q].append((2, out, in_, s, slow))
        self._mark((s, v), r, w)
        self.n_ops += 1

    def barrier(self):
        for e in self.ENG:
            wd = self.waited[e]
            q = self.q[e]
            for s, v in self.semval.items():
                if v > 0 and wd.get(s, 0) < v and not (e == "pe" and s == "pe"):
                    wd[s] = v
                    q.append((0, s, v))

    def flush(self):
        with self.nc.Block() as block:
            self.emit(block)
        for e in self.ENG:
            self.q[e] = []

    def emit(self, block):
        names = {"pe": "tensor", "act": "scalar", "dve": "vector", "pool": "gpsimd", "sp": "sync"}
        sem = self.sem
        for e, attr in names.items():
            items = self.q[e]

            def body(eng, items=items):
                for it in items:
                    if it[0] == 0:
                        eng.wait_ge(sem[it[1]], it[2])
                    elif it[0] == 1:
                        it[1](eng).then_inc(sem[it[2]], 1)
                    else:
                        if it[4]:
                            eng.dma_start(out=it[1], in_=it[2], allow_slow_non_contiguous=True).then_inc(sem[it[3]], 16)
                        else:
                            eng.dma_start(out=it[1], in_=it[2]).then_inc(sem[it[3]], 16)

            getattr(block, attr)(body)


class Ring:
    def __init__(self, tiles):
        self.tiles = tiles
        self.bufs = [Buf() for _ in tiles]
        self.i = 0

    def next(self):
        k = self.i % len(self.tiles)
        self.i += 1
        return self.tiles[k], self.bufs[k]


class Feeder:
    def __init__(self, S, ring, jobs):
        self.S, self.ring, self.jobs = S, ring, jobs
        self.R = len(ring.tiles)
        self.issued = 0
        self.got = 0
        self.done = 0
        self.slots = {}

    def _issue(self):
        i = self.issued
        view_fn, src = self.jobs[i]
        t, b = self.ring.next()
        v = view_fn(t)
        self.S.dma(v, src, w=[b])
        self.slots[i] = (v, b)
        self.issued += 1

    def pump(self):
        while self.issued < len(self.jobs) and self.issued < self.done + self.R:
            self._issue()

    def get(self):
        self.pump()
        assert self.got < self.issued, "feeder ring too small"
        r = self.slots.pop(self.got)
        self.got += 1
        return r

    def release(self, n=1):
        self.done += n
        self.pump()


def _bf(a):
    return np.asarray(a, np.float32).astype(ml_dtypes.bfloat16)


def make_consts(T):
    c = {}
    i = np.arange(128)
    same = (i[:, None] // 64) == (i[None, :] // 64)
    up = i[:, None] <= i[None, :]
    c["c_identb"] = _bf(np.eye(128))
    c["c_identf"] = np.eye(128, dtype=np.float32)
    c["c_tris"] = np.where(same & up, -1.0 / 16.0, 0.0).astype(np.float32)
    c["c_tri1"] = np.where(same & up, 1.0, 0.0).astype(np.float32)
    c["c_blk"] = np.where(same, 1.0, 0.0).astype(np.float32)
    ch = np.zeros((128, 256), np.float32)
    ch[:64, :128] = 1.0
    ch[64:, 128:] = 1.0
    c["c_chsel"] = ch
    c["c_amask"] = _bf(np.where(same & up, 1.0, 0.0))
    mneg = np.where(same & up, 0.0, -1e9).astype(np.float32)
    c["c_mneg4"] = np.tile(mneg, (1, 4))
    c["c_onesf"] = np.ones((128, 128), np.float32)
    slopes = (2.0 ** (-8.0 * np.arange(1, 17) / 16.0)).astype(np.float64)
    irel = np.arange(512, dtype=np.float64)
    qa = np.zeros((16, 3, 512), np.float32)
    for h in range(16):
        v = (-slopes[h] * irel).astype(np.float32)
        hi = v.astype(ml_dtypes.bfloat16).astype(np.float32)
        r1 = v - hi
        mid = r1.astype(ml_dtypes.bfloat16).astype(np.float32)
        lo = (r1 - mid).astype(ml_dtypes.bfloat16).astype(np.float32)
        qa[h, 0], qa[h, 1], qa[h, 2] = hi, mid, lo
    c["c_qa"] = _bf(np.tile(qa, (1, 1, T // 512)))
    j = np.arange(128, dtype=np.float64)
    dv = np.arange(-64, 4, dtype=np.float64)
    kb = slopes[None, :, None] * (128.0 * dv[None, None, :] + j[:, None, None])
    c["c_kbsw"] = kb.astype(np.float32).reshape(128, 16 * 68)
    qg = np.arange(16, dtype=np.float64)
    nt = np.arange(4, dtype=np.float64)
    kc = slopes[None, :, None, None] * (16.0 * (128.0 * nt[None, None, None, :] + j[:, None, None, None]) + 31.0
                                        - 512.0 * qg[None, None, :, None])
    c["c_kbc"] = kc.astype(np.float32).reshape(128, 16 * 16 * 4)
    ii = np.arange(512)
    wm = np.zeros((8, 128, 512), np.float32)
    for m in range(8):
        u = ii[None, :] - i[:, None] + 512 - 128 * m
        wm[m] = ((u >= 0) & (u < 512)).astype(np.float32)
    c["c_wm"] = _bf((wm.transpose(1, 0, 2).reshape(128, 8 * 512) - 1.0) * 30000.0)
    cm = np.zeros((5, 128, 512), np.float32)
    for k, cc in enumerate([31, -481, -993, -1505, -2017]):
        cm[k] = ((ii[None, :] - 16 * i[:, None]) >= cc).astype(np.float32)
    c["c_cm"] = _bf((cm.transpose(1, 0, 2).reshape(128, 5 * 512) - 1.0) * 30000.0)
    n_cmp = (8192 - 32) // 16 + 1
    n_sel = 128
    ov = np.zeros((512, 129), np.float32)
    tok = (np.arange(n_cmp) * 16)[:, None] + np.arange(32)[None, :]
    np.add.at(ov, (np.repeat(np.arange(n_cmp), 32), (tok // 64).ravel()), 1.0 / 32)
    ov[:n_cmp, 128] = 1.0
    c["c_ov"] = _bf(ov.reshape(4, 128, 129).transpose(1, 0, 2).reshape(128, 4 * 129))
    e = np.zeros((128, T), np.float32)
    e[np.arange(T) // 64, np.arange(T)] = 1.0
    c["c_e"] = _bf(e)
    wd = np.zeros((128, 254), np.float32)
    dd = np.arange(254) - 126
    cur = (i >= 64).astype(np.int64)
    for q in range(128):
        row = np.zeros(254, np.float32)
        fut = dd > cur[q]
        row[fut] = -1e30 - 1e28 * dd[fut]
        row[dd == cur[q]] = 2e30
        row[dd == cur[q] - 1] = 3e30
        wd[q] = row
    c["c_wd"] = wd
    c["c_ones_bf"] = _bf(np.ones((128, 512)))
    return c


W_SPECS = [
    ("w_in", (D, DIN)), ("gla_a2", (16, 1024)), ("gla_a_bias", (1024,)), ("gla_norm", (256,)),
    ("nsa_cmp_pos", (2, 32, 64)), ("nsa_cmp_w1", (2, 2048, 64)), ("nsa_cmp_w2", (2, 64, 64)),
    ("ssm_conv_w", (4, 2048)), ("ssm_conv_b", (2048,)), ("ssm_dt_bias", (16,)), ("ssm_a_log", (16,)),
    ("ssm_d", (16,)), ("ssm_norm", (1024,)), ("w_branch", (3, 1024, D)), ("w_out", (D, D)),
    ("norm_pre_mix", (D,)), ("norm_post_mix", (D,)), ("norm_pre_ffn", (D,)), ("norm_post_ffn", (D,)),
    ("w_ffn_gate", (D, DFF)), ("w_ffn_up", (D, DFF)), ("w_ffn_down", (DFF, D)),
]


def build(T, NL, consts, dbg=()):
    nc = bass.Bass("TRN2", target_bir_lowering=False)
    es0 = ExitStack()
    S = Sched(nc, es0)
    NT = T // 128

    def din(name, shape, dt=F32):
        return nc.dram_tensor(name, list(shape), dt, kind="ExternalInput").ap()

    def dscr(name, shape, dt):
        if name in dbg:
            return nc.dram_tensor(name, list(shape), dt, kind="ExternalOutput").ap()
        return nc.dram_tensor(name, list(shape), dt).ap()

    x_in = din("x", (T, D))
    Wd = {n: din(n, (NL,) + s) for n, s in W_SPECS}
    Cd = {n: din(n, a.shape, BF16 if a.dtype != np.float32 else F32) for n, a in consts.items()}
    out = nc.dram_tensor("out", [T, D], F32, kind="ExternalOutput").ap()

    wb_in = dscr("wb_in", (NL, D, DIN), BF16)
    wb_br = dscr("wb_br", (NL, 3072, D), BF16)
    wb_out = dscr("wb_out", (NL, D, D), BF16)
    wb_g = dscr("wb_g", (NL, D, DFF), BF16)
    wb_u = dscr("wb_u", (NL, D, DFF), BF16)
    wb_d = dscr("wb_d", (NL, DFF, D), BF16)
    s_qT = dscr("s_qT", (1024, T), F32)
    s_kT = dscr("s_kT", (1024, T), F32)
    s_gv = dscr("s_gv", (T, 1024), BF16)
    s_gr = dscr("s_gr", (T, 1024), F32)
    s_aT = dscr("s_aT", (16, T), F32)
    s_nqT = dscr("s_nqT", (1024, T), BF16)
    s_nkvT = dscr("s_nkvT", (768, T), BF16)
    s_nkv = dscr("s_nkv", (T, 768), BF16)
    s_ngT = dscr("s_ngT", (48, T), F32)
    s_sz = dscr("s_sz", (T, 1024), F32)
    s_sxT = dscr("s_sxT", (2048, T), F32)
    s_sdt = dscr("s_sdt", (T, 16), F32)
    s_mgT = dscr("s_mgT", (6144, T), BF16)
    s_yT = [dscr("s_yT%d" % i, (1024, T), BF16) for i in range(3)]
    s_selT = dscr("s_selT", (2, 128, T), BF16)

    PS = [es0.enter_context(nc.psum_tensor("ps%d" % i, [128, 512], F32)) for i in range(8)]
    PSB = [Buf() for _ in range(8)]
    ps_i = [0]

    def psum(pool=None):
        if pool is not None:
            k = pool[ps_i[0] % len(pool)]
        else:
            k = ps_i[0] % 8
        ps_i[0] += 1
        return PS[k], PSB[k]

    sb_n = [0]

    def sb(es, name, shape, dt):
        sb_n[0] += 1
        return es.enter_context(nc.sbuf_tensor("%s_%d" % (name, sb_n[0]), list(shape), dt))

    def ACT(out_, in_, func, r, w, **kw):
        S.act(lambda e: e.activation(out=out_, in_=in_, func=func, **kw), r, w)

    def TSC(eng, out_, in0, s1, s2, op0, op1, r, w):
        if op1 is None:
            S.op(eng, lambda e: e.tensor_scalar(out=out_, in0=in0, scalar1=s1, scalar2=None, op0=op0), r, w)
        else:
            S.op(eng, lambda e: e.tensor_scalar(out=out_, in0=in0, scalar1=s1, scalar2=s2, op0=op0, op1=op1), r, w)

    def STT(eng, out_, in0, sc, in1, op0, op1, r, w):
        S.op(eng, lambda e: e.scalar_tensor_tensor(out=out_, in0=in0, scalar=sc, in1=in1, op0=op0, op1=op1), r, w)

    def TT(eng, out_, in0, in1, op, r, w):
        S.op(eng, lambda e: e.tensor_tensor(out=out_, in0=in0, in1=in1, op=op), r, w)

    def CP(eng, out_, in_, r, w):
        if eng == "act":
            S.act(lambda e: e.activation(out=out_, in_=in_, func=AF.Copy), r, w)
        else:
            S.op(eng, lambda e: e.tensor_copy(out=out_, in_=in_), r, w)

    def MM(out_, lhsT, rhs, start, stop, r, w):
        S.pe(lambda e: e.matmul(out_, lhsT, rhs, start=start, stop=stop, skip_group_check=True), r, w)

    def TR(out_, in_, ident, r, w):
        S.pe(lambda e: e.transpose(out_, in_, ident), r, w)

    def RSUM(eng, out_, in_, r, w):
        S.op(eng, lambda e: e.reduce_sum(out=out_, in_=in_, axis=AX.X), r, w)

    def RECIP(out_, in_, r, w):
        S.dve(lambda e: e.reciprocal(out=out_, in_=in_), r, w)

    def MSET(eng, ap, val, w):
        S.op(eng, lambda e: e.memset(ap, val), (), w)

    cst = {}
    cstb = Buf()
    for n in ("c_identb", "c_identf", "c_tris", "c_tri1", "c_blk", "c_chsel", "c_amask", "c_mneg4", "c_onesf"):
        a = consts[n]
        t = sb(es0, "k" + n, a.shape, BF16 if a.dtype != np.float32 else F32)
        S.dma(t[:], Cd[n][:, :], w=[cstb])
        cst[n] = t
    identb, identf = cst["c_identb"], cst["c_identf"]

    def bview(dram2d):
        return dram2d.rearrange("(kc p) n -> p kc n", p=128)

    def phase_cast():
        with ExitStack() as es:
            ring = Ring([sb(es, "cst%d" % i, [128, 8192], BF16) for i in range(3)])
            jobs = []
            for l in range(NL):
                jobs += [(Wd["w_in"][l], wb_in[l], D, DIN),
                         (Wd["w_branch"][l].rearrange("b k n -> (b k) n"), wb_br[l], 3072, D),
                         (Wd["w_out"][l], wb_out[l], D, D),
                         (Wd["w_ffn_gate"][l], wb_g[l], D, DFF), (Wd["w_ffn_up"][l], wb_u[l], D, DFF),
                         (Wd["w_ffn_down"][l], wb_d[l], DFF, D)]
            for src, dst, K, N in jobs:
                for r0 in range(0, K, 128):
                    for c0 in range(0, N, 8192):
                        w_ = min(8192, N - c0)
                        t, b = ring.next()
                        S.dma(t[:, :w_], src[r0:r0 + 128, c0:c0 + w_], w=[b], q="pool")
                        S.dma(dst[r0:r0 + 128, c0:c0 + w_], t[:, :w_], r=[b], q="sp")
            S.barrier()
            S.flush()

    def rms_to_bf16(xt, xb, gain, gainb, hb, hbb, junk, junkb, sm, smb):
        ACT(junk[:], xt, AF.Square, [xb], [junkb])
        RSUM("dve", sm[:, 0:1], junk[:], [junkb], [smb])
        ACT(sm[:, 1:2], sm[:, 0:1], AF.Sqrt, [smb], [smb], scale=1.0 / D, bias=EPS)
        RECIP(sm[:, 2:3], sm[:, 1:2], [smb], [smb])
        STT("dve", hb, xt, sm[:, 2:3], gain[:], ALU.mult, ALU.mult, [xb, smb, gainb], [hbb])

    def transpose_rows(hb, hbb, dstT, dstb, col0, nk=16, evac="act", pool=None):
        for half in range(0, nk, 8):
            n = min(8, nk - half)
            ps, pb = psum(pool)
            psv = ps[:].bitcast(BF16)
            for k in range(n):
                TR(psv[:, k * 128:(k + 1) * 128], hb[:, (half + k) * 128:(half + k + 1) * 128], identb[:], [hbb, cstb], [pb])
            CP(evac, dstT[:, half:half + n, col0:col0 + 128],
               psv[:, :n * 128].rearrange("p (k t) -> p k t", t=128), [pb], [dstb])

    TS1 = min(1024, T)
    P1_JOBS = [
        (C_GQ, 1024, "F", s_qT, None), (C_GK, 1024, "F", s_kT, None),
        (C_GV, 1024, "T", s_gv, None), (C_GR, 1024, "T", s_gr, AF.Silu),
        (C_GA, 16, "F", s_aT, None),
        (C_NQ, 1024, "F", s_nqT, "scale8"),
        (C_NKV, 768, "F", s_nkvT, None), (C_NKV, 768, "T", s_nkv, None),
        (C_NG, 48, "F", s_ngT, AF.Sigmoid),
        (C_SZ, 1024, "T", s_sz, AF.Silu), (C_SX, 2048, "F", s_sxT, None), (C_SDT, 16, "T", s_sdt, None),
        (C_MG, 6144, "F", s_mgT, AF.Sigmoid),
    ]

    def phase_p1(l):
        xsrc = x_in if l == 0 else out
        with ExitStack() as es:
            gain = sb(es, "p1gain", [128, D], F32)
            gainb = Buf()
            S.dma(gain[:], Wd["norm_pre_mix"][l:l + 1, :].partition_broadcast(128), w=[gainb])
            xt_r = Ring([sb(es, "p1xt%d" % i, [128, D], F32) for i in range(2)])
            hb_r = Ring([sb(es, "p1hb%d" % i, [128, D], BF16) for i in range(2)])
            junk = sb(es, "p1junk", [128, D], F32)
            junkb = Buf()
            sm_r = Ring([sb(es, "p1sm%d" % i, [128, 4], F32) for i in range(4)])
            hT = sb(es, "p1hT", [128, 16, TS1], BF16)
            hTb = Buf()
            w_r = Ring([sb(es, "p1w%d" % i, [128, 16, 512], BF16) for i in range(3)])
            stf_r = Ring([sb(es, "p1sf%d" % i, [128, 512], F32) for i in range(3)])
            stb_r = Ring([sb(es, "p1sb%d" % i, [128, 512], BF16) for i in range(3)])
            wv = bview(wb_in[l])
            wjobs = []
            for _ts in range(T // TS1):
                for (c0, ncols, mode, dest, fn) in P1_JOBS:
                    for cb in range(0, ncols, 512):
                        nb = min(512, ncols - cb)
                        wjobs.append(((lambda t, nb=nb: t[:, :, :nb]), wv[:, :, c0 + cb:c0 + cb + nb]))
            feed = Feeder(S, w_r, wjobs)
            for ts in range(T // TS1):
                tb = ts * TS1
                for tt in range(TS1 // 128):
                    t0 = tb + tt * 128
                    xt, xb = xt_r.next()
                    S.dma(xt[:], xsrc[t0:t0 + 128, :], w=[xb])
                    hb, hbb = hb_r.next()
                    sm, smb = sm_r.next()
                    rms_to_bf16(xt[:], xb, gain, gainb, hb[:], hbb, junk, junkb, sm, smb)
                    transpose_rows(hb, hbb, hT, hTb, tt * 128)
                for (c0, ncols, mode, dest, fn) in P1_JOBS:
                    isbf = dest.dtype == BF16
                    for cb in range(0, ncols, 512):
                        nb = min(512, ncols - cb)
                        wt, wtb = feed.get()
                        if mode == "F":
                            for fs in range(0, nb, 128):
                                nf = min(128, nb - fs)
                                for tg in range(0, TS1, 512):
                                    ps, pb = psum()
                                    for kc in range(16):
                                        MM(ps[:nf, :], wt[:, kc, fs:fs + nf], hT[:, kc, tg:tg + 512], kc == 0, kc == 15,
                                           [wtb, hTb], [pb])
                                    st, stb = (stb_r if isbf else stf_r).next()
                                    if fn == "scale8":
                                        ACT(st[:nf, :], ps[:nf, :], AF.Copy, [pb], [stb], scale=0.125)
                                    elif fn is None:
                                        CP("dve", st[:nf, :], ps[:nf, :], [pb], [stb])
                                    else:
                                        ACT(st[:nf, :], ps[:nf, :], fn, [pb], [stb])
                                    f0 = cb + fs
                                    S.dma(dest[f0:f0 + nf, tb + tg:tb + tg + 512], st[:nf, :], r=[stb])
                        else:
                            for tt in range(TS1 // 128):
                                ps, pb = psum()
                                for kc in range(16):
                                    MM(ps[:, :nb], hT[:, kc, tt * 128:(tt + 1) * 128], wt[:, kc, :nb], kc == 0, kc == 15,
                                       [wtb, hTb], [pb])
                                st, stb = (stb_r if isbf else stf_r).next()
                                if fn is None:
                                    CP("dve", st[:, :nb], ps[:, :nb], [pb], [stb])
                                else:
                                    ACT(st[:, :nb], ps[:, :nb], fn, [pb], [stb])
                                t0 = tb + tt * 128
                                S.dma(dest[t0:t0 + 128, cb:cb + nb], st[:, :nb], r=[stb])
                        feed.release()
            S.barrier()
            S.flush()

    TS2 = min(512, T)
    NT2 = TS2 // 128

    def phase_p2(l):
        xsrc = x_in if l == 0 else out
        with ExitStack() as es:
            gA = sb(es, "p2gA", [128, D], F32)
            gB = sb(es, "p2gB", [128, D], F32)
            gAb, gBb = Buf(), Buf()
            big = sb(es, "p2big", [128, 48 * TS2], BF16)
            yT = [big[:, b * 8 * TS2:(b + 1) * 8 * TS2].rearrange("p (k t) -> p k t", t=TS2) for b in range(3)]
            mT = big[:, 32 * TS2:48 * TS2].rearrange("p (k t) -> p k t", t=TS2)
            zf = big[:, 0:32 * TS2].bitcast(F32).rearrange("p (n c) -> p n c", c=D)
            aT = big[:, 0:44 * TS2].rearrange("p (k t) -> p k t", t=TS2)
            yb = Buf()
            mb = [Buf() for _ in range(16)]
            zb = [Buf() for _ in range(NT2)]
            ab = [Buf() for _ in range(44)]
            xt = sb(es, "p2xt", [128, NT2, D], F32)
            xtb = Buf()
            h2T = sb(es, "p2h2T", [128, 16, TS2], BF16)
            h2Tb = Buf()
            w_r = Ring([sb(es, "p2w%d" % i, [128, 8192], BF16) for i in range(3)])
            g_r = Ring([sb(es, "p2g%d" % i, [128, 3, TS2], BF16) for i in range(2)])
            junk = sb(es, "p2junk", [128, D], F32)
            junkb = Buf()
            hb_r = Ring([sb(es, "p2hb%d" % i, [128, D], BF16) for i in range(2)])
            x1_r = Ring([sb(es, "p2x1%d" % i, [128, D], F32) for i in range(1)])
            sm_r = Ring([sb(es, "p2sm%d" % i, [128, 4], F32) for i in range(4)])
            tmp_r = Ring([sb(es, "p2tmp%d" % i, [128, TS2], F32) for i in range(3)])
            wbr = wb_br[l].rearrange("(b kc p) n -> p b kc n", p=128, kc=8)
            wov = bview(wb_out[l])
            wgv, wuv = bview(wb_g[l]), bview(wb_u[l])
            wdv = bview(wb_d[l])
            mgv = s_mgT.rearrange("(b f) t -> f b t", b=3)
            wjobs = []
            v8 = lambda t: t[:, :4096].rearrange("p (k n) -> p k n", n=512)
            v16 = lambda t: t[:].rearrange("p (k n) -> p k n", n=512)
            v11 = lambda t: t[:, :11 * 512].rearrange("p (k n) -> p k n", n=512)
            for _ts in range(T // TS2):
                for cb in range(4):
                    for b in range(3):
                        wjobs.append((v8, wbr[:, b, :, cb * 512:(cb + 1) * 512]))
                for cb in range(4):
                    wjobs.append((v16, wov[:, :, cb * 512:(cb + 1) * 512]))
                for fb in range(11):
                    wjobs.append((v16, wgv[:, :, fb * 512:(fb + 1) * 512]))
                    wjobs.append((v16, wuv[:, :, fb * 512:(fb + 1) * 512]))
                for cb in range(4):
                    for kq in range(4):
                        wjobs.append((v11, wdv[:, kq * 11:(kq + 1) * 11, cb * 512:(cb + 1) * 512]))
            feed = Feeder(S, w_r, wjobs)
            for ts in range(T // TS2):
                tb = ts * TS2
                S.dma(gA[:], Wd["norm_post_mix"][l:l + 1, :].partition_broadcast(128), w=[gAb])
                if ts == 0:
                    S.dma(gB[:], Wd["norm_pre_ffn"][l:l + 1, :].partition_broadcast(128), w=[gBb])
                for b in range(3):
                    S.dma(yT[b], s_yT[b].rearrange("(k p) t -> p k t", p=128)[:, :, tb:tb + TS2], w=[yb])
                for n in range(NT2):
                    S.dma(xt[:, n, :], xsrc[tb + n * 128:tb + (n + 1) * 128, :], w=[xtb])
                for cb in range(4):
                    wts = []
                    for b in range(3):
                        wts.append(feed.get())
                    for fs in range(4):
                        fc = cb * 4 + fs
                        gt, gtb = g_r.next()
                        S.dma(gt[:], mgv[fc * 128:(fc + 1) * 128, :, tb:tb + TS2], w=[gtb])
                        tmp, tmpb = tmp_r.next()
                        for b in range(3):
                            ps, pb = psum()
                            wtv, wtb = wts[b]
                            for kc in range(8):
                                MM(ps[:, :TS2], wtv[:, kc, fs * 128:(fs + 1) * 128], yT[b][:, kc, :], kc == 0, kc == 7,
                                   [wtb, yb], [pb])
                            if b == 0:
                                TT("dve", tmp[:], ps[:, :TS2], gt[:, 0, :], ALU.mult, [pb, gtb], [tmpb])
                            elif b == 1:
                                tmp2, tmp2b = tmp_r.next()
                                TT("dve", tmp2[:], ps[:, :TS2], gt[:, 1, :], ALU.mult, [pb, gtb], [tmp2b])
                                TT("pool", tmp[:], tmp[:], tmp2[:], ALU.add, [tmpb, tmp2b], [tmpb])
                            else:
                                tmp2, tmp2b = tmp_r.next()
                                TT("dve", tmp2[:], ps[:, :TS2], gt[:, 2, :], ALU.mult, [pb, gtb], [tmp2b])
                                TT("pool", mT[:, fc, :], tmp[:], tmp2[:], ALU.add, [tmpb, tmp2b], [mb[fc]])
                    feed.release(3)
                S.barrier()
                for cb in range(4):
                    wtv, wtb = feed.get()
                    for n in range(NT2):
                        ps, pb = psum()
                        for kc in range(16):
                            MM(ps[:], mT[:, kc, n * 128:(n + 1) * 128], wtv[:, kc, :], kc == 0, kc == 15, [wtb, mb[kc]], [pb])
                        CP("act", zf[:, n, cb * 512:(cb + 1) * 512], ps[:], [pb], [zb[n]])
                    feed.release()
                for n in range(NT2):
                    sm, smb = sm_r.next()
                    ACT(junk[:], zf[:, n, :], AF.Square, [zb[n]], [junkb])
                    RSUM("dve", sm[:, 0:1], junk[:], [junkb], [smb])
                    ACT(sm[:, 1:2], sm[:, 0:1], AF.Sqrt, [smb], [smb], scale=1.0 / D, bias=EPS)
                    RECIP(sm[:, 2:3], sm[:, 1:2], [smb], [smb])
                    STT("dve", junk[:], zf[:, n, :], sm[:, 2:3], gA[:], ALU.mult, ALU.mult, [zb[n], smb, gAb], [junkb])
                    TT("pool", xt[:, n, :], xt[:, n, :], junk[:], ALU.add, [xtb, junkb], [xtb])
                    S.dma(out[tb + n * 128:tb + (n + 1) * 128, :], xt[:, n, :], r=[xtb])
                    hb, hbb = hb_r.next()
                    sm, smb = sm_r.next()
                    rms_to_bf16(xt[:, n, :], xtb, gB, gBb, hb[:], hbb, junk, junkb, sm, smb)
                    transpose_rows(hb, hbb, h2T, h2Tb, n * 128)
                S.barrier()
                S.dma(gA[:], Wd["norm_post_ffn"][l:l + 1, :].partition_broadcast(128), w=[gAb])
                for fb in range(11):
                    wgv_, wgb = feed.get()
                    wuv_, wub = feed.get()
                    for fs in range(4):
                        psg, pgb = psum()
                        for kc in range(16):
                            MM(psg[:, :TS2], wgv_[:, kc, fs * 128:(fs + 1) * 128], h2T[:, kc, :], kc == 0, kc == 15, [wgb, h2Tb], [pgb])
                        psu, pub = psum()
                        for kc in range(16):
                            MM(psu[:, :TS2], wuv_[:, kc, fs * 128:(fs + 1) * 128], h2T[:, kc, :], kc == 0, kc == 15, [wub, h2Tb], [pub])
                        tmp, tmpb = tmp_r.next()
                        ACT(tmp[:], psg[:, :TS2], AF.Silu, [pgb], [tmpb])
                        TT("dve", aT[:, fb * 4 + fs, :], tmp[:], psu[:, :TS2], ALU.mult, [tmpb, pub], [ab[fb * 4 + fs]])
                    feed.release(2)
                for cb in range(4):
                    pss = [psum() for _ in range(NT2)]
                    for kq in range(4):
                        wtv, wtb = feed.get()
                        for n in range(NT2):
                            ps, pb = pss[n]
                            for k in range(11):
                                kc = kq * 11 + k
                                MM(ps[:], aT[:, kc, n * 128:(n + 1) * 128], wtv[:, k, :], kc == 0, kc == 43, [wtb, ab[kc]], [pb])
                        feed.release()
                    for n in range(NT2):
                        ps, pb = pss[n]
                        CP("act", xt[:, n, cb * 512:(cb + 1) * 512], ps[:], [pb], [xtb])
                for n in range(NT2):
                    x1, x1b = x1_r.next()
                    S.dma(x1[:], out[tb + n * 128:tb + (n + 1) * 128, :], w=[x1b])
                    sm, smb = sm_r.next()
                    ACT(junk[:], xt[:, n, :], AF.Square, [xtb], [junkb])
                    RSUM("dve", sm[:, 0:1], junk[:], [junkb], [smb])
                    ACT(sm[:, 1:2], sm[:, 0:1], AF.Sqrt, [smb], [smb], scale=1.0 / D, bias=EPS)
                    RECIP(sm[:, 2:3], sm[:, 1:2], [smb], [smb])
                    STT("dve", junk[:], xt[:, n, :], sm[:, 2:3], gA[:], ALU.mult, ALU.mult, [xtb, smb, gAb], [junkb])
                    TT("pool", x1[:], x1[:], junk[:], ALU.add, [x1b, junkb], [x1b])
                    S.dma(out[tb + n * 128:tb + (n + 1) * 128, :], x1[:], r=[x1b])
                S.barrier()
                S.flush()

    def phase_gla(l):
        TG = 512
        with ExitStack() as es:
            wa2 = sb(es, "g_wa2", [17, 1024], F32)
            wa2b = Buf()
            S.dma(wa2[0:16, :], Wd["gla_a2"][l], w=[wa2b])
            S.dma(wa2[16:17, :], Wd["gla_a_bias"][l:l + 1, :], w=[wa2b])
            gng = sb(es, "g_gng", [128, 256], F32)
            gngb = Buf()
            S.dma(gng[:], Wd["gla_norm"][l:l + 1, :].partition_broadcast(128), w=[gngb])
            am4 = sb(es, "g_am4", [128, 512], BF16)
            am4b = Buf()
            for h in range(4):
                S.dma(am4[:, h * 128:(h + 1) * 128], Cd["c_amask"][:, :], w=[am4b])
            aTa = sb(es, "g_aTa", [17, TG], F32)
            aTab = Buf()
            MSET("dve", aTa[:], 1.0, [aTab])
            lt = sb(es, "g_lt", [128, 4, 1024], F32)
            ltb = [Buf() for _ in range(4)]
            Eq = sb(es, "g_Eq", [128, 8, TG], F32)
            Eqb = [Buf() for _ in range(8)]
            Ei_r = Ring([sb(es, "g_Ei%d" % i, [128, TG], F32) for i in range(2)])
            q_r = Ring([sb(es, "g_q%d" % i, [128, TG], F32) for i in range(2)])
            k_r = Ring([sb(es, "g_k%d" % i, [128, TG], F32) for i in range(2)])
            e_r = Ring([sb(es, "g_e%d" % i, [128, 512], F32) for i in range(2)])
            qdT = sb(es, "g_qdT", [128, 8, TG], BF16)
            kiT = sb(es, "g_kiT", [128, 8, TG], BF16)
            keT = sb(es, "g_keT", [128, 8, TG], BF16)
            qdb = [Buf() for _ in range(8)]
            kib = [Buf() for _ in range(8)]
            keb = [Buf() for _ in range(8)]
            ketok = sb(es, "g_ketok", [128, 4, 1024], BF16)
            ketokb = [Buf() for _ in range(4)]
            gv = sb(es, "g_gv", [128, 4, 1024], BF16)
            gvb = Buf()
            gr = sb(es, "g_gr", [128, 4, 1024], F32)
            grb = Buf()
            Sf = sb(es, "g_Sf", [128, 2, 4, 256], F32)
            Sb_ = sb(es, "g_Sb", [128, 2, 4, 256], BF16)
            Sfb, Sbb = Buf(), Buf()
            MSET("dve", Sf[:], 0.0, [Sfb])
            MSET("pool", Sb_[:], 0.0, [Sbb])
            att_r = Ring([sb(es, "g_att%d" % i, [128, 512], BF16) for i in range(2)])
            junk = sb(es, "g_junk", [128, 1024], F32)
            junkb = Buf()
            ytmp = sb(es, "g_ytmp", [128, 1024], F32)
            ytmpb = Buf()
            ybf_r = Ring([sb(es, "g_ybf%d" % i, [128, 1024], BF16) for i in range(2)])
            yTg = sb(es, "g_yTg", [128, 8, TG], BF16)
            yTgb = Buf()
            sm_r = Ring([sb(es, "g_sm%d" % i, [128, 12], F32) for i in range(4)])
            tris = cst["c_tris"]
            for g in range(T // TG):
                t0 = g * TG
                S.dma(aTa[0:16, :], s_aT[:, t0:t0 + TG], w=[aTab])
                S.dma(gv[:], s_gv[t0:t0 + TG, :].rearrange("(n p) c -> p n c", p=128), w=[gvb])
                S.dma(gr[:], s_gr[t0:t0 + TG, :].rearrange("(n p) c -> p n c", p=128), w=[grb])
                for tt in range(4):
                    for hf in range(2):
                        ps, pb = psum((2, 3))
                        MM(ps[:], aTa[:, tt * 128:(tt + 1) * 128], wa2[:, hf * 512:(hf + 1) * 512], True, True, [aTab, wa2b], [pb])
                        e_, eb = e_r.next()
                        ACT(e_[:], ps[:], AF.Exp, [pb], [eb], scale=-1.0)
                        ACT(lt[:, tt, hf * 512:(hf + 1) * 512], e_[:], AF.Ln, [eb], [ltb[tt]], bias=1.0)
                for fc in range(8):
                    ps, pb = psum((2, 3))
                    for tt in range(4):
                        MM(ps[:, tt * 128:(tt + 1) * 128], lt[:, tt, fc * 128:(fc + 1) * 128], tris[:], True, True, [ltb[tt], cstb], [pb])
                    ACT(Eq[:, fc, :], ps[:], AF.Exp, [pb], [Eqb[fc]])
                    Ei, Eib = Ei_r.next()
                    ACT(Ei[:], ps[:], AF.Exp, [pb], [Eib], scale=-1.0)
                    qt, qtb = q_r.next()
                    kt, ktb = k_r.next()
                    S.dma(qt[:], s_qT[fc * 128:(fc + 1) * 128, t0:t0 + TG], w=[qtb])
                    S.dma(kt[:], s_kT[fc * 128:(fc + 1) * 128, t0:t0 + TG], w=[ktb])
                    STT("dve", qdT[:, fc, :], qt[:], 0.0625, Eq[:, fc, :], ALU.mult, ALU.mult, [qtb, Eqb[fc]], [qdb[fc]])
                    TT("pool", kiT[:, fc, :], kt[:], Ei[:], ALU.mult, [ktb, Eib], [kib[fc]])
                    for c8 in range(8):
                        cs = slice(c8 * 64, (c8 + 1) * 64)
                        STT("dve", keT[:, fc, cs], kt[:, cs], Eq[:, fc, c8 * 64 + 63:c8 * 64 + 64], Ei[:, cs],
                            ALU.mult, ALU.mult, [ktb, Eqb[fc], Eib], [keb[fc]])
                for tt in range(4):
                    ps, pb = psum((2, 3))
                    psv = ps[:].bitcast(BF16)
                    for fc in range(8):
                        TR(psv[:, fc * 128:(fc + 1) * 128], keT[:, fc, tt * 128:(tt + 1) * 128], identb[:], [keb[fc], cstb], [pb])
                    CP("act", ketok[:, tt, :], psv[:, :1024], [pb], [ketokb[tt]])
                for tt in range(4):
                    ts_ = slice(tt * 128, (tt + 1) * 128)
                    ps, pb = psum((2, 3))
                    for h in range(4):
                        for c in range(2):
                            MM(ps[:, h * 128:(h + 1) * 128], kiT[:, 2 * h + c, ts_], qdT[:, 2 * h + c, ts_], c == 0, c == 1,
                               [kib[2 * h + c], qdb[2 * h + c]], [pb])
                    att, attb = att_r.next()
                    TT("dve", att[:], ps[:], am4[:], ALU.mult, [pb, am4b], [attb])
                    po = [(PS[0], PSB[0]), (PS[1], PSB[1])]
                    for ch in range(2):
                        c8 = tt * 2 + ch
                        r0 = ch * 64
                        rs = slice(r0, r0 + 64)
                        cs = slice(tt * 128 + r0, tt * 128 + r0 + 64)
                        for h in range(4):
                            pso, pob = po[h // 2]
                            oc = slice((h % 2) * 256, (h % 2) * 256 + 256)
                            MM(pso[rs, oc], qdT[:, 2 * h, cs], Sb_[:, 0, h, :], True, False, [qdb[2 * h], Sbb], [pob])
                            MM(pso[rs, oc], qdT[:, 2 * h + 1, cs], Sb_[:, 1, h, :], False, False, [qdb[2 * h + 1], Sbb], [pob])
                            MM(pso[rs, oc], att[rs, h * 128 + r0:h * 128 + r0 + 64], gv[rs, tt, h * 256:(h + 1) * 256], False, True,
                               [attb, gvb], [pob])
                        pu = [(PS[4 + h_], PSB[4 + h_]) for h_ in range(4)]
                        for h in range(4):
                            for c in range(2):
                                psu, pub = pu[h]
                                MM(psu[:, c * 256:(c + 1) * 256], ketok[rs, tt, (2 * h + c) * 128:(2 * h + c + 1) * 128],
                                   gv[rs, tt, h * 256:(h + 1) * 256], True, True, [ketokb[tt], gvb], [pub])
                        for h in range(4):
                            for c in range(2):
                                psu, pub = pu[h]
                                STT("dve", Sf[:, c, h, :], Sf[:, c, h, :], Eq[:, 2 * h + c, c8 * 64 + 63:c8 * 64 + 64],
                                    psu[:, c * 256:(c + 1) * 256], ALU.mult, ALU.add, [Sfb, Eqb[2 * h + c], pub], [Sfb])
                        CP("act", Sb_[:], Sf[:], [Sfb], [Sbb])
                    sm, smb = sm_r.next()
                    for hh in range(2):
                        pso, pob = po[hh]
                        ACT(junk[:, hh * 512:(hh + 1) * 512], pso[:], AF.Square, [pob], [junkb])
                    RSUM("dve", sm[:, 0:4], junk[:].rearrange("p (h d) -> p h d", d=256), [junkb], [smb])
                    ACT(sm[:, 4:8], sm[:, 0:4], AF.Sqrt, [smb], [smb], scale=1.0 / 256, bias=EPS)
                    RECIP(sm[:, 8:12], sm[:, 4:8], [smb], [smb])
                    for h in range(4):
                        pso, pob = po[h // 2]
                        oc = slice((h % 2) * 256, (h % 2) * 256 + 256)
                        STT("dve", ytmp[:, h * 256:(h + 1) * 256], pso[:, oc], sm[:, 8 + h:9 + h], gng[:], ALU.mult, ALU.mult,
                            [pob, smb, gngb], [ytmpb])
                    yb_, ybb = ybf_r.next()
                    TT("pool", yb_[:], ytmp[:], gr[:, tt, :], ALU.mult, [ytmpb, grb], [ybb])
                    transpose_rows(yb_, ybb, yTg, yTgb, tt * 128, nk=8, pool=(2, 3))
                S.dma(s_yT[0].rearrange("(k p) t -> p k t", p=128)[:, :, t0:t0 + TG], yTg[:], r=[yTgb])
            S.barrier()
            S.flush()

    def phase_ssd(l):
        TG = 512

        def bl(ap2, k):
            return ap2.unsqueeze(2).to_broadcast([128, ap2.shape[1], k])

        def bm(ap2, n):
            return ap2.unsqueeze(1).to_broadcast([128, n, ap2.shape[1]])

        def v3(ap2, k):
            return ap2.rearrange("p (n k) -> p n k", k=k)

        with ExitStack() as es:
            cw = sb(es, "s_cw", [128, 16, 4], F32)
            cbias = sb(es, "s_cb", [128, 16], F32)
            prm = sb(es, "s_prm", [128, 4, 16], F32)
            gn = sb(es, "s_gn", [128, 1024], F32)
            pb_ = Buf()
            for k in range(4):
                S.dma(cw[:, :, k], Wd["ssm_conv_w"][l, k].rearrange("(c p) -> p c", p=128), w=[pb_], slow=True)
            S.dma(cbias[:], Wd["ssm_conv_b"][l].rearrange("(c p) -> p c", p=128), w=[pb_], slow=True)
            S.dma(prm[:, 0, :], Wd["ssm_dt_bias"][l:l + 1, :].partition_broadcast(128), w=[pb_])
            S.dma(prm[:, 1, :], Wd["ssm_a_log"][l:l + 1, :].partition_broadcast(128), w=[pb_])
            S.dma(prm[:, 2, :], Wd["ssm_d"][l:l + 1, :].partition_broadcast(128), w=[pb_])
            S.dma(gn[:], Wd["ssm_norm"][l:l + 1, :].partition_broadcast(128), w=[pb_])
            ACT(prm[:, 3, :], prm[:, 1, :], AF.Exp, [pb_], [pb_])
            TSC("dve", prm[:, 1, :], prm[:, 3, :], -1.0, None, ALU.mult, None, [pb_], [pb_])
            xin_r = Ring([sb(es, "s_xin%d" % i, [128, TG + 3], F32) for i in range(2)])
            acc_r = Ring([sb(es, "s_acc%d" % i, [128, TG], F32) for i in range(2)])
            xsT = sb(es, "s_xsT", [128, 8, TG], F32)
            xsTb = [Buf() for _ in range(8)]
            BTf = sb(es, "s_BTf", [128, 4, TG], F32)
            BTfb = [Buf() for _ in range(4)]
            BT = sb(es, "s_BT", [128, 4, TG], BF16)
            CT = sb(es, "s_CT", [128, 4, TG], BF16)
            BTb = [Buf() for _ in range(4)]
            CTb = [Buf() for _ in range(4)]
            xs_r = Ring([sb(es, "s_xs%d" % i, [128, 1024], F32) for i in range(2)])
            bt_r = Ring([sb(es, "s_bt%d" % i, [128, 512], BF16) for i in range(2)])
            sz_r = Ring([sb(es, "s_sz%d" % i, [128, 1024], F32) for i in range(2)])
            dt_r = Ring([sb(es, "s_dt%d" % i, [128, 8, 16], F32) for i in range(3)])
            ecl_r = Ring([sb(es, "s_ecl%d" % i, [128, 32], F32) for i in range(2)])
            xdt_r = Ring([sb(es, "s_xdt%d" % i, [128, 1024], BF16) for i in range(2)])
            xde_r = Ring([sb(es, "s_xde%d" % i, [128, 1024], BF16) for i in range(2)])
            cbm_r = Ring([sb(es, "s_cbm%d" % i, [128, 128], F32) for i in range(2)])
            dg_r = Ring([sb(es, "s_dg%d" % i, [128, 512], F32) for i in range(2)])
            tt_r = Ring([sb(es, "s_t%d" % i, [128, 512], F32) for i in range(2)])
            w_r = Ring([sb(es, "s_w%d" % i, [128, 512], BF16) for i in range(2)])
            STf = sb(es, "s_STf", [128, 4, 256], F32)
            STb = sb(es, "s_STb", [128, 4, 256], BF16)
            STfb, STbb = Buf(), Buf()
            MSET("dve", STf[:], 0.0, [STfb])
            MSET("pool", STb[:], 0.0, [STbb])
            yi = sb(es, "s_yi", [128, 1024], F32)
            yib = Buf()
            y2 = sb(es, "s_y2", [128, 1024], F32)
            y2b = Buf()
            junk = sb(es, "s_junk", [128, 1024], F32)
            junkb = Buf()
            ybf_r = Ring([sb(es, "s_ybf%d" % i, [128, 1024], BF16) for i in range(2)])
            yTg = sb(es, "s_yTg", [128, 8, TG], BF16)
            yTgb = Buf()
            sm_r = Ring([sb(es, "s_sm%d" % i, [128, 12], F32) for i in range(4)])
            tri1, blk, chsel, mneg4, onesf = cst["c_tri1"], cst["c_blk"], cst["c_chsel"], cst["c_mneg4"], cst["c_onesf"]
            amask = cst["c_amask"]
            for g in range(T // TG):
                t0 = g * TG
                for c in range(16):
                    xin, xinb = xin_r.next()
                    if g == 0:
                        MSET("pool", xin[:, 0:3], 0.0, [xinb])
                        S.dma(xin[:, 3:], s_sxT[c * 128:(c + 1) * 128, 0:TG], w=[xinb])
                    else:
                        S.dma(xin[:], s_sxT[c * 128:(c + 1) * 128, t0 - 3:t0 + TG], w=[xinb])
                    acc, accb = acc_r.next()
                    TSC("dve", acc[:], xin[:, 3:3 + TG], cw[:, c, 3:4], None, ALU.mult, None, [xinb, pb_], [accb])
                    for k in range(3):
                        STT("dve", acc[:], xin[:, k:k + TG], cw[:, c, k:k + 1], acc[:], ALU.mult, ALU.add, [xinb, pb_, accb], [accb])
                    if c < 8:
                        ACT(xsT[:, c, :], acc[:], AF.Silu, [accb, pb_], [xsTb[c]], bias=cbias[:, c:c + 1])
                    elif c < 12:
                        ACT(BTf[:, c - 8, :], acc[:], AF.Silu, [accb, pb_], [BTfb[c - 8]], bias=cbias[:, c:c + 1])
                        CP("pool", BT[:, c - 8, :], BTf[:, c - 8, :], [BTfb[c - 8]], [BTb[c - 8]])
                    else:
                        ACT(CT[:, c - 12, :], acc[:], AF.Silu, [accb, pb_], [CTb[c - 12]], bias=cbias[:, c:c + 1])
                for tt in range(4):
                    tsl = slice(tt * 128, (tt + 1) * 128)
                    tok0 = t0 + tt * 128
                    xs, xsb = xs_r.next()
                    for half in range(2):
                        ps, pb = psum((1,))
                        for k in range(4):
                            c = half * 4 + k
                            TR(ps[:, k * 128:(k + 1) * 128], xsT[:, c, tsl], identf[:], [xsTb[c], cstb], [pb])
                        CP("act", xs[:, half * 512:(half + 1) * 512], ps[:], [pb], [xsb])
                    btok, btokb = bt_r.next()
                    ps, pb = psum((1,))
                    for k in range(4):
                        TR(ps[:, k * 128:(k + 1) * 128], BTf[:, k, tsl], identf[:], [BTfb[k], cstb], [pb])
                    CP("act", btok[:], ps[:], [pb], [btokb])
                    sz, szb = sz_r.next()
                    S.dma(sz[:], s_sz[tok0:tok0 + 128, :], w=[szb])
                    dtt, dtb = dt_r.next()
                    S.dma(dtt[:, 0, :], s_sdt[tok0:tok0 + 128, :], w=[dtb])
                    TT("dve", dtt[:, 6, :], dtt[:, 0, :], prm[:, 0, :], ALU.add, [dtb, pb_], [dtb])
                    ACT(dtt[:, 6, :], dtt[:, 6, :], AF.Exp, [dtb], [dtb])
                    ACT(dtt[:, 1, :], dtt[:, 6, :], AF.Ln, [dtb], [dtb], bias=1.0)
                    TT("dve", dtt[:, 2, :], dtt[:, 1, :], prm[:, 1, :], ALU.mult, [dtb, pb_], [dtb])
                    ps, pb = psum((0,))
                    MM(ps[:, 0:16], tri1[:], dtt[:, 2, :], True, True, [cstb, dtb], [pb])
                    MM(ps[:, 16:32], blk[:], dtt[:, 2, :], True, True, [cstb, dtb], [pb])
                    MM(ps[:, 32:48], chsel[:, 0:128], dtt[:, 2, :], True, True, [cstb, dtb], [pb])
                    MM(ps[:, 48:64], chsel[:, 128:256], dtt[:, 2, :], True, True, [cstb, dtb], [pb])
                    CP("dve", dtt[:, 3, :], ps[:, 0:16], [pb], [dtb])
                    ACT(dtt[:, 4, :], ps[:, 0:16], AF.Exp, [pb], [dtb])
                    TT("dve", dtt[:, 6, :], ps[:, 16:32], dtt[:, 3, :], ALU.subtract, [pb, dtb], [dtb])
                    ACT(dtt[:, 5, :], dtt[:, 6, :], AF.Exp, [dtb], [dtb])
                    ecl, eclb = ecl_r.next()
                    ACT(ecl[:], ps[:, 32:64], AF.Exp, [pb], [eclb])
                    xdt, xdtb = xdt_r.next()
                    xde, xdeb = xde_r.next()
                    TT("dve", dtt[:, 7, :], dtt[:, 1, :], dtt[:, 5, :], ALU.mult, [dtb], [dtb])
                    TT("dve", v3(xdt[:], 64), v3(xs[:], 64), bl(dtt[:, 1, :], 64), ALU.mult, [xsb, dtb], [xdtb])
                    TT("pool", v3(xde[:], 64), v3(xs[:], 64), bl(dtt[:, 7, :], 64), ALU.mult, [xsb, dtb], [xdeb])
                    psY = [(PS[3], PSB[3]), (PS[4], PSB[4])]
                    psI = [(PS[5], PSB[5]), (PS[6], PSB[6])]
                    for gq in range(4):
                        ps, pb = psum((1,))
                        MM(ps[:, 0:128], BT[:, gq, tsl], CT[:, gq, tsl], True, True, [BTb[gq], CTb[gq]], [pb])
                        cbm, cbmb = cbm_r.next()
                        TT("dve", cbm[:], ps[:, 0:128], amask[:], ALU.mult, [pb, cstb], [cbmb])
                        dg, dgb = dg_r.next()
                        TT("pool", v3(dg[:], 128), bm(identf[:], 4), bl(dtt[:, 3, gq * 4:gq * 4 + 4], 128), ALU.mult, [cstb, dtb], [dgb])
                        ps2, pb2 = psum((2,))
                        MM(ps2[:], onesf[:], dg[:], True, False, [cstb, dgb], [pb2])
                        MM(ps2[:], identf[:], mneg4[:], False, True, [cstb], [pb2])
                        tq, tqb = tt_r.next()
                        TT("dve", v3(tq[:], 128), v3(ps2[:], 128), bl(dtt[:, 3, gq * 4:gq * 4 + 4], 128), ALU.subtract, [pb2, dtb], [tqb])
                        ACT(tq[:], tq[:], AF.Exp, [tqb], [tqb])
                        wq, wqb = w_r.next()
                        TT("pool", v3(wq[:], 128), v3(tq[:], 128), bm(cbm[:], 4), ALU.mult, [tqb, cbmb], [wqb])
                        py, pyb = psY[gq // 2]
                        for hq in range(4):
                            h = gq * 4 + hq
                            oc = slice((gq % 2) * 256 + hq * 64, (gq % 2) * 256 + hq * 64 + 64)
                            MM(py[:, oc], wq[:, hq * 128:(hq + 1) * 128], xdt[:, h * 64:(h + 1) * 64], True, True, [wqb, xdtb], [pyb])
                    for ch in range(2):
                        r0 = ch * 64
                        rs = slice(r0, r0 + 64)
                        cs = slice(tt * 128 + r0, tt * 128 + r0 + 64)
                        for gq in range(4):
                            pi, pib = psI[gq // 2]
                            MM(pi[rs, (gq % 2) * 256:(gq % 2) * 256 + 256], CT[:, gq, cs], STb[:, gq, :], True, True, [CTb[gq], STbb], [pib])
                        for gq in range(4):
                            pS, pSb = psum((7, 0))
                            MM(pS[:, 0:256], btok[rs, gq * 128:(gq + 1) * 128], xde[rs, gq * 256:(gq + 1) * 256], True, True, [btokb, xdeb], [pSb])
                            TT("pool", v3(STf[:, gq, :], 64), v3(STf[:, gq, :], 64), bl(ecl[:, ch * 16 + gq * 4:ch * 16 + gq * 4 + 4], 64), ALU.mult,
                               [STfb, eclb], [STfb])
                            TT("dve", STf[:, gq, :], STf[:, gq, :], pS[:, 0:256], ALU.add, [STfb, pSb], [STfb])
                        CP("act", STb[:], STf[:], [STfb], [STbb])
                    for hh in range(2):
                        py, pyb = psY[hh]
                        CP("act", yi[:, hh * 512:(hh + 1) * 512], py[:], [pyb], [yib])
                    for hh in range(2):
                        pi, pib = psI[hh]
                        TT("dve", v3(y2[:, hh * 512:(hh + 1) * 512], 64), v3(pi[:], 64), bl(dtt[:, 4, hh * 8:hh * 8 + 8], 64), ALU.mult,
                           [pib, dtb], [y2b])
                    TT("pool", y2[:], y2[:], yi[:], ALU.add, [y2b, yib], [y2b])
                    TT("dve", v3(junk[:], 64), v3(xs[:], 64), bl(prm[:, 2, :], 64), ALU.mult, [xsb, pb_], [junkb])
                    TT("pool", y2[:], y2[:], junk[:], ALU.add, [y2b, junkb], [y2b])
                    TT("pool", y2[:], y2[:], sz[:], ALU.mult, [y2b, szb], [y2b])
                    sm, smb = sm_r.next()
                    ACT(junk[:], y2[:], AF.Square, [y2b], [junkb])
                    RSUM("dve", sm[:, 0:4], junk[:].rearrange("p (h d) -> p h d", d=256), [junkb], [smb])
                    ACT(sm[:, 4:8], sm[:, 0:4], AF.Sqrt, [smb], [smb], scale=1.0 / 256, bias=EPS)
                    RECIP(sm[:, 8:12], sm[:, 4:8], [smb], [smb])
                    yb_, ybb = ybf_r.next()
                    for gq in range(4):
                        gs = slice(gq * 256, (gq + 1) * 256)
                        STT("dve", yb_[:, gs], y2[:, gs], sm[:, 8 + gq:9 + gq], gn[:, gs], ALU.mult, ALU.mult, [y2b, smb, pb_], [ybb])
                    transpose_rows(yb_, ybb, yTg, yTgb, tt * 128, nk=8, pool=(1,))
                S.dma(s_yT[2].rearrange("(k p) t -> p k t", p=128)[:, :, t0:t0 + TG], yTg[:], r=[yTgb])
            S.barrier()
            S.flush()

    s_ocmp = dscr("s_ocmp", (1024, T), F32)
    NQG = T // 512
    NCMP = T // 16 - 1
    NTC = (NCMP + 127) // 128
    CM_C = [31, -481, -993, -1505, -2017]

    def phase_nsa(l):
        with ExitStack() as es:
            kb = Buf()
            E = sb(es, "n_E", [128, T], BF16)
            S.dma(E[:], Cd["c_e"][:, :], w=[kb])
            wm = sb(es, "n_wm", [128, 8 * 512], BF16)
            S.dma(wm[:], Cd["c_wm"][:, :], w=[kb])
            cm = sb(es, "n_cm", [128, 5 * 512], BF16)
            S.dma(cm[:], Cd["c_cm"][:, :], w=[kb])
            kbsw = sb(es, "n_kbsw", [128, 16 * 68], F32)
            S.dma(kbsw[:], Cd["c_kbsw"][:, :], w=[kb])
            kbc = sb(es, "n_kbc", [128, 1024], F32)
            S.dma(kbc[:], Cd["c_kbc"][:, :], w=[kb])
            ov = sb(es, "n_ov", [128, 4, 129], BF16)
            S.dma(ov[:], Cd["c_ov"].rearrange("p (n c) -> p n c", c=129), w=[kb])
            wdt = sb(es, "n_wd", [128, 254], F32)
            S.dma(wdt[:], Cd["c_wd"][:, :], w=[kb])
            onesf = cst["c_onesf"]
            w1s = sb(es, "n_w1", [128, 2, 16, 64], BF16)
            w2s = sb(es, "n_w2", [64, 2, 64], BF16)
            posf = sb(es, "n_posf", [128, 2, 16], F32)
            posb = sb(es, "n_posb", [128, 2, 16], BF16)
            ccon = sb(es, "n_ccon", [64, 2], F32)
            for j in range(2):
                S.dma(w1s[:, j], Wd["nsa_cmp_w1"][l, j].rearrange("(c p) o -> p c o", p=128), w=[kb], q="pool")
                S.dma(w2s[:, j, :], Wd["nsa_cmp_w2"][l, j], w=[kb], q="pool")
                S.dma(posf[:, j, :], Wd["nsa_cmp_pos"][l, j].rearrange("(c two) d -> (two d) c", two=2), w=[kb], slow=True)
            CP("dve", posb[:], posf[:], [kb], [kb])
            for j in range(2):
                ps, pb = psum((7,))
                for c in range(16):
                    MM(ps[:64, 0:1], w1s[:, j, c, :], posb[:, j, c:c + 1], c == 0, c == 15, [kb], [pb])
                CP("dve", ccon[:, j:j + 1], ps[:64, 0:1], [pb], [kb])
            AT = sb(es, "n_AT", [64, 512], BF16)
            ATb = Buf()
            Kc = sb(es, "n_Kc", [67, 512], BF16)
            Vc = sb(es, "n_Vc", [128, 4, 65], BF16)
            Ks = sb(es, "n_Ks", [67, T], BF16)
            Kw = sb(es, "n_Kw", [67, T], BF16)
            Vs = sb(es, "n_Vs", [128, NT, 65], BF16)
            Vw = sb(es, "n_Vw", [128, NT, 65], BF16)
            kvb = Buf()
            selT = sb(es, "n_selT", [128, T], BF16)
            selTb = Buf()
            kcs2, kcs2b = selT, selTb
            impacc = sb(es, "n_imp", [128, 4, 128], F32)
            impb = [Buf() for _ in range(4)]
            q_r = Ring([sb(es, "n_q%d" % i, [67, 512], BF16) for i in range(6)])
            p_r = Ring([sb(es, "n_p%d" % i, [128, 512], BF16) for i in range(16)])
            osb_r = Ring([sb(es, "n_osb%d" % i, [65, 512], F32) for i in range(6)])
            fr_r = Ring([sb(es, "n_fr%d" % i, [65, 512], F32) for i in range(6)])
            gt_r = Ring([sb(es, "n_gt%d" % i, [65, 1, 512], F32) for i in range(6)])
            accy_r = Ring([sb(es, "n_acc%d" % i, [64, 512], F32) for i in range(8)])
            ctr_r = Ring([sb(es, "n_ctr%d" % i, [64, 512], F32) for i in range(4)])
            ybf_r = Ring([sb(es, "n_yb%d" % i, [64, 512], BF16) for i in range(3)])
            sm_r = Ring([sb(es, "n_sm%d" % i, [128, 20], F32) for i in range(4)])
            ti_r = Ring([sb(es, "n_ti%d" % i, [128, 128], F32) for i in range(4)])
            sb_r = Ring([sb(es, "n_sb%d" % i, [128, 128], BF16) for i in range(4)])

            def load_q(h, qg):
                qt, qtb = q_r.next()
                S.dma(qt[0:64, :], s_nqT[h * 64:(h + 1) * 64, qg * 512:(qg + 1) * 512], w=[qtb])
                S.dma(qt[64:67, :], Cd["c_qa"][h, :, qg * 512:(qg + 1) * 512], w=[qtb])
                return qt, qtb

            def load_g(h, qg, brs):
                gt, gtb = gt_r.next()
                for k_, br in enumerate(brs):
                    S.dma(gt[64:65, k_, :], s_ngT[br * 16 + h:br * 16 + h + 1, qg * 512:(qg + 1) * 512], w=[gtb])
                return gt, gtb

            pend = []
            LAG = 5
            SPOOL = [(4, 5, 6)]

            def unit_back():
                it = pend.pop(0)
                if callable(it):
                    it()
                    return
                Vap, pt, ptb, pso, psob, first, last = it
                MM(pso[0:65, :], Vap, pt[:], first, last, [kvb, ptb], [psob])

            def flush_units():
                while pend:
                    unit_back()

            def tile_unit(qt, qtb, Kap, Vap, bias_ap, extras, pso, psob, first, last):
                ps, pb = psum(SPOOL[0])
                MM(ps[:], Kap, qt[:], True, len(extras) == 0, [kvb, qtb], [pb])
                for ie, (lt_, rt_, bufs_) in enumerate(extras):
                    MM(ps[:], lt_, rt_, False, ie == len(extras) - 1, bufs_, [pb])
                pt, ptb = p_r.next()
                ACT(pt[:], ps[:], AF.Exp, [pb, kb], [ptb], bias=bias_ap)
                pend.append((Vap, pt, ptb, pso, psob, first, last))
                while len(pend) > LAG:
                    unit_back()
                return pt, ptb

            def fin_front(pso, psob, gt, gtb, gslot):
                osb, osbb = osb_r.next()
                CP("act", osb[:], pso[0:65, :], [psob], [osbb])
                fr, frb = fr_r.next()
                TSC("dve", fr[64:65, :], osb[64:65, :], 1e-30, None, ALU.max, None, [osbb], [frb])
                RECIP(fr[64:65, :], fr[64:65, :], [frb], [frb])
                TT("dve", fr[64:65, :], fr[64:65, :], gt[64:65, gslot, :], ALU.mult, [frb, gtb], [frb])
                return osb, osbb, fr, frb

            def fin_back(rec):
                osb, osbb, fr, frb, kind, tgt = rec
                psb_, psbb = psum((7,))
                MM(psb_[0:64, :], onesf[64:65, 0:64], fr[64:65, :], True, True, [cstb, frb], [psbb])
                if kind == "cmp":
                    h, q0 = tgt
                    ctr, ctrb = ctr_r.next()
                    TT("dve", ctr[:], osb[0:64, :], psb_[0:64, :], ALU.mult, [osbb, psbb], [ctrb])
                    S.dma(s_ocmp[h * 64:(h + 1) * 64, q0:q0 + 512], ctr[:], r=[ctrb])
                else:
                    ac, acb, store = tgt
                    ctr, ctrb = ctr_r.next()
                    TT("dve", ctr[:], osb[0:64, :], psb_[0:64, :], ALU.mult, [osbb, psbb], [ctrb])
                    TT("pool", ac[:], ac[:], ctr[:], ALU.add, [acb, ctrb], [acb])
                    if store is not None:
                        h, q0 = store
                        yb_, ybb = ybf_r.next()
                        CP("pool", yb_[:], ac[:], [acb], [ybb])
                        S.dma(s_yT[1][h * 64:(h + 1) * 64, q0:q0 + 512], yb_[:], r=[ybb])

            for gi in range(2):
                MSET("dve", Ks[:], 1.0, [kvb])
                MSET("pool", Kw[:], 1.0, [kvb])
                MSET("dve", Vs[:], 1.0, [kvb])
                MSET("pool", Vw[:], 1.0, [kvb])
                MSET("dve", Kc[:], 1.0, [kvb])
                MSET("pool", Vc[:], 1.0, [kvb])
                S.dma(Ks[0:64, :], s_nkvT[256 + gi * 64:256 + (gi + 1) * 64, :], w=[kvb])
                S.dma(Kw[0:64, :], s_nkvT[512 + gi * 64:512 + (gi + 1) * 64, :], w=[kvb])
                nkv_v = s_nkv.rearrange("(n p) c -> p n c", p=128)
                S.dma(Vs[:, :, 0:64], nkv_v[:, :, 384 + gi * 64:384 + (gi + 1) * 64], w=[kvb])
                S.dma(Vw[:, :, 0:64], nkv_v[:, :, 640 + gi * 64:640 + (gi + 1) * 64], w=[kvb])
                for j in range(2):
                    MSET("dve", kcs2[:], 0.0, [kcs2b])
                    S.dma(kcs2[0:64, :], s_nkvT[j * 128 + gi * 64:j * 128 + (gi + 1) * 64, :], w=[kcs2b])
                    S.dma(kcs2[64:128, 0:T - 1], s_nkvT[j * 128 + gi * 64:j * 128 + (gi + 1) * 64, 1:T], w=[kcs2b])
                    kv_ = kcs2[:].rearrange("p (n s) -> p n s", s=16)
                    ps, pb = psum((7,))
                    for c in range(16):
                        o_ = 2 * c
                        rhs = kv_[:, o_ // 16:o_ // 16 + NCMP, o_ % 16]
                        MM(ps[:64, 0:NCMP], w1s[:, j, c, :], rhs, c == 0, c == 15, [kb, kcs2b], [pb])
                    MSET("dve", AT[:], 0.0, [ATb])
                    ACT(AT[:, 0:NCMP], ps[:64, 0:NCMP], AF.Silu, [pb, kb], [ATb], bias=ccon[:, j:j + 1])
                    if j == 0:
                        ps2, pb2 = psum((7,))
                        MM(ps2[:64, 0:NCMP], w2s[:, 0, :], AT[:, 0:NCMP], True, True, [kb, ATb], [pb2])
                        CP("act", Kc[0:64, 0:NCMP], ps2[:64, 0:NCMP], [pb2], [kvb])
                    else:
                        for nt in range(NTC):
                            ps2, pb2 = psum((7,))
                            MM(ps2[:, 0:64], AT[:, nt * 128:(nt + 1) * 128], w2s[:, 1, :], True, True, [ATb, kb], [pb2])
                            CP("act", Vc[:, nt, 0:64], ps2[:, 0:64], [pb2], [kvb])
                SPOOL[0] = (4, 5)
                prevs = [[]]
                bi = 0
                for qg in range(NQG):
                    q0 = qg * 512
                    for j4 in range(4):
                        MSET("pool", impacc[:, j4, :], 0.0, [impb[j4]])
                    nts = [nt for nt in range(NTC) if 16 * (128 * nt) + 31 <= q0 + 511]
                    for hp in range(4):
                        heads = [gi * 8 + hp * 2 + i for i in range(2)]
                        qs = [load_q(h, qg) for h in heads]
                        accb = [(PS[(bi % 2) * 2 + i], PSB[(bi % 2) * 2 + i]) for i in range(2)]
                        bi += 1
                        pts = [[], []]
                        for i_, nt in enumerate(nts):
                            full = 16 * (128 * nt + 127) + 31 <= q0
                            extras = []
                            if not full:
                                mi = CM_C.index(2048 * nt + 31 - q0)
                                extras = [(identb[:], cm[:, mi * 512:(mi + 1) * 512], [cstb, kb])]
                            for i2, h in enumerate(heads):
                                bias_ap = kbc[:, (h * 16 + qg) * 4 + nt:(h * 16 + qg) * 4 + nt + 1]
                                pt, ptb = tile_unit(qs[i2][0], qs[i2][1], Kc[:, nt * 128:(nt + 1) * 128], Vc[:, nt, :], bias_ap, extras,
                                                    accb[i2][0], accb[i2][1], i_ == 0, i_ == len(nts) - 1)
                                pts[i2].append((nt, pt, ptb))
                        def after_a(heads=heads, accb=accb, pts=pts, q0=q0, qg=qg):
                            gs = [load_g(h, qg, (0,)) for h in heads]
                            cur = []
                            for i2, h in enumerate(heads):
                                osb, osbb, fr, frb = fin_front(accb[i2][0], accb[i2][1], gs[i2][0], gs[i2][1], 0)
                                cur.append((osb, osbb, fr, frb, "cmp", (h, q0)))
                            for i2, h in enumerate(heads):
                                for j4 in range(4):
                                    psi, psib = psum((6, 7))
                                    for i_, (nt, pt, ptb) in enumerate(pts[i2]):
                                        MM(psi[:, 0:129], pt[:, j4 * 128:(j4 + 1) * 128], ov[:, nt, :], i_ == 0, i_ == len(pts[i2]) - 1,
                                           [ptb, kb], [psib])
                                    sm, smb = sm_r.next()
                                    TSC("dve", sm[:, 0:1], psi[:, 128:129], 1e-30, None, ALU.max, None, [psib], [smb])
                                    RECIP(sm[:, 1:2], sm[:, 0:1], [smb], [smb])
                                    STT("dve", impacc[:, j4, :], psi[:, 0:128], sm[:, 1:2], impacc[:, j4, :], ALU.mult, ALU.add,
                                        [psib, smb, impb[j4]], [impb[j4]])
                            for rec in prevs[0]:
                                fin_back(rec)
                            prevs[0] = cur
                        pend.append(after_a)
                    flush_units()
                    for j4 in range(4):
                        qt_ = qg * 4 + j4
                        ti, tib = ti_r.next()
                        TT("dve", ti[:], impacc[:, j4, :], wdt[:, 126 - 2 * qt_:254 - 2 * qt_], ALU.add, [impb[j4], kb], [tib])
                        MSET("dve", ti[:, 0:1], 4e30, [tib])
                        sm, smb = sm_r.next()
                        S.dve(lambda e, o=sm[:, 0:8], i=ti[:]: e.max(out=o, in_=i), [tib], [smb])
                        t2, t2b = ti_r.next()
                        S.dve(lambda e, o=t2[:], r_=sm[:, 0:8], i=ti[:]: e.match_replace(out=o, in_to_replace=r_, in_values=i, imm_value=-3e38),
                              [tib, smb], [t2b])
                        S.dve(lambda e, o=sm[:, 8:16], i=t2[:]: e.max(out=o, in_=i), [t2b], [smb])
                        sbt, sbtb = sb_r.next()
                        TSC("dve", sbt[:], ti[:], sm[:, 15:16], None, ALU.is_ge, None, [tib, smb], [sbtb])
                        sb2, sb2b = sb_r.next()
                        TSC("dve", sb2[:], sbt[:], -1.0, 30000.0, ALU.add, ALU.mult, [sbtb], [sb2b])
                        ps, pb = psum((6, 7))
                        psv = ps[:].bitcast(BF16)
                        TR(psv[:, 0:128], sb2[:], identb[:], [sb2b, cstb], [pb])
                        CP("act", selT[:, qt_ * 128:(qt_ + 1) * 128], psv[:, 0:128], [pb], [selTb])
                flush_units()
                for rec in prevs[0]:
                    fin_back(rec)
                prevs[0] = []
                if "s_selT" in dbg:
                    S.dma(s_selT[gi], selT[:], r=[selTb])
                SPOOL[0] = (4, 5, 6)
                for qg in range(NQG):
                    q0 = qg * 512
                    for hp in range(4):
                        heads = [gi * 8 + hp * 2 + i for i in range(2)]
                        qs = [load_q(h, qg) for h in heads]
                        accs = []
                        for h in heads:
                            ac, acb = accy_r.next()
                            S.dma(ac[:], s_ocmp[h * 64:(h + 1) * 64, q0:q0 + 512], w=[acb])
                            accs.append((ac, acb))
                        for br in (1, 2):
                            if br == 1:
                                kts = list(range(0, min(4 * qg + 4, NT)))
                            else:
                                kts = [kt for kt in range(4 * qg - 4, 4 * qg + 4) if 0 <= kt < NT]
                            accb = [(PS[(bi % 2) * 2 + i], PSB[(bi % 2) * 2 + i]) for i in range(2)]
                            bi += 1
                            for i_, kt in enumerate(kts):
                                m = kt - 4 * qg
                                extras = []
                                if br == 1:
                                    extras.append((E[:, kt * 128:(kt + 1) * 128], selT[:, q0:q0 + 512], [kb, selTb]))
                                    if m >= 0:
                                        extras.append((identb[:], wm[:, (m + 4) * 512:(m + 5) * 512], [cstb, kb]))
                                    Kt, Vt = Ks, Vs
                                else:
                                    extras.append((identb[:], wm[:, (m + 4) * 512:(m + 5) * 512], [cstb, kb]))
                                    Kt, Vt = Kw, Vw
                                for i2, h in enumerate(heads):
                                    bias_ap = kbsw[:, h * 68 + m + 64:h * 68 + m + 65]
                                    tile_unit(qs[i2][0], qs[i2][1], Kt[:, kt * 128:(kt + 1) * 128], Vt[:, kt, :], bias_ap, extras,
                                              accb[i2][0], accb[i2][1], i_ == 0, i_ == len(kts) - 1)
                            def after_c(heads=heads, accb=accb, accs=accs, br=br, q0=q0, qg=qg):
                                gs = [load_g(h, qg, (br,)) for h in heads]
                                cur = []
                                for i2, h in enumerate(heads):
                                    osb, osbb, fr, frb = fin_front(accb[i2][0], accb[i2][1], gs[i2][0], gs[i2][1], 0)
                                    cur.append((osb, osbb, fr, frb, "acc", (accs[i2][0], accs[i2][1], (h, q0) if br == 2 else None)))
                                for rec in prevs[0]:
                                    fin_back(rec)
                                prevs[0] = cur
                            pend.append(after_c)
                flush_units()
                for rec in prevs[0]:
                    fin_back(rec)
                prevs[0] = []
                S.barrier()
                S.flush()

    PHASES = {"cast": phase_cast, "p1": phase_p1, "p2": phase_p2, "gla": phase_gla, "ssd": phase_ssd, "nsa": phase_nsa}
    return nc, S, PHASES, es0


_CACHE = {}


def kernel(**inputs):
    x = np.ascontiguousarray(np.asarray(inputs["x"], np.float32))
    B, T, _ = x.shape
    NL = int(np.asarray(inputs["w_in"]).shape[0])
    consts = make_consts(T)
    nc, S, PH, es0 = build(T, NL, consts)
    PH["cast"]()
    for l in range(NL):
        PH["p1"](l)
        PH["gla"](l)
        PH["ssd"](l)
        PH["nsa"](l)
        PH["p2"](l)
    es0.close()
    in_maps = []
    for b in range(B):
        m = {"x": x[b]}
        for n, _s in W_SPECS:
            m[n] = np.ascontiguousarray(np.asarray(inputs[n], np.float32))
        m.update(consts)
        in_maps.append(m)
    res = run_bass_kernel_spmd(nc, in_maps, core_ids=list(range(B)))
    return np.stack([np.asarray(res.results[b]["out"], np.float32) for b in range(B)], axis=0)
```
